# Optimizing a Trainium2 kernel written in Bass

```python
import math
import jax
import jax.numpy as jnp
from jax import lax
import numpy as np

D_MODEL = 1024
BATCH = 4
SEQ = 8192
DEPTH = 4

CTX_LEN = 256
GRID_W = 64
CONV_K = 4
NORM_EPS = 1e-6

LRU_W = 1024
LRU_BLOCKS = 8
LRU_BW = LRU_W // LRU_BLOCKS
LRU_C = 8.0

GDN_HEADS = 8
GDN_DK = 128
GDN_DV = 128
GDN_CHUNK = 64
GDN_QKV = 2 * GDN_HEADS * GDN_DK + GDN_HEADS * GDN_DV
GDN_SCALE = GDN_DK ** -0.5

SSD_INNER = 1024
SSD_HEADDIM = 64
SSD_HEADS = SSD_INNER // SSD_HEADDIM
SSD_GROUPS = 2
SSD_STATE = 128
SSD_CHUNK = 128
SSD_XBC = SSD_INNER + 2 * SSD_GROUPS * SSD_STATE

N_BRANCH = 3
BRANCH_W = 1024

N_GROUPS = 4
EXPERTS_PER_GROUP = 8
N_EXPERTS = N_GROUPS * EXPERTS_PER_GROUP
TOP_K = 2
EXPERT_FF = 512
MOE_BLOCK = 128

IN_SIZES = (LRU_W, LRU_W, GDN_QKV, GDN_HEADS * GDN_DV, 2 * GDN_HEADS, 2 * GDN_HEADS,
            SSD_INNER, SSD_XBC, 2 * SSD_HEADS, N_BRANCH * D_MODEL)
IN_WIDTH = sum(IN_SIZES)

kernel_name = "hybrid_lru_gdn_ssd_hmoe_dit"


def _rmsnorm(x, w):
    xf = x.astype(jnp.float32)
    y = xf * lax.rsqrt(jnp.mean(xf * xf, axis=-1, keepdims=True) + NORM_EPS)
    return (y * w.astype(jnp.float32)).astype(x.dtype)


def _l2norm(x):
    xf = x.astype(jnp.float32)
    return (xf * lax.rsqrt(jnp.sum(xf * xf, axis=-1, keepdims=True) + NORM_EPS)).astype(x.dtype)


def _dwconv(x, w, b=None):
    k, ch = w.shape
    y = lax.conv_general_dilated(x, w[:, None, :].astype(x.dtype), window_strides=(1,),
                                 padding=[(k // 2, k - 1 - k // 2)],
                                 dimension_numbers=("NWC", "WIO", "NWC"), feature_group_count=ch)
    return y if b is None else y + b


def _to_colmajor(t, rows):
    b, l = t.shape[:2]
    rest = t.shape[2:]
    return t.reshape(b, rows, GRID_W, *rest).swapaxes(1, 2).reshape(b, l, *rest)


def _from_colmajor(t, rows):
    b, l = t.shape[:2]
    rest = t.shape[2:]
    return t.reshape(b, GRID_W, rows, *rest).swapaxes(1, 2).reshape(b, l, *rest)


def _run_direction(scan_fn, ctx_args, lat_args, state0, reverse):
    if reverse:
        ctx_args = tuple(jnp.flip(a, axis=1) for a in ctx_args)
        lat_args = tuple(jnp.flip(a, axis=1) for a in lat_args)
    y_c, s_c = scan_fn(*ctx_args, state0)
    y_l, _ = scan_fn(*lat_args, s_c)
    if reverse:
        y_c, y_l = jnp.flip(y_c, axis=1), jnp.flip(y_l, axis=1)
    return y_c, y_l


def _lru_scan(log_a, v, h0):
    def combine(e, l):
        return e[0] * l[0], l[0] * e[1] + l[1]
    a_cum, h = lax.associative_scan(combine, (jnp.exp(log_a), v), axis=1)
    h = h + a_cum * h0[:, None, :]
    return h, h[:, -1]


def _lru_gates(u, wa, ba, wi, bi, lam):
    b, l, _ = u.shape
    ub = u.reshape(b, l, LRU_BLOCKS, LRU_BW)
    r = jax.nn.sigmoid((jnp.einsum("blnj,njk->blnk", ub, wa).reshape(b, l, LRU_W) + ba).astype(jnp.float32))
    i = jax.nn.sigmoid((jnp.einsum("blnj,njk->blnk", ub, wi).reshape(b, l, LRU_W) + bi).astype(jnp.float32))
    log_a = -LRU_C * r * jax.nn.softplus(-lam.astype(jnp.float32))
    v = jnp.sqrt(-jnp.expm1(2.0 * log_a)) * i * u.astype(jnp.float32)
    return log_a, v


def _lru_branch(x_c, y_c, x_l, y_l, conv_w, conv_b, wa, ba, wi, bi, lam):
    u_c = _dwconv(x_c, conv_w, conv_b)
    u_l = _dwconv(x_l, conv_w, conv_b)
    h0 = jnp.zeros((x_c.shape[0], LRU_W), jnp.float32)
    h_c = 0.0
    h_l = 0.0
    for d in range(2):
        c_args = _lru_gates(u_c, wa[d], ba[d], wi[d], bi[d], lam[d])
        l_args = _lru_gates(u_l, wa[d], ba[d], wi[d], bi[d], lam[d])
        o_c, o_l = _run_direction(_lru_scan, c_args, l_args, h0, d == 1)
        h_c = h_c + o_c
        h_l = h_l + o_l
    return (h_c.astype(x_c.dtype) * jax.nn.gelu(y_c),
            h_l.astype(x_l.dtype) * jax.nn.gelu(y_l))


def _gdn_chunk_scan(q, k, v, g, beta, s0):
    b, l, h, _ = q.shape
    dv = v.shape[-1]
    n = l // GDN_CHUNK

    def chunks(t):
        t = t.astype(jnp.float32).reshape(b, n, GDN_CHUNK, h, *t.shape[3:])
        return jnp.moveaxis(jnp.swapaxes(t, 2, 3), 1, 0)

    incl = jnp.tril(jnp.ones((GDN_CHUNK, GDN_CHUNK), bool))
    strict = jnp.tril(jnp.ones((GDN_CHUNK, GDN_CHUNK), bool), -1)
    eye = jnp.eye(GDN_CHUNK, dtype=jnp.float32)

    def step(s, inp):
        qc, kc, vc, gc, bc = inp
        gcs = jnp.cumsum(gc, axis=-1)
        dec = jnp.exp(jnp.where(incl, gcs[..., :, None] - gcs[..., None, :], -jnp.inf))
        kb = kc * bc[..., None]
        a_low = jnp.where(strict, jnp.einsum("bhid,bhjd->bhij", kb, kc) * dec, 0.0)
        rhs = jnp.concatenate([vc * bc[..., None], kb * jnp.exp(gcs)[..., None]], axis=-1)
        sol = lax.linalg.triangular_solve(eye + a_low, rhs, left_side=True, lower=True, unit_diagonal=True)
        u, w = sol[..., :dv], sol[..., dv:]
        v_new = u - jnp.einsum("bhcd,bhde->bhce", w, s)
        attn = jnp.einsum("bhid,bhjd->bhij", qc, kc) * dec
        o = (jnp.einsum("bhcd,bhde->bhce", qc * jnp.exp(gcs)[..., None], s)
             + jnp.einsum("bhij,bhje->bhie", attn, v_new))
        k_dec = kc * jnp.exp(gcs[..., -1:] - gcs)[..., None]
        s = s * jnp.exp(gcs[..., -1])[..., None, None] + jnp.einsum("bhcd,bhce->bhde", k_dec, v_new)
        return s, o

    s_fin, o = lax.scan(step, s0, tuple(chunks(t) for t in (q, k, v, g, beta)))
    o = jnp.moveaxis(o, 0, 1).swapaxes(2, 3).reshape(b, l, h, dv)
    return o, s_fin


def _gdn_prep(qkv, b_raw, a_raw, conv_w, a_log, dt_bias):
    b, l, _ = qkv.shape
    u = jax.nn.silu(_dwconv(qkv, conv_w))
    q, k, v = jnp.split(u, [GDN_HEADS * GDN_DK, 2 * GDN_HEADS * GDN_DK], axis=-1)
    q = _l2norm(q.reshape(b, l, GDN_HEADS, GDN_DK)) * GDN_SCALE
    k = _l2norm(k.reshape(b, l, GDN_HEADS, GDN_DK))
    v = v.reshape(b, l, GDN_HEADS, GDN_DV)
    beta = jax.nn.sigmoid(b_raw.astype(jnp.float32)).reshape(b, l, 2, GDN_HEADS)
    g = -jnp.exp(a_log.astype(jnp.float32)) * jax.nn.softplus(
        a_raw.astype(jnp.float32).reshape(b, l, 2, GDN_HEADS) + dt_bias.astype(jnp.float32))
    return q, k, v, g, beta


def _gdn_branch(qkv_c, z_c, b_c, a_c, qkv_l, z_l, b_l, a_l, conv_w, a_log, dt_bias, norm_w):
    qc, kc, vc, gc, bc = _gdn_prep(qkv_c, b_c, a_c, conv_w, a_log, dt_bias)
    ql, kl, vl, gl, bl = _gdn_prep(qkv_l, b_l, a_l, conv_w, a_log, dt_bias)
    s0 = jnp.zeros((qkv_c.shape[0], GDN_HEADS, GDN_DK, GDN_DV), jnp.float32)
    o_c = 0.0
    o_l = 0.0
    for d in range(2):
        c_args = (qc, kc, vc, gc[:, :, d], bc[:, :, d])
        l_args = (ql, kl, vl, gl[:, :, d], bl[:, :, d])
        y_c, y_l = _run_direction(_gdn_chunk_scan, c_args, l_args, s0, d == 1)
        o_c = o_c + y_c
        o_l = o_l + y_l

    def gated_norm(o, z):
        b, l, _ = z.shape
        y = _rmsnorm(o.astype(z.dtype), norm_w) * jax.nn.silu(z.reshape(b, l, GDN_HEADS, GDN_DV))
        return y.reshape(b, l, GDN_HEADS * GDN_DV)

    return gated_norm(o_c, z_c), gated_norm(o_l, z_l)


def _ssd_chunk_scan(xdt, adt, bm, cm, s0):
    b, l, h, p = xdt.shape
    g, n_st = bm.shape[2], bm.shape[3]
    e = h // g
    nc = l // SSD_CHUNK
    x = xdt.astype(jnp.float32).reshape(b, nc, SSD_CHUNK, g, e, p)
    a = adt.astype(jnp.float32).reshape(b, nc, SSD_CHUNK, g, e).transpose(0, 1, 3, 4, 2)
    bc = bm.astype(jnp.float32).reshape(b, nc, SSD_CHUNK, g, n_st)
    cc = cm.astype(jnp.float32).reshape(b, nc, SSD_CHUNK, g, n_st)
    incl = jnp.tril(jnp.ones((SSD_CHUNK, SSD_CHUNK), bool))

    def step(s, inp):
        x_c, a_c, b_c, c_c = inp
        acs = jnp.cumsum(a_c, axis=-1)
        lm = jnp.exp(jnp.where(incl, acs[..., :, None] - acs[..., None, :], -jnp.inf))
        cb = jnp.einsum("blgn,bsgn->bgls", c_c, b_c)
        y = jnp.einsum("bgls,bgels,bsgep->blgep", cb, lm, x_c)
        y = y + jnp.einsum("blgn,bgepn,bgel->blgep", c_c, s, jnp.exp(acs))
        s = (s * jnp.exp(acs[..., -1])[..., None, None]
             + jnp.einsum("bsgn,bges,bsgep->bgepn", b_c, jnp.exp(acs[..., -1:] - acs), x_c))
        return s, y

    xs = tuple(jnp.moveaxis(t, 1, 0) for t in (x, a, bc, cc))
    s_fin, y = lax.scan(step, s0, xs)
    return jnp.moveaxis(y, 0, 1).reshape(b, l, h, p), s_fin


def _ssd_prep(xbc, dt_raw, conv_w, conv_b, dt_bias):
    b, l, _ = xbc.shape
    u = jax.nn.silu(_dwconv(xbc, conv_w, conv_b))
    xs, bm, cm = jnp.split(u, [SSD_INNER, SSD_INNER + SSD_GROUPS * SSD_STATE], axis=-1)
    xs = xs.reshape(b, l, SSD_HEADS, SSD_HEADDIM)
    bm = bm.reshape(b, l, SSD_GROUPS, SSD_STATE)
    cm = cm.reshape(b, l, SSD_GROUPS, SSD_STATE)
    dt = jax.nn.softplus(dt_raw.astype(jnp.float32).reshape(b, l, 2, SSD_HEADS) + dt_bias.astype(jnp.float32))
    return xs, bm, cm, dt


def _ssd_branch(z_c, xbc_c, sdt_c, z_l, xbc_l, sdt_l, rows, conv_w, conv_b, a_log, dt_bias, d_skip, norm_w):
    xs_c, bm_c, cm_c, dt_c = _ssd_prep(xbc_c, sdt_c, conv_w, conv_b, dt_bias)
    xs_l, bm_l, cm_l, dt_l = _ssd_prep(_to_colmajor(xbc_l, rows), _to_colmajor(sdt_l, rows),
                                       conv_w, conv_b, dt_bias)
    a = -jnp.exp(a_log.astype(jnp.float32))
    e = SSD_HEADS // SSD_GROUPS
    s0 = jnp.zeros((xbc_c.shape[0], SSD_GROUPS, e, SSD_HEADDIM, SSD_STATE), jnp.float32)
    y_c = 0.0
    y_l = 0.0
    for d in range(2):
        c_args = (xs_c * dt_c[:, :, d, :, None], a[d] * dt_c[:, :, d], bm_c, cm_c)
        l_args = (xs_l * dt_l[:, :, d, :, None], a[d] * dt_l[:, :, d], bm_l, cm_l)
        o_c, o_l = _run_direction(_ssd_chunk_scan, c_args, l_args, s0, d == 1)
        y_c = y_c + o_c + d_skip[d][:, None] * xs_c
        y_l = y_l + o_l + d_skip[d][:, None] * xs_l
    y_l = _from_colmajor(y_l, rows)

    def gated_norm(y, z):
        b, l, _ = z.shape
        yz = y.reshape(b, l, SSD_INNER).astype(z.dtype) * jax.nn.silu(z)
        yz = _rmsnorm(yz.reshape(b, l, SSD_GROUPS, SSD_INNER // SSD_GROUPS),
                      norm_w.reshape(SSD_GROUPS, SSD_INNER // SSD_GROUPS))
        return yz.reshape(b, l, SSD_INNER)

    return gated_norm(y_c, z_c), gated_norm(y_l, z_l)


def _merge(branches, gate_pre, w_branch, w_out):
    b, l, gw = gate_pre.shape
    gates = jax.nn.sigmoid(gate_pre.astype(jnp.float32)).astype(gate_pre.dtype).reshape(b, l, N_BRANCH, gw // N_BRANCH)
    acc = 0.0
    for k in range(N_BRANCH):
        acc = acc + gates[:, :, k] * (branches[k] @ w_branch[k])
    return acc @ w_out


def _token_mixer(h_c, h_l, rows, need_ctx_out, w_in, lru_conv_w, lru_conv_b, lru_wa, lru_ba, lru_wi, lru_bi,
                 lru_lambda, gdn_conv_w, gdn_a_log, gdn_dt_bias, gdn_norm_w, ssd_conv_w, ssd_conv_b, ssd_a_log,
                 ssd_dt_bias, ssd_d, ssd_norm_w, w_branch, w_out):
    offs = np.cumsum(IN_SIZES)[:-1].tolist()
    w_parts = jnp.split(w_in, offs, axis=-1)
    (lx_c, ly_c, qkv_c, gz_c, gb_c, ga_c, sz_c, xbc_c, sdt_c, gate_c) = [h_c @ w for w in w_parts]
    (lx_l, ly_l, qkv_l, gz_l, gb_l, ga_l, sz_l, xbc_l, sdt_l, gate_l) = [h_l @ w for w in w_parts]
    lru_c, lru_l = _lru_branch(lx_c, ly_c, lx_l, ly_l, lru_conv_w, lru_conv_b, lru_wa, lru_ba, lru_wi, lru_bi,
                               lru_lambda)
    gdn_c, gdn_l = _gdn_branch(qkv_c, gz_c, gb_c, ga_c, qkv_l, gz_l, gb_l, ga_l, gdn_conv_w, gdn_a_log,
                               gdn_dt_bias, gdn_norm_w)
    ssd_c, ssd_l = _ssd_branch(sz_c, xbc_c, sdt_c, sz_l, xbc_l, sdt_l, rows, ssd_conv_w, ssd_conv_b, ssd_a_log,
                               ssd_dt_bias, ssd_d, ssd_norm_w)
    out_l = _merge((lru_l, gdn_l, ssd_l), gate_l, w_branch, w_out)
    out_c = _merge((lru_c, gdn_c, ssd_c), gate_c, w_branch, w_out) if need_ctx_out else None
    return out_c, out_l


def _hier_moe(h, wg, bg, we, be, w1, w3, w2):
    t, d = h.shape
    g_logits = (h @ wg).astype(jnp.float32) + bg.astype(jnp.float32)
    g_prob = jax.nn.softmax(g_logits, axis=-1)
    g_idx = jnp.argmax(g_logits, axis=-1)
    p_group = jnp.take_along_axis(g_prob, g_idx[:, None], axis=1)[:, 0]
    e_logits = ((h @ we).astype(jnp.float32) + be.astype(jnp.float32)).reshape(t, N_GROUPS, EXPERTS_PER_GROUP)
    e_in_group = jnp.take_along_axis(e_logits, g_idx[:, None, None], axis=1)[:, 0]
    top_v, top_i = lax.top_k(e_in_group, TOP_K)
    weights = jax.nn.softmax(top_v, axis=-1) * p_group[:, None]
    expert_id = (g_idx[:, None] * EXPERTS_PER_GROUP + top_i).reshape(-1).astype(jnp.int32)
    token_id = jnp.repeat(jnp.arange(t, dtype=jnp.int32), TOP_K)
    weight = weights.reshape(-1)
    n_assign = t * TOP_K
    order = jnp.argsort(expert_id)
    e_sorted = expert_id[order]
    counts = jnp.bincount(expert_id, length=N_EXPERTS)
    padded = (counts + MOE_BLOCK - 1) // MOE_BLOCK * MOE_BLOCK
    starts = jnp.cumsum(counts) - counts
    pad_ends = jnp.cumsum(padded)
    pad_starts = pad_ends - padded
    dest = pad_starts[e_sorted] + jnp.arange(n_assign, dtype=jnp.int32) - starts[e_sorted]
    n_blocks = -(-(n_assign + N_EXPERTS * (MOE_BLOCK - 1)) // MOE_BLOCK)
    n_rows = n_blocks * MOE_BLOCK
    tok_buf = jnp.full((n_rows,), t, jnp.int32).at[dest].set(token_id[order])
    w_buf = jnp.zeros((n_rows,), jnp.float32).at[dest].set(weight[order])
    block_start = jnp.arange(n_blocks, dtype=jnp.int32) * MOE_BLOCK
    block_expert = jnp.minimum(jnp.sum(block_start[:, None] >= pad_ends[None, :], axis=1), N_EXPERTS - 1)
    h_pad = jnp.concatenate([h, jnp.zeros((1, d), h.dtype)], axis=0)
    xb = h_pad[tok_buf].reshape(n_blocks, MOE_BLOCK, d)

    def expert_block(args):
        xblk, e = args
        return (jax.nn.silu(xblk @ w1[e]) * (xblk @ w3[e])) @ w2[e]

    yb = lax.map(expert_block, (xb, block_expert)).reshape(n_rows, d)
    y = jnp.zeros((t + 1, d), h.dtype).at[tok_buf].add(yb * w_buf[:, None].astype(h.dtype))
    return y[:t]


def setup_inputs(seed: int = 0) -> dict:
    key = jax.random.key(seed)
    ks = jax.random.split(key, 40)
    idx = iter(range(40))

    def nrm(shape, scale):
        return jax.random.normal(ks[next(idx)], shape, jnp.float32) * scale

    def unif(shape, lo, hi):
        return jax.random.uniform(ks[next(idx)], shape, jnp.float32, lo, hi)

    def gain(shape):
        return 1.0 + nrm(shape, 0.02)

    def dt_bias(shape):
        dt = jnp.exp(unif(shape, math.log(1e-3), math.log(1e-1)))
        return dt + jnp.log(-jnp.expm1(-dt))

    def lru_lambda(shape):
        s = unif(shape, 0.9, 0.999) ** (1.0 / LRU_C)
        return jnp.log(s) - jnp.log1p(-s)

    d = D_MODEL
    return {
        "x": nrm((BATCH, SEQ, d), 1.0),
        "c": nrm((BATCH, d), 1.0),
        "ctx": nrm((BATCH, CTX_LEN, d), 1.0),
        "c_ctx": nrm((d,), 1.0),
        "w_mod": nrm((DEPTH, d, 6 * d), 0.5 * d ** -0.5),
        "b_mod": nrm((DEPTH, 6 * d), 0.01),
        "norm1_w": gain((DEPTH, d)),
        "norm2_w": gain((DEPTH, d)),
        "w_in": nrm((DEPTH, d, IN_WIDTH), d ** -0.5),
        "lru_conv_w": nrm((DEPTH, CONV_K, LRU_W), CONV_K ** -0.5),
        "lru_conv_b": nrm((DEPTH, LRU_W), 0.01),
        "lru_wa": nrm((DEPTH, 2, LRU_BLOCKS, LRU_BW, LRU_BW), LRU_BW ** -0.5),
        "lru_ba": nrm((DEPTH, 2, LRU_W), 0.01),
        "lru_wi": nrm((DEPTH, 2, LRU_BLOCKS, LRU_BW, LRU_BW), LRU_BW ** -0.5),
        "lru_bi": nrm((DEPTH, 2, LRU_W), 0.01),
        "lru_lambda": lru_lambda((DEPTH, 2, LRU_W)),
        "gdn_conv_w": nrm((DEPTH, CONV_K, GDN_QKV), CONV_K ** -0.5),
        "gdn_a_log": jnp.log(unif((DEPTH, 2, GDN_HEADS), 1.0, 16.0)),
        "gdn_dt_bias": dt_bias((DEPTH, 2, GDN_HEADS)),
        "gdn_norm_w": gain((DEPTH, GDN_DV)),
        "ssd_conv_w": nrm((DEPTH, CONV_K, SSD_XBC), CONV_K ** -0.5),
        "ssd_conv_b": nrm((DEPTH, SSD_XBC), 0.01),
        "ssd_a_log": jnp.log(unif((DEPTH, 2, SSD_HEADS), 1.0, 16.0)),
        "ssd_dt_bias": dt_bias((DEPTH, 2, SSD_HEADS)),
        "ssd_d": 1.0 + nrm((DEPTH, 2, SSD_HEADS), 0.1),
        "ssd_norm_w": gain((DEPTH, SSD_INNER)),
        "w_branch": nrm((DEPTH, N_BRANCH, BRANCH_W, d), BRANCH_W ** -0.5),
        "w_out": nrm((DEPTH, d, d), d ** -0.5),
        "router_group_w": nrm((DEPTH, d, N_GROUPS), d ** -0.5),
        "router_group_b": nrm((DEPTH, N_GROUPS), 0.01),
        "router_expert_w": nrm((DEPTH, d, N_EXPERTS), d ** -0.5),
        "router_expert_b": nrm((DEPTH, N_EXPERTS), 0.01),
        "expert_w1": nrm((DEPTH, N_EXPERTS, d, EXPERT_FF), d ** -0.5),
        "expert_w3": nrm((DEPTH, N_EXPERTS, d, EXPERT_FF), d ** -0.5),
        "expert_w2": nrm((DEPTH, N_EXPERTS, EXPERT_FF, d), EXPERT_FF ** -0.5),
        "final_norm_w": gain((d,)),
    }


def reference(x, c, ctx, c_ctx, w_mod, b_mod, norm1_w, norm2_w, w_in, lru_conv_w, lru_conv_b, lru_wa, lru_ba,
              lru_wi, lru_bi, lru_lambda, gdn_conv_w, gdn_a_log, gdn_dt_bias, gdn_norm_w, ssd_conv_w, ssd_conv_b,
              ssd_a_log, ssd_dt_bias, ssd_d, ssd_norm_w, w_branch, w_out, router_group_w, router_group_b,
              router_expert_w, router_expert_b, expert_w1, expert_w3, expert_w2, final_norm_w):
    bsz, seq, d = x.shape
    rows = seq // GRID_W
    n_ctx_tok = bsz * ctx.shape[1]
    x_l, x_c = x, ctx
    act_l = jax.nn.silu(c)
    act_c = jax.nn.silu(c_ctx)
    for i in range(DEPTH):
        last = i == DEPTH - 1
        mod_l = jnp.split((act_l @ w_mod[i] + b_mod[i])[:, None, :], 6, axis=-1)
        mod_c = jnp.split((act_c @ w_mod[i] + b_mod[i])[None, None, :], 6, axis=-1)
        h_l = _rmsnorm(x_l, norm1_w[i]) * (1 + mod_l[1]) + mod_l[0]
        h_c = _rmsnorm(x_c, norm1_w[i]) * (1 + mod_c[1]) + mod_c[0]
        mix_c, mix_l = _token_mixer(h_c, h_l, rows, not last, w_in[i], lru_conv_w[i], lru_conv_b[i], lru_wa[i],
                                    lru_ba[i], lru_wi[i], lru_bi[i], lru_lambda[i], gdn_conv_w[i], gdn_a_log[i],
                                    gdn_dt_bias[i], gdn_norm_w[i], ssd_conv_w[i], ssd_conv_b[i], ssd_a_log[i],
                                    ssd_dt_bias[i], ssd_d[i], ssd_norm_w[i], w_branch[i], w_out[i])
        x_l = x_l + mod_l[2] * mix_l
        h_l = _rmsnorm(x_l, norm2_w[i]) * (1 + mod_l[4]) + mod_l[3]
        moe_w = (router_group_w[i], router_group_b[i], router_expert_w[i], router_expert_b[i],
                 expert_w1[i], expert_w3[i], expert_w2[i])
        if last:
            x_l = x_l + mod_l[5] * _hier_moe(h_l.reshape(-1, d), *moe_w).reshape(x_l.shape)
        else:
            x_c = x_c + mod_c[2] * mix_c
            h_c = _rmsnorm(x_c, norm2_w[i]) * (1 + mod_c[4]) + mod_c[3]
            y = _hier_moe(jnp.concatenate([h_c.reshape(-1, d), h_l.reshape(-1, d)], axis=0), *moe_w)
            x_c = x_c + mod_c[5] * y[:n_ctx_tok].reshape(x_c.shape)
            x_l = x_l + mod_l[5] * y[n_ctx_tok:].reshape(x_l.shape)
    return _rmsnorm(x_l, final_norm_w)
```

```python
import numpy as np
from contextlib import ExitStack
import concourse.bass as bass
import concourse.mybir as mybir
from concourse.bass_utils import run_bass_kernel_spmd

F32 = mybir.dt.float32
BF16 = mybir.dt.bfloat16
I32 = mybir.dt.int32
ALU = mybir.AluOpType
AF = mybir.ActivationFunctionType
AX = mybir.AxisListType

D = 1024
KC = 8
NEXP = 32
NGRP = 4
EPG = 8
EPS = 1e-6
IN_W = 11840
SAME_ENG_SYNC = True


class Buf:
    def __init__(self, t, name):
        self.t = t
        self.name = name
        self.lw = None
        self.rd = {}

    def __getitem__(self, k):
        return self.t[k]


class CutHere(Exception):
    pass


class KB:
    ENG = ["pe", "act", "dve", "pool", "sp"]

    def __init__(self, nc, es):
        self.nc = nc
        self.es = es
        self.stream = {e: [] for e in self.ENG}
        self.semh = {}
        self.cnt = {e: 0 for e in self.ENG}
        self.waited = {e: {} for e in self.ENG}
        for e in self.ENG:
            self.semh["c_" + e] = es.enter_context(nc.semaphore("c_" + e))
        self.slots = {}
        self.slot_i = {}
        for q, n in (("sp", 8), ("act", 4), ("pool", 6)):
            self.slots[q] = []
            for i in range(n):
                key = "d_%s%d" % (q, i)
                self.semh[key] = es.enter_context(nc.semaphore(key))
                self.slots[q].append([key, 0])
            self.slot_i[q] = 0
        self.nins = 0

    def sb(self, st, name, shape, dt):
        self.uid = getattr(self, "uid", 0) + 1
        name = "%s_%d" % (name, self.uid)
        return Buf(st.enter_context(self.nc.sbuf_tensor(name, list(shape), dt)), name)

    def ps(self, st, name, shape, dt=F32):
        self.uid = getattr(self, "uid", 0) + 1
        name = "%s_%d" % (name, self.uid)
        return Buf(st.enter_context(self.nc.psum_tensor(name, list(shape), dt)), name)

    def _deps(self, r, w):
        deps = {}

        def add(s, v):
            if deps.get(s, 0) < v:
                deps[s] = v
        for b in r:
            if b.lw is not None:
                add(*b.lw)
        for b in w:
            if b.lw is not None:
                add(*b.lw)
            for s, v in b.rd.items():
                add(s, v)
        return deps

    def _filter(self, eng, deps):
        out = []
        wd = self.waited[eng]
        for s, v in deps.items():
            if s == "c_" + eng and (eng == "pe" or not SAME_ENG_SYNC):
                continue
            if wd.get(s, 0) >= v:
                continue
            wd[s] = v
            out.append((s, v))
        return out

    def capture(self):
        self._cap = []

    def end_capture(self):
        c = self._cap
        self._cap = None
        return c

    def emit_rr(self, lists):
        its = [iter(l) for l in lists if l]
        while its:
            for it in list(its):
                try:
                    item = next(it)
                except StopIteration:
                    its.remove(it)
                    continue
                if item[0] == "op":
                    self.op(*item[1:])
                else:
                    self.dma(item[1], item[2], item[3], r=item[4], w=item[5], **item[6])

    def op(self, eng, fn, r=(), w=()):
        if getattr(self, "_cap", None) is not None:
            self._cap.append(("op", eng, fn, r, w))
            return
        waits = self._filter(eng, self._deps(r, w))
        self.cnt[eng] += 1
        seq = self.cnt[eng]
        key = "c_" + eng
        self.stream[eng].append((waits, fn, key, 1))
        for b in r:
            b.rd[key] = seq
        for b in w:
            b.lw = (key, seq)
            b.rd = {}
        self.nins += 1

    def dma(self, q, out, in_, r=(), w=(), **kw):
        if getattr(self, "_cap", None) is not None:
            self._cap.append(("dma", q, out, in_, r, w, kw))
            return
        sl = self.slots[q][self.slot_i[q]]
        self.slot_i[q] = (self.slot_i[q] + 1) % len(self.slots[q])
        deps = self._deps(r, w)
        if sl[1] > 0 and deps.get(sl[0], 0) < 16 * sl[1]:
            deps[sl[0]] = 16 * sl[1]
        waits = self._filter(q, deps)
        sl[1] += 1
        val = 16 * sl[1]
        self.stream[q].append((waits, lambda e: e.dma_start(out=out, in_=in_, **kw), sl[0], 16))
        for b in r:
            b.rd[sl[0]] = val
        for b in w:
            b.lw = (sl[0], val)
            b.rd = {}
        self.nins += 1

    def barrier(self):
        allv = {}
        for e in self.ENG:
            if self.cnt[e] > 0:
                allv["c_" + e] = self.cnt[e]
        for q in self.slots:
            for key, uses in self.slots[q]:
                if uses > 0:
                    allv[key] = 16 * uses
        for e in self.ENG:
            d = {s: v for s, v in allv.items() if s != "c_" + e}
            waits = self._filter(e, d)
            if waits:
                self.stream[e].append((waits, None, None, 0))

    def flush(self):
        nc = self.nc
        semh = self.semh
        with nc.Block() as block:
            for eng, deco in (("pe", block.tensor), ("act", block.scalar), ("dve", block.vector),
                              ("pool", block.gpsimd), ("sp", block.sync)):
                items = self.stream[eng]
                self.stream[eng] = []
                if not items:
                    continue

                def body(e, items=items):
                    for waits, fn, key, inc in items:
                        for s, v in waits:
                            e.wait_ge(semh[s], v)
                        if fn is not None:
                            fn(e).then_inc(semh[key], inc)
                deco(body)


def groups_of(n, g=512):
    out = []
    s = 0
    while s < n:
        out.append((s, min(g, n - s)))
        s += g
    return out


SEGS = [(0, 48, 0), (6176, 20, 48 * 128), (8768, 24, 68 * 128)]
P_LX, P_LY, P_Q, P_K, P_V, P_GZ = 0, 1024, 2048, 3072, 4096, 5120
P_SZ, P_XS, P_BM, P_CM, P_GATE = 6144, 7168, 8192, 8448, 8704
P_ROWS = 92 * 128


class Cfg:
    def __init__(self, Lc=256, Ll=8192, depth=4, ff=512, dbg=()):
        self.Lc, self.Ll, self.depth, self.ff = Lc, Ll, depth, ff
        self.T = Lc + Ll
        self.rows = Ll // 64
        self.dbg = tuple(dbg)


def build(cfg):
    nc = bass.Bass("TRN2", target_bir_lowering=False)
    T, Lc, Ll, DEPTH, FF = cfg.T, cfg.Lc, cfg.Ll, cfg.depth, cfg.ff
    NT = T // 128
    dt_in = {}

    def din(name, shape):
        dt_in[name] = nc.dram_tensor(name, list(shape), F32, kind="ExternalInput").ap()
        return dt_in[name]

    x_in = din("x", [Ll, D])
    c_in = din("c", [1, D])
    ctx_in = din("ctx", [Lc, D])
    cctx_in = din("c_ctx", [1, D])
    w_mod = din("w_mod", [DEPTH, D, 6 * D])
    b_mod = din("b_mod", [DEPTH, 6 * D])
    norm1_w = din("norm1_w", [DEPTH, D])
    norm2_w = din("norm2_w", [DEPTH, D])
    w_in = din("w_in", [DEPTH, D, IN_W])
    lru_conv_w = din("lru_conv_w", [DEPTH, 4, 1024])
    lru_conv_b = din("lru_conv_b", [DEPTH, 1024])
    lru_wa = din("lru_wa", [DEPTH, 2, 8, 128, 128])
    lru_ba = din("lru_ba", [DEPTH, 2, 1024])
    lru_wi = din("lru_wi", [DEPTH, 2, 8, 128, 128])
    lru_bi = din("lru_bi", [DEPTH, 2, 1024])
    lru_lambda = din("lru_lambda", [DEPTH, 2, 1024])
    gdn_conv_w = din("gdn_conv_w", [DEPTH, 4, 3072])
    gdn_a_log = din("gdn_a_log", [DEPTH, 16])
    gdn_dt_bias = din("gdn_dt_bias", [DEPTH, 16])
    gdn_norm_w = din("gdn_norm_w", [DEPTH, 128])
    ssd_conv_w = din("ssd_conv_w", [DEPTH, 4, 1536])
    ssd_conv_b = din("ssd_conv_b", [DEPTH, 1536])
    ssd_a_log = din("ssd_a_log", [DEPTH, 32])
    ssd_dt_bias = din("ssd_dt_bias", [DEPTH, 32])
    ssd_d = din("ssd_d", [DEPTH, 32])
    ssd_norm_w = din("ssd_norm_w", [DEPTH, 1024])
    w_branch = din("w_branch", [DEPTH, 3, 1024, D])
    w_out = din("w_out", [DEPTH, D, D])
    rg_w = din("router_group_w", [DEPTH, D, NGRP])
    rg_b = din("router_group_b", [DEPTH, NGRP])
    re_w = din("router_expert_w", [DEPTH, D, NEXP])
    re_b = din("router_expert_b", [DEPTH, NEXP])
    ew1 = din("expert_w1", [DEPTH, NEXP, D, FF])
    ew3 = din("expert_w3", [DEPTH, NEXP, D, FF])
    ew2 = din("expert_w2", [DEPTH, NEXP, FF, D])
    fin_w = din("final_norm_w", [1, D])

    out_d = nc.dram_tensor("out", [Ll, D], F32, kind="ExternalOutput").ap()
    dbg_out = {}

    def dbg_tensor(name, shape, dt=F32):
        dbg_out[name] = nc.dram_tensor("dbg_" + name, list(shape), dt, kind="ExternalOutput").ap()
        return dbg_out[name]

    def scratch(name, shape, dt):
        if name in cfg.dbg:
            return dbg_tensor(name, shape, dt)
        return nc.dram_tensor("s_" + name, list(shape), dt, kind="Internal").ap()

    XT = scratch("XT", [D, T], F32)
    P = scratch("P", [P_ROWS, T], BF16)
    PS = scratch("PS", [T, 64], F32)
    BR = scratch("BR", [3, 1024, T], BF16)
    OG = scratch("OG", [2, T, 1024], F32)
    YS = scratch("YS", [2, T, 1024], F32)
    XC = scratch("XC", [1536, T], BF16)
    RW = scratch("RW", [T, NEXP], F32)

    with ExitStack() as es:
        kb = KB(nc, es)
        ident_f = kb.sb(es, "ident_f", [128, 128], F32)
        ident_b = kb.sb(es, "ident_b", [128, 128], BF16)
        ones_f = kb.sb(es, "ones_f", [128, 128], F32)
        ones_b = kb.sb(es, "ones_b", [128, 128], BF16)
        act_lc = kb.sb(es, "act_lc", [128, KC, 2], F32)
        modv = kb.sb(es, "modv", [128, 48, 2], F32)
        AB = kb.sb(es, "AB", [128, 2, 2, KC, 2], F32)

        def mk_ident(t, dt):
            kb.op("pool", lambda e: e.memset(t[:], 0.0), w=[t])
            kb.op("pool", lambda e: e.affine_select(out=t[:], in_=t[:], pattern=[[-1, 128]],
                                                     compare_op=ALU.not_equal, fill=1.0, base=0,
                                                     channel_multiplier=1), r=[t], w=[t])
        mk_ident(ident_f, F32)
        mk_ident(ident_b, BF16)
        kb.op("pool", lambda e: e.memset(ones_f[:], 1.0), w=[ones_f])
        kb.op("pool", lambda e: e.memset(ones_b[:], 1.0), w=[ones_b])

        def load_vec_fm(st, pst, dst, dst_ap, src2d, n):
            tmp = kb.sb(st, "lv_tmp%d" % kb.nins, [n, 128], F32)
            kb.dma("sp", tmp[:], src2d, w=[tmp])
            kb.op("pe", lambda e: e.transpose(pst[:, 0:n], tmp[:], ident_f[0:n, 0:n]), r=[tmp, ident_f], w=[pst])
            kb.op("dve", lambda e: e.tensor_copy(out=dst_ap, in_=pst[:, 0:n]), r=[pst], w=[dst])

        with ExitStack() as st:
            xin = [kb.sb(st, "xin%d" % i, [128, D], F32) for i in range(2)]
            xo = [kb.sb(st, "xo%d" % i, [128, KC, 128], F32) for i in range(2)]
            pt = [kb.ps(st, "pt%d" % i, [128, 4, 128]) for i in range(4)]
            cv = kb.sb(st, "cv", [128, KC, 2], F32)
            sg = kb.sb(st, "sg", [128, KC, 2], F32)
            ptv = kb.ps(st, "ptv", [128, 128])
            load_vec_fm(st, ptv, cv, cv[:, :, 0], c_in.rearrange("o (k p) -> (o k) p", p=128), KC)
            load_vec_fm(st, ptv, cv, cv[:, :, 1], cctx_in.rearrange("o (k p) -> (o k) p", p=128), KC)
            kb.op("act", lambda e: e.activation(out=sg[:], in_=cv[:], func=AF.Sigmoid), r=[cv], w=[sg])
            kb.op("dve", lambda e: e.tensor_tensor(out=act_lc[:], in0=cv[:], in1=sg[:], op=ALU.mult),
                  r=[cv, sg], w=[act_lc])
            for ti in range(NT):
                src = ctx_in[ti * 128:(ti + 1) * 128, :] if ti < Lc // 128 else \
                    x_in[ti * 128 - Lc:(ti + 1) * 128 - Lc, :]
                xi = xin[ti % 2]
                xoo = xo[ti % 2]
                kb.dma("sp", xi[:], src, w=[xi])
                for hh in range(2):
                    p = pt[(ti * 2 + hh) % 4]
                    for j in range(4):
                        kc = hh * 4 + j
                        kb.op("pe", lambda e, p=p, j=j, kc=kc, xi=xi: e.transpose(
                            p[:, j, :], xi[:, kc * 128:(kc + 1) * 128], ident_f[:]), r=[xi, ident_f], w=[p])
                    eng = "act" if hh == 0 else "dve"
                    if eng == "act":
                        kb.op("act", lambda e, p=p, xoo=xoo, hh=hh: e.copy(out=xoo[:, hh * 4:(hh + 1) * 4, :], in_=p[:]),
                              r=[p], w=[xoo])
                    else:
                        kb.op("dve", lambda e, p=p, xoo=xoo, hh=hh: e.tensor_copy(out=xoo[:, hh * 4:(hh + 1) * 4, :], in_=p[:]),
                              r=[p], w=[xoo])
                kb.dma("sp", XT.rearrange("(k p) t -> p k t", p=128)[:, :, ti * 128:(ti + 1) * 128], xoo[:], r=[xoo])
            kb.barrier()
            kb.flush()

        for li in range(DEPTH):
            last = li == DEPTH - 1
            if "stop0" in cfg.dbg:
                break
            with ExitStack() as st:
                wm = [kb.sb(st, "wm%d" % i, [128, KC, 512], F32) for i in range(2)]
                pm = kb.ps(st, "pm", [128, 48, 2])
                ptm = kb.ps(st, "ptm", [128, 128])
                bm = kb.sb(st, "bm", [128, 48], F32)
                nw = kb.sb(st, "nw", [128, 2, KC], F32)
                load_vec_fm(st, ptm, bm, bm[:], b_mod[li].rearrange("(k p) -> k p", p=128), 48)
                load_vec_fm(st, ptm, nw, nw[:, 0, :], norm1_w[li].rearrange("(k p) -> k p", p=128), KC)
                load_vec_fm(st, ptm, nw, nw[:, 1, :], norm2_w[li].rearrange("(k p) -> k p", p=128), KC)
                for cb in range(12):
                    w = wm[cb % 2]
                    kb.dma("sp" if cb % 2 == 0 else "pool", w[:],
                           w_mod[li].rearrange("(k p) n -> p k n", p=128)[:, :, cb * 512:(cb + 1) * 512], w=[w])
                    for j in range(4):
                        cc = cb * 4 + j
                        for kc in range(KC):
                            kb.op("pe", lambda e, w=w, j=j, kc=kc, cc=cc: e.matmul(
                                pm[:, cc, :], lhsT=w[:, kc, j * 128:(j + 1) * 128], rhs=act_lc[:, kc, :],
                                start=(kc == 0), stop=(kc == KC - 1)), r=[w, act_lc], w=[pm])
                kb.op("dve", lambda e: e.tensor_tensor(out=modv[:], in0=pm[:],
                                                        in1=bm[:].unsqueeze(2).to_broadcast([128, 48, 2]), op=ALU.add),
                      r=[pm, bm], w=[modv])
                for ni, (sh, sc) in enumerate(((0, 1), (3, 4))):
                    kb.op("dve", lambda e, ni=ni, sc=sc: e.scalar_tensor_tensor(
                        out=AB[:, ni, 0, :, :], in0=modv[:, sc * 8:(sc + 1) * 8, :], scalar=1.0,
                        in1=nw[:, ni, :].unsqueeze(2).to_broadcast([128, KC, 2]), op0=ALU.add, op1=ALU.mult),
                        r=[modv, nw], w=[AB])
                    kb.op("dve", lambda e, ni=ni, sh=sh: e.tensor_copy(
                        out=AB[:, ni, 1, :, :], in_=modv[:, sh * 8:(sh + 1) * 8, :]), r=[modv], w=[AB])
                kb.barrier()
                kb.flush()

            if "stopM" in cfg.dbg:
                break
            halves = [(0, T)] if T <= 4224 else [(0, T // 2 // 128 * 128), (T // 2 // 128 * 128, T)]
            for (h0, h1) in halves:
                HT = h1 - h0
                with ExitStack() as st:
                    hT = kb.sb(st, "hT", [128, KC, HT], BF16)
                    xg = [kb.sb(st, "xg%d" % i, [128, KC, 512], F32) for i in range(2)]
                    sq = [kb.sb(st, "sq%d" % i, [128, KC, 512], F32) for i in range(2)]
                    rs = [kb.sb(st, "rs%d" % i, [128, 512], F32) for i in range(2)]
                    pss = [kb.ps(st, "pss%d" % i, [128, 512]) for i in range(2)]
                    emit_norm(kb, nc, XT, AB, 0, hT, h0, HT, Lc, xg, sq, rs, pss, ones_f)
                    if "stopA1" in cfg.dbg:
                        kb.barrier()
                        kb.flush()
                        break
                    wsf = kb.sb(st, "wsf", [128, KC, 64], F32)
                    wsb = kb.sb(st, "wsb", [128, KC, 64], BF16)
                    wv = w_in[li].rearrange("(k p) n -> p k n", p=128)
                    kb.dma("sp", wsf[:, :, 0:32], wv[:, :, 6144:6176], w=[wsf])
                    kb.dma("sp", wsf[:, :, 32:64], wv[:, :, 8736:8768], w=[wsf])
                    kb.op("pool", lambda e: e.tensor_copy(out=wsb[:], in_=wsf[:]), r=[wsf], w=[wsb])
                    pp = [kb.ps(st, "pp%d" % i, [128, 512]) for i in range(4)]
                    sst = [kb.sb(st, "sst%d" % i, [128, 64], F32) for i in range(2)]
                    for ti in range(HT // 128):
                        p = pp[ti % 4]
                        so = sst[ti % 2]
                        for kc in range(KC):
                            kb.op("pe", lambda e, p=p, kc=kc, ti=ti: e.matmul(
                                p[:, 0:64], lhsT=hT[:, kc, ti * 128:(ti + 1) * 128], rhs=wsb[:, kc, :],
                                start=(kc == 0), stop=(kc == KC - 1)), r=[hT, wsb], w=[p])
                        kb.op("act", lambda e, p=p, so=so: e.copy(out=so[:], in_=p[:, 0:64]), r=[p], w=[so])
                        kb.dma("sp", PS[h0 + ti * 128:h0 + (ti + 1) * 128, :], so[:], r=[so])
                    if "stopA2" in cfg.dbg:
                        kb.barrier()
                        kb.flush()
                        break
                    wf = [kb.sb(st, "wf%d" % i, [128, KC, 512], F32) for i in range(2)]
                    wb = [kb.sb(st, "wb%d" % i, [128, KC, 512], BF16) for i in range(2)]
                    ost = [kb.sb(st, "ost%d" % i, [128, 512], BF16) for i in range(4)]
                    blocks = []
                    for (c0, nch, r0) in SEGS:
                        for b in range(nch // 4):
                            blocks.append((c0 + b * 512, r0 + b * 512))
                    cnt = 0
                    for bi, (c0, r0) in enumerate(blocks):
                        f = wf[bi % 2]
                        wbb = wb[bi % 2]
                        kb.dma("sp" if (bi % 2 == 0 or "spOnly" in cfg.dbg) else "pool", f[:], wv[:, :, c0:c0 + 512], w=[f])
                        kb.op("pool", lambda e, f=f, wbb=wbb: e.tensor_copy(out=wbb[:, 0:4, :], in_=f[:, 0:4, :]),
                              r=[f], w=[wbb])
                        kb.op("pool", lambda e, f=f, wbb=wbb: e.tensor_copy(out=wbb[:, 4:8, :], in_=f[:, 4:8, :]),
                              r=[f], w=[wbb])
                        for (g0, gs) in groups_of(HT):
                            for j in range(4):
                                p = pp[cnt % 4]
                                o = ost[cnt % 4]
                                for kc in range(KC):
                                    kb.op("pe", lambda e, p=p, kc=kc, j=j, wbb=wbb, g0=g0, gs=gs: e.matmul(
                                        p[:, 0:gs], lhsT=wbb[:, kc, j * 128:(j + 1) * 128], rhs=hT[:, kc, g0:g0 + gs],
                                        start=(kc == 0), stop=(kc == KC - 1)), r=[wbb, hT], w=[p])
                                if cnt % 2 == 0:
                                    kb.op("act", lambda e, p=p, o=o, gs=gs: e.copy(out=o[:, 0:gs], in_=p[:, 0:gs]),
                                          r=[p], w=[o])
                                else:
                                    kb.op("dve", lambda e, p=p, o=o, gs=gs: e.tensor_copy(out=o[:, 0:gs], in_=p[:, 0:gs]),
                                          r=[p], w=[o])
                                if "noPout" not in cfg.dbg:
                                    kb.dma("sp", P[r0 + j * 128:r0 + (j + 1) * 128, h0 + g0:h0 + g0 + gs], o[:, 0:gs], r=[o])
                                cnt += 1
                    kb.barrier()
                    kb.flush()
            if "stopA" in cfg.dbg:
                break
            W = dict(lru_conv_w=lru_conv_w, lru_conv_b=lru_conv_b, lru_wa=lru_wa, lru_ba=lru_ba, lru_wi=lru_wi,
                     lru_bi=lru_bi, lru_lambda=lru_lambda, w_branch=w_branch, w_out=w_out, rg_w=rg_w, rg_b=rg_b,
                     re_w=re_w, re_b=re_b, ew1=ew1, ew3=ew3, ew2=ew2)
            C = dict(ident_f=ident_f, ident_b=ident_b, ones_f=ones_f, ones_b=ones_b, modv=modv, AB=AB)
            phase_lru(kb, cfg, li, W, C, P, BR, load_vec_fm)
            if "noGDN" not in cfg.dbg:
                phase_gdn(kb, cfg, li, dict(W, gdn_conv_w=gdn_conv_w, gdn_a_log=gdn_a_log, gdn_dt_bias=gdn_dt_bias,
                                            gdn_norm_w=gdn_norm_w), C, P, PS, OG, BR, load_vec_fm)
            if "stopD" in cfg.dbg:
                break
            if "noSSD" not in cfg.dbg:
                phase_ssd(kb, cfg, li, dict(ssd_conv_w=ssd_conv_w, ssd_conv_b=ssd_conv_b, ssd_a_log=ssd_a_log,
                                            ssd_dt_bias=ssd_dt_bias, ssd_d=ssd_d, ssd_norm_w=ssd_norm_w),
                          C, P, PS, XC, YS, BR, load_vec_fm)
            if "stopS" in cfg.dbg:
                break
            zl = [k for k, f in ((1, "noGDN"), (2, "noSSD")) if f in cfg.dbg]
            if zl:
                with ExitStack() as st:
                    z = kb.sb(st, "zbr", [128, T], BF16)
                    kb.op("pool", lambda e: e.memset(z[:], 0.0), w=[z])
                    for k in zl:
                        for ct in range(8):
                            kb.dma("sp", BR[k, ct * 128:(ct + 1) * 128, :], z[:], r=[z])
                    kb.barrier()
                    kb.flush()
            if "stopL" in cfg.dbg:
                break
            phase_merge(kb, cfg, li, W, C, P, BR, XT)
            if "stopG" in cfg.dbg:
                break
            phase_moe(kb, cfg, li, W, C, XT, load_vec_fm)

        if not any(k.startswith("stop") for k in cfg.dbg):
            phase_final(kb, cfg, C, XT, fin_w, out_d, load_vec_fm)
        kb.barrier()
        kb.flush()
    return nc, dbg_out


def emit_norm(kb, nc, XT, AB, ni, hT, h0, HT, Lc, xg, sq, rs, pss, ones_f, hbase=None):
    XTv = XT.rearrange("(k p) t -> p k t", p=128)
    for gi, (g0, gs) in enumerate(groups_of(HT)):
        x = xg[gi % len(xg)]
        s = sq[gi % len(sq)]
        r = rs[gi % len(rs)]
        p = pss[gi % len(pss)]
        kb.dma("sp" if gi % 2 == 0 else "pool", x[:, :, 0:gs], XTv[:, :, h0 + g0:h0 + g0 + gs], w=[x])
        kb.op("act", lambda e, x=x, s=s, gs=gs: e.activation(out=s[:, :, 0:gs], in_=x[:, :, 0:gs], func=AF.Square),
              r=[x], w=[s])
        for kc in range(KC):
            kb.op("pe", lambda e, p=p, s=s, kc=kc, gs=gs: e.matmul(p[:, 0:gs], lhsT=ones_f[:], rhs=s[:, kc, 0:gs],
                                                                  start=(kc == 0), stop=(kc == KC - 1)),
                  r=[s, ones_f], w=[p])
        kb.op("act", lambda e, p=p, r=r, gs=gs: e.activation(out=r[:, 0:gs], in_=p[:, 0:gs], func=AF.Sqrt,
                                                            scale=1.0 / D, bias=EPS), r=[p], w=[r])
        kb.op("dve", lambda e, r=r, gs=gs: e.reciprocal(out=r[:, 0:gs], in_=r[:, 0:gs]), r=[r], w=[r])
        kb.op("dve", lambda e, x=x, r=r, gs=gs: e.tensor_tensor(
            out=x[:, :, 0:gs], in0=x[:, :, 0:gs], in1=r[:, 0:gs].unsqueeze(1).to_broadcast([128, KC, gs]),
            op=ALU.mult), r=[x, r], w=[x])
        a0 = h0 + g0
        segs = []
        if a0 < Lc:
            segs.append((0, min(gs, Lc - a0), 1))
        if a0 + gs > Lc:
            segs.append((max(0, Lc - a0), gs, 0))
        for kc in range(KC):
            for (s0, s1, which) in segs:
                eng = "pool" if kc % 2 == 0 else "dve"
                kb.op(eng, lambda e, x=x, kc=kc, s0=s0, s1=s1, which=which, g0=g0: e.tensor_scalar(
                    out=hT[:, kc, g0 + s0:g0 + s1], in0=x[:, kc, s0:s1],
                    scalar1=AB[:, ni, 0, kc, which:which + 1], scalar2=AB[:, ni, 1, kc, which:which + 1],
                    op0=ALU.mult, op1=ALU.add), r=[x, AB], w=[hT])


def core_inputs(inp, b, cfg):
    m = {}
    for k, v in inp.items():
        v = np.asarray(v)
        if k == "x":
            m[k] = np.ascontiguousarray(v[b], dtype=np.float32)
        elif k == "c":
            m[k] = np.ascontiguousarray(v[b:b + 1], dtype=np.float32)
        elif k == "ctx":
            m[k] = np.ascontiguousarray(v[b], dtype=np.float32)
        elif k in ("c_ctx", "final_norm_w"):
            m[k] = np.ascontiguousarray(v.reshape(1, -1), dtype=np.float32)
        elif k in ("gdn_a_log", "gdn_dt_bias", "ssd_a_log", "ssd_dt_bias", "ssd_d"):
            m[k] = np.ascontiguousarray(v.reshape(v.shape[0], -1), dtype=np.float32)
        else:
            m[k] = np.ascontiguousarray(v, dtype=np.float32)
    return m


def conv_fm(kb, eng, acc, x, cw, taps, bias_ap, segs, bias_buf=None):
    for (s0, s1) in segs:
        if bias_ap is not None:
            kb.op(eng, lambda e, s0=s0, s1=s1: e.tensor_scalar(out=acc[:, s0:s1], in0=x[:, s0:s1], scalar1=taps[2],
                                                              scalar2=bias_ap, op0=ALU.mult, op1=ALU.add),
                  r=[x, cw, bias_buf], w=[acc])
        else:
            kb.op(eng, lambda e, s0=s0, s1=s1: e.tensor_scalar(out=acc[:, s0:s1], in0=x[:, s0:s1], scalar1=taps[2],
                                                              scalar2=None, op0=ALU.mult), r=[x, cw], w=[acc])
        for j, off in ((0, -2), (1, -1), (3, 1)):
            if off < 0:
                o0, o1, i0, i1 = s0 - off, s1, s0, s1 + off
            else:
                o0, o1, i0, i1 = s0, s1 - off, s0 + off, s1
            kb.op("dve", lambda e, o0=o0, o1=o1, i0=i0, i1=i1, j=j: e.scalar_tensor_tensor(
                out=acc[:, o0:o1], in0=x[:, i0:i1], scalar=taps[j], in1=acc[:, o0:o1], op0=ALU.mult, op1=ALU.add),
                r=[x, cw, acc], w=[acc])


def phase_lru(kb, cfg, li, W, C, P, BR, load_vec_fm):
    T, Lc = cfg.T, cfg.Lc
    with ExitStack() as st:
        ptv = kb.ps(st, "l_ptv", [128, 128])
        cw = kb.sb(st, "l_cw", [128, 32], F32)
        cbias = kb.sb(st, "l_cb", [128, 8], F32)
        bab = kb.sb(st, "l_ba", [128, 2, 16], F32)
        lam = kb.sb(st, "l_lam", [128, 16], F32)
        cneg = kb.sb(st, "l_cneg", [128, 2, 16], F32)
        load_vec_fm(st, ptv, cw, cw[:], W["lru_conv_w"][li].rearrange("j (k p) -> (j k) p", p=128), 32)
        load_vec_fm(st, ptv, cbias, cbias[:], W["lru_conv_b"][li].rearrange("(k p) -> k p", p=128), 8)
        load_vec_fm(st, ptv, bab, bab[:, 0, :], W["lru_ba"][li].rearrange("d (k p) -> (d k) p", p=128), 16)
        load_vec_fm(st, ptv, bab, bab[:, 1, :], W["lru_bi"][li].rearrange("d (k p) -> (d k) p", p=128), 16)
        load_vec_fm(st, ptv, lam, lam[:], W["lru_lambda"][li].rearrange("d (k p) -> (d k) p", p=128), 16)
        kb.op("act", lambda e: e.activation(out=lam[:], in_=lam[:], func=AF.Exp, scale=-1.0), r=[lam], w=[lam])
        kb.op("act", lambda e: e.activation(out=lam[:], in_=lam[:], func=AF.Ln, bias=1.0), r=[lam], w=[lam])
        kb.op("dve", lambda e: e.tensor_scalar(out=cneg[:, 0, :], in0=lam[:], scalar1=-8.0, scalar2=None, op0=ALU.mult),
              r=[lam], w=[cneg])
        kb.op("dve", lambda e: e.tensor_scalar(out=cneg[:, 1, :], in0=lam[:], scalar1=-16.0, scalar2=None, op0=ALU.mult),
              r=[lam], w=[cneg])
        xb = kb.sb(st, "l_xb", [128, T], BF16)
        ub = kb.sb(st, "l_ub", [128, T], BF16)
        a = kb.sb(st, "l_a", [128, T], F32)
        v = kb.sb(st, "l_v", [128, T], F32)
        hs = kb.sb(st, "l_hs", [128, T], F32)
        hb = kb.sb(st, "l_hb", [128, T], F32)
        wgf = kb.sb(st, "l_wgf", [128, 4, 128], F32)
        wgb = kb.sb(st, "l_wgb", [128, 4, 128], BF16)
        tm = [kb.sb(st, "l_t%d" % i, [128, 512], F32) for i in range(4)]
        ob = [kb.sb(st, "l_ob%d" % i, [128, 512], BF16) for i in range(2)]
        pg = [kb.ps(st, "l_pg%d" % i, [128, 512]) for i in range(4)]
        segs = [(0, Lc), (Lc, T)]
        for ct in range(8):
            kb.dma("sp", xb[:], P[P_LX + ct * 128:P_LX + (ct + 1) * 128, :], w=[xb])
            conv_fm(kb, "dve", v, xb, cw, [cw[:, j * 8 + ct:j * 8 + ct + 1] for j in range(4)], cbias[:, ct:ct + 1], segs, bias_buf=cbias)
            kb.op("act", lambda e: e.copy(out=ub[:], in_=v[:]), r=[v], w=[ub])
            kb.dma("sp", xb[:], P[P_LY + ct * 128:P_LY + (ct + 1) * 128, :], w=[xb])
            for d in range(2):
                kb.dma("sp", wgf[:, d * 2 + 0, :], W["lru_wa"][li, d, ct], w=[wgf])
                kb.dma("sp", wgf[:, d * 2 + 1, :], W["lru_wi"][li, d, ct], w=[wgf])
            kb.op("pool", lambda e: e.tensor_copy(out=wgb[:], in_=wgf[:]), r=[wgf], w=[wgb])
            for d in range(2):
                col = d * 8 + ct
                for gi, (g0, gs) in enumerate(groups_of(T)):
                    pa, pi = pg[(gi % 2) * 2], pg[(gi % 2) * 2 + 1]
                    rt, it, t2 = tm[0], tm[1], tm[2]
                    kb.op("pe", lambda e, pa=pa, d=d, g0=g0, gs=gs: e.matmul(pa[:, 0:gs], lhsT=wgb[:, d * 2, :],
                                                                            rhs=ub[:, g0:g0 + gs], start=True, stop=True),
                          r=[wgb, ub], w=[pa])
                    kb.op("pe", lambda e, pi=pi, d=d, g0=g0, gs=gs: e.matmul(pi[:, 0:gs], lhsT=wgb[:, d * 2 + 1, :],
                                                                            rhs=ub[:, g0:g0 + gs], start=True, stop=True),
                          r=[wgb, ub], w=[pi])
                    kb.op("act", lambda e, pa=pa, gs=gs, col=col: e.activation(
                        out=rt[:, 0:gs], in_=pa[:, 0:gs], func=AF.Sigmoid, bias=bab[:, 0, col:col + 1]),
                        r=[pa, bab], w=[rt])
                    kb.op("act", lambda e, pi=pi, gs=gs, col=col: e.activation(
                        out=it[:, 0:gs], in_=pi[:, 0:gs], func=AF.Sigmoid, bias=bab[:, 1, col:col + 1]),
                        r=[pi, bab], w=[it])
                    kb.op("act", lambda e, g0=g0, gs=gs, col=col: e.activation(
                        out=a[:, g0:g0 + gs], in_=rt[:, 0:gs], func=AF.Exp, scale=cneg[:, 0, col:col + 1]),
                        r=[rt, cneg], w=[a])
                    kb.op("act", lambda e, gs=gs, col=col: e.activation(
                        out=t2[:, 0:gs], in_=rt[:, 0:gs], func=AF.Exp, scale=cneg[:, 1, col:col + 1]),
                        r=[rt, cneg], w=[t2])
                    kb.op("dve", lambda e, gs=gs: e.tensor_scalar(out=t2[:, 0:gs], in0=t2[:, 0:gs], scalar1=-1.0,
                                                                 scalar2=1.0, op0=ALU.mult, op1=ALU.add),
                          r=[t2], w=[t2])
                    kb.op("dve", lambda e, gs=gs: e.tensor_scalar(out=t2[:, 0:gs], in0=t2[:, 0:gs], scalar1=0.0, scalar2=None,
                                                                 op0=ALU.max), r=[t2], w=[t2])
                    kb.op("act", lambda e, gs=gs: e.activation(out=t2[:, 0:gs], in_=t2[:, 0:gs], func=AF.Sqrt),
                          r=[t2], w=[t2])
                    kb.op("dve", lambda e, g0=g0, gs=gs: e.tensor_tensor(out=it[:, 0:gs], in0=it[:, 0:gs],
                                                                        in1=ub[:, g0:g0 + gs], op=ALU.mult),
                          r=[it, ub], w=[it])
                    kb.op("dve", lambda e, g0=g0, gs=gs: e.tensor_tensor(out=v[:, g0:g0 + gs], in0=it[:, 0:gs],
                                                                        in1=t2[:, 0:gs], op=ALU.mult),
                          r=[it, t2], w=[v])
                if d == 0:
                    kb.op("dve", lambda e: e.tensor_tensor_scan(out=hs[:], data0=a[:], data1=v[:], initial=0.0,
                                                                op0=ALU.mult, op1=ALU.add), r=[a, v], w=[hs])
                else:
                    kb.op("dve", lambda e: e.tensor_tensor_scan(out=hb[:, 0:Lc][:, ::-1], data0=a[:, 0:Lc][:, ::-1],
                                                                data1=v[:, 0:Lc][:, ::-1], initial=0.0,
                                                                op0=ALU.mult, op1=ALU.add), r=[a, v], w=[hb])
                    kb.op("dve", lambda e: e.tensor_tensor_scan(out=hb[:, Lc:T][:, ::-1], data0=a[:, Lc:T][:, ::-1],
                                                                data1=v[:, Lc:T][:, ::-1], initial=hb[:, 0:1],
                                                                op0=ALU.mult, op1=ALU.add), r=[a, v, hb], w=[hb])
            for gi, (g0, gs) in enumerate(groups_of(T)):
                t0, t1 = tm[0], tm[1]
                o = ob[gi % 2]
                kb.op("dve", lambda e, g0=g0, gs=gs: e.tensor_tensor(out=t0[:, 0:gs], in0=xb[:, g0:g0 + gs],
                                                                    in1=xb[:, g0:g0 + gs], op=ALU.mult), r=[xb], w=[t0])
                kb.op("dve", lambda e, gs=gs: e.tensor_scalar(out=t0[:, 0:gs], in0=t0[:, 0:gs], scalar1=0.044715,
                                                             scalar2=1.0, op0=ALU.mult, op1=ALU.add), r=[t0], w=[t0])
                kb.op("dve", lambda e, g0=g0, gs=gs: e.tensor_tensor(out=t0[:, 0:gs], in0=t0[:, 0:gs],
                                                                    in1=xb[:, g0:g0 + gs], op=ALU.mult), r=[t0, xb], w=[t0])
                kb.op("act", lambda e, gs=gs: e.activation(out=t0[:, 0:gs], in_=t0[:, 0:gs], func=AF.Sigmoid,
                                                          scale=1.5957691216057308), r=[t0], w=[t0])
                kb.op("dve", lambda e, g0=g0, gs=gs: e.tensor_tensor(out=t0[:, 0:gs], in0=t0[:, 0:gs],
                                                                    in1=xb[:, g0:g0 + gs], op=ALU.mult), r=[t0, xb], w=[t0])
                kb.op("dve", lambda e, g0=g0, gs=gs: e.tensor_tensor(out=t1[:, 0:gs], in0=hs[:, g0:g0 + gs],
                                                                    in1=hb[:, g0:g0 + gs], op=ALU.add), r=[hs, hb], w=[t1])
                kb.op("dve", lambda e, gs=gs, o=o: e.tensor_tensor(out=o[:, 0:gs], in0=t0[:, 0:gs], in1=t1[:, 0:gs],
                                                                  op=ALU.mult), r=[t0, t1], w=[o])
                kb.dma("sp", BR[0, ct * 128:(ct + 1) * 128, g0:g0 + gs], o[:, 0:gs], r=[o])
        kb.barrier()
        kb.flush()


def gate_segs(a0, gs, Lc):
    segs = []
    if a0 < Lc:
        segs.append((0, min(gs, Lc - a0), 1))
    if a0 + gs > Lc:
        segs.append((max(0, Lc - a0), gs, 0))
    return segs


def phase_merge(kb, cfg, li, W, C, P, BR, XT):
    T, Lc = cfg.T, cfg.Lc
    modv = C["modv"]
    with ExitStack() as st:
        wbr = kb.sb(st, "m_wbr", [128, 3, KC, D], BF16)
        wo = kb.sb(st, "m_wo", [128, KC, D], BF16)
        stg = [kb.sb(st, "m_stg%d" % i, [128, KC, 512], F32) for i in range(1)]
        n = 0
        for k in range(4):
            src = (W["w_branch"][li, k] if k < 3 else W["w_out"][li]).rearrange("(k p) n -> p k n", p=128)
            for hh in range(2):
                s = stg[0]
                kb.dma("sp" if n % 2 == 0 else "pool", s[:], src[:, :, hh * 512:(hh + 1) * 512], w=[s])
                dst = wbr[:, k, :, hh * 512:(hh + 1) * 512] if k < 3 else wo[:, :, hh * 512:(hh + 1) * 512]
                kb.op("pool" if n % 2 == 0 else "act",
                      (lambda e, dst=dst, s=s: e.tensor_copy(out=dst, in_=s[:])) if n % 2 == 0 else
                      (lambda e, dst=dst, s=s: e.copy(out=dst, in_=s[:])), r=[s], w=[wbr if k < 3 else wo])
                n += 1
        brt = [kb.sb(st, "m_br%d" % i, [128, 3, KC, 512], BF16) for i in range(1)]
        gt = [kb.sb(st, "m_gt%d" % i, [128, 3, KC, 512], BF16) for i in range(1)]
        sg = [kb.sb(st, "m_sg%d" % i, [128, 512], F32) for i in range(2)]
        acc = kb.sb(st, "m_acc", [128, KC, 512], F32)
        accb = kb.sb(st, "m_accb", [128, KC, 512], BF16)
        xg = [kb.sb(st, "m_xg%d" % i, [128, KC, 512], F32) for i in range(1)]
        pp = [kb.ps(st, "m_pp%d" % i, [128, 512]) for i in range(4)]
        XTv = XT.rearrange("(k p) t -> p k t", p=128)
        cnt = 0
        for gi, (g0, gs) in enumerate(groups_of(T)):
            b_, g_, x_ = brt[0], gt[0], xg[0]
            for k in range(3):
                kb.dma("sp", b_[:, k, :, 0:gs], BR[k].rearrange("(k p) t -> p k t", p=128)[:, :, g0:g0 + gs], w=[b_])
                kb.dma("pool", g_[:, k, :, 0:gs],
                       P[P_GATE + k * 1024:P_GATE + (k + 1) * 1024, :].rearrange("(k p) t -> p k t", p=128)[:, :, g0:g0 + gs],
                       w=[g_])
            kb.dma("sp", x_[:, :, 0:gs], XTv[:, :, g0:g0 + gs], w=[x_])
            for dc in range(KC):
                for k in range(3):
                    p = pp[cnt % 4]
                    s_ = sg[cnt % 2]
                    cnt += 1
                    for kc in range(KC):
                        kb.op("pe", lambda e, p=p, k=k, kc=kc, dc=dc, b_=b_, gs=gs: e.matmul(
                            p[:, 0:gs], lhsT=wbr[:, k, kc, dc * 128:(dc + 1) * 128], rhs=b_[:, k, kc, 0:gs],
                            start=(kc == 0), stop=(kc == KC - 1)), r=[wbr, b_], w=[p])
                    kb.op("act", lambda e, s_=s_, g_=g_, k=k, dc=dc, gs=gs: e.activation(
                        out=s_[:, 0:gs], in_=g_[:, k, dc, 0:gs], func=AF.Sigmoid), r=[g_], w=[s_])
                    if k == 0:
                        kb.op("dve", lambda e, p=p, s_=s_, dc=dc, gs=gs: e.tensor_tensor(
                            out=acc[:, dc, 0:gs], in0=p[:, 0:gs], in1=s_[:, 0:gs], op=ALU.mult), r=[p, s_], w=[acc])
                    else:
                        kb.op("dve", lambda e, p=p, s_=s_, gs=gs: e.tensor_tensor(
                            out=s_[:, 0:gs], in0=p[:, 0:gs], in1=s_[:, 0:gs], op=ALU.mult), r=[p, s_], w=[s_])
                        kb.op("dve", lambda e, s_=s_, dc=dc, gs=gs: e.tensor_tensor(
                            out=acc[:, dc, 0:gs], in0=acc[:, dc, 0:gs], in1=s_[:, 0:gs], op=ALU.add), r=[acc, s_], w=[acc])
            kb.op("act", lambda e, gs=gs: e.copy(out=accb[:, :, 0:gs], in_=acc[:, :, 0:gs]), r=[acc], w=[accb])
            for dc in range(KC):
                p = pp[cnt % 4]
                cnt += 1
                for kc in range(KC):
                    kb.op("pe", lambda e, p=p, kc=kc, dc=dc, gs=gs: e.matmul(
                        p[:, 0:gs], lhsT=wo[:, kc, dc * 128:(dc + 1) * 128], rhs=accb[:, kc, 0:gs],
                        start=(kc == 0), stop=(kc == KC - 1)), r=[wo, accb], w=[p])
                for (s0, s1, which) in gate_segs(g0, gs, Lc):
                    kb.op("dve", lambda e, p=p, x_=x_, dc=dc, s0=s0, s1=s1, which=which: e.scalar_tensor_tensor(
                        out=x_[:, dc, s0:s1], in0=p[:, s0:s1], scalar=modv[:, 16 + dc, which:which + 1],
                        in1=x_[:, dc, s0:s1], op0=ALU.mult, op1=ALU.add), r=[p, x_, modv], w=[x_])
            kb.dma("sp", XTv[:, :, g0:g0 + gs], x_[:, :, 0:gs], r=[x_])
        kb.barrier()
        kb.flush()


def phase_moe(kb, cfg, li, W, C, XT, load_vec_fm):
    T, Lc, FF = cfg.T, cfg.Lc, cfg.ff
    FC = FF // 128
    modv, AB, ones_f, ident_f = C["modv"], C["AB"], C["ones_f"], C["ident_f"]
    NT_ = T // 128
    nq = (NT_ + 13) // 14
    bounds = [(NT_ * i // nq) * 128 for i in range(nq + 1)]
    quarters = [(bounds[i], bounds[i + 1]) for i in range(nq)]
    XTv = XT.rearrange("(k p) t -> p k t", p=128)
    for (h0, h1) in quarters:
        HT = h1 - h0
        NTq = HT // 128
        with ExitStack() as st:
            hq = kb.sb(st, "e_hq", [128, KC, HT], BF16)
            yacc = kb.sb(st, "e_yacc", [128, KC, HT], BF16)
            with ExitStack() as st2:
                xg = [kb.sb(st2, "e_xg%d" % i, [128, KC, 512], F32) for i in range(2)]
                sq = [kb.sb(st2, "e_sq%d" % i, [128, KC, 512], F32) for i in range(2)]
                rs = [kb.sb(st2, "e_rs%d" % i, [128, 512], F32) for i in range(2)]
                pss = [kb.ps(st2, "e_pss%d" % i, [128, 512]) for i in range(2)]
                emit_norm(kb, None, XT, AB, 1, hq, h0, HT, Lc, xg, sq, rs, pss, ones_f)
                kb.barrier()
                kb.flush()
            wrf = kb.sb(st, "e_wrf", [128, KC, 36], F32)
            wrb = kb.sb(st, "e_wrb", [128, KC, 36], BF16)
            kb.dma("sp", wrf[:, :, 0:4], W["rg_w"][li].rearrange("(k p) n -> p k n", p=128), w=[wrf])
            kb.dma("sp", wrf[:, :, 4:36], W["re_w"][li].rearrange("(k p) n -> p k n", p=128), w=[wrf])
            kb.op("pool", lambda e: e.tensor_copy(out=wrb[:], in_=wrf[:]), r=[wrf], w=[wrb])
            rb = kb.sb(st, "e_rb", [128, 36], F32)
            kb.dma("sp", rb[:, 0:4], W["rg_b"][li:li + 1, :].to_broadcast([128, 4]), w=[rb])
            kb.dma("sp", rb[:, 4:36], W["re_b"][li:li + 1, :].to_broadcast([128, 32]), w=[rb])
            L = kb.sb(st, "e_L", [128, NTq, 36], F32)
            st3 = ExitStack()
            pl = [kb.ps(st3, "e_pl%d" % i, [128, 64]) for i in range(2)]
            for ti in range(NTq):
                p = pl[ti % 2]
                for kc in range(KC):
                    kb.op("pe", lambda e, p=p, kc=kc, ti=ti: e.matmul(
                        p[:, 0:36], lhsT=hq[:, kc, ti * 128:(ti + 1) * 128], rhs=wrb[:, kc, :],
                        start=(kc == 0), stop=(kc == KC - 1)), r=[hq, wrb], w=[p])
                kb.op("dve", lambda e, p=p, ti=ti: e.tensor_tensor(out=L[:, ti, :], in0=p[:, 0:36], in1=rb[:], op=ALU.add),
                      r=[p, rb], w=[L])
            gm = kb.sb(st, "e_gm", [128, NTq], F32)
            og = kb.sb(st, "e_og", [128, NTq, 4], F32)
            eg = kb.sb(st, "e_eg", [128, NTq, 4], F32)
            pgp = kb.sb(st, "e_pgp", [128, NTq], F32)
            t48 = kb.sb(st, "e_t48", [128, NTq, 4, 8], F32)
            es8 = kb.sb(st, "e_es8", [128, NTq, 8], F32)
            eq1 = kb.sb(st, "e_eq1", [128, NTq, 8], F32)
            eq2 = kb.sb(st, "e_eq2", [128, NTq, 8], F32)
            v1 = kb.sb(st, "e_v1", [128, NTq], F32)
            v2 = kb.sb(st, "e_v2", [128, NTq], F32)
            wa_ = kb.sb(st, "e_wa", [128, NTq], F32)
            wb_ = kb.sb(st, "e_wb", [128, NTq], F32)
            RW = kb.sb(st, "e_RW", [128, NTq, 4, 8], F32)

            def bc(ap, shape):
                return ap.to_broadcast(shape)
            dv = lambda fn, r, w: kb.op("dve", fn, r=r, w=w)
            dv(lambda e: e.tensor_reduce(out=gm[:], in_=L[:, :, 0:4], axis=AX.X, op=ALU.max), [L], [gm])
            dv(lambda e: e.tensor_tensor(out=og[:], in0=L[:, :, 0:4], in1=bc(gm[:].unsqueeze(2), [128, NTq, 4]),
                                         op=ALU.is_equal), [L, gm], [og])
            dv(lambda e: e.tensor_tensor(out=eg[:], in0=L[:, :, 0:4], in1=bc(gm[:].unsqueeze(2), [128, NTq, 4]),
                                         op=ALU.subtract), [L, gm], [eg])
            kb.op("act", lambda e: e.activation(out=eg[:], in_=eg[:], func=AF.Exp), r=[eg], w=[eg])
            dv(lambda e: e.tensor_reduce(out=pgp[:], in_=eg[:], axis=AX.X, op=ALU.add), [eg], [pgp])
            dv(lambda e: e.reciprocal(out=pgp[:], in_=pgp[:]), [pgp], [pgp])
            dv(lambda e: e.tensor_tensor(out=t48[:], in0=L[:, :, 4:36].rearrange("p t (g i) -> p t g i", g=4),
                                         in1=bc(og[:].unsqueeze(3), [128, NTq, 4, 8]), op=ALU.mult), [L, og], [t48])
            dv(lambda e: e.tensor_reduce(out=es8[:], in_=t48[:].rearrange("p t g i -> p t i g"), axis=AX.X, op=ALU.add),
               [t48], [es8])
            dv(lambda e: e.tensor_reduce(out=v1[:], in_=es8[:], axis=AX.X, op=ALU.max), [es8], [v1])
            dv(lambda e: e.tensor_tensor(out=eq1[:], in0=es8[:], in1=bc(v1[:].unsqueeze(2), [128, NTq, 8]),
                                         op=ALU.is_equal), [es8, v1], [eq1])
            dv(lambda e: e.scalar_tensor_tensor(out=es8[:], in0=eq1[:], scalar=-1e30, in1=es8[:], op0=ALU.mult,
                                                op1=ALU.add), [eq1, es8], [es8])
            dv(lambda e: e.tensor_reduce(out=v2[:], in_=es8[:], axis=AX.X, op=ALU.max), [es8], [v2])
            dv(lambda e: e.tensor_tensor(out=eq2[:], in0=es8[:], in1=bc(v2[:].unsqueeze(2), [128, NTq, 8]),
                                         op=ALU.is_equal), [es8, v2], [eq2])
            dv(lambda e: e.tensor_tensor(out=wa_[:], in0=v2[:], in1=v1[:], op=ALU.subtract), [v1, v2], [wa_])
            kb.op("act", lambda e: e.activation(out=wa_[:], in_=wa_[:], func=AF.Exp), r=[wa_], w=[wa_])
            dv(lambda e: e.tensor_scalar(out=wa_[:], in0=wa_[:], scalar1=1.0, scalar2=None, op0=ALU.add), [wa_], [wa_])
            dv(lambda e: e.reciprocal(out=wa_[:], in_=wa_[:]), [wa_], [wa_])
            dv(lambda e: e.tensor_tensor(out=wa_[:], in0=wa_[:], in1=pgp[:], op=ALU.mult), [wa_, pgp], [wa_])
            dv(lambda e: e.tensor_tensor(out=wb_[:], in0=pgp[:], in1=wa_[:], op=ALU.subtract), [wa_, pgp], [wb_])
            dv(lambda e: e.tensor_tensor(out=eq1[:], in0=eq1[:], in1=bc(wa_[:].unsqueeze(2), [128, NTq, 8]), op=ALU.mult),
               [eq1, wa_], [eq1])
            dv(lambda e: e.tensor_tensor(out=eq2[:], in0=eq2[:], in1=bc(wb_[:].unsqueeze(2), [128, NTq, 8]), op=ALU.mult),
               [eq2, wb_], [eq2])
            dv(lambda e: e.tensor_tensor(out=eq1[:], in0=eq1[:], in1=eq2[:], op=ALU.add), [eq1, eq2], [eq1])
            dv(lambda e: e.tensor_tensor(out=RW[:], in0=bc(eq1[:].unsqueeze(2), [128, NTq, 4, 8]),
                                         in1=bc(og[:].unsqueeze(3), [128, NTq, 4, 8]), op=ALU.mult), [eq1, og], [RW])
            RWT = kb.sb(st, "e_RWT", [32, HT], F32)
            ptr = [kb.ps(st3, "e_ptr%d" % i, [32, 128]) for i in range(2)]
            for ti in range(NTq):
                p = ptr[ti % 2]
                kb.op("pe", lambda e, p=p, ti=ti: e.transpose(p[:], RW[:, ti, :, :].rearrange("p g i -> p (g i)"), ident_f[:]),
                      r=[RW, ident_f], w=[p])
                kb.op("act", lambda e, p=p, ti=ti: e.copy(out=RWT[:, ti * 128:(ti + 1) * 128], in_=p[:]), r=[p], w=[RWT])
            kb.op("pool", lambda e: e.memset(yacc[:], 0.0), w=[yacc])
            kb.barrier()
            kb.flush()
            st3.close()
            w1f = kb.sb(st, "e_w1f", [128, KC, FF], F32)
            w3f = kb.sb(st, "e_w3f", [128, KC, FF], F32)
            w2f = kb.sb(st, "e_w2f", [128, FC, D], F32)
            w1b = [kb.sb(st, "e_w1b%d" % i, [128, KC, FF], BF16) for i in range(2)]
            w3b = [kb.sb(st, "e_w3b%d" % i, [128, KC, FF], BF16) for i in range(2)]
            w2b = [kb.sb(st, "e_w2b%d" % i, [128, FC, D], BF16) for i in range(2)]
            sel = kb.sb(st, "e_sel", [32, 128], F32)
            actT = [kb.sb(st, "e_act%d" % i, [128, FC, 512], BF16) for i in range(2)]
            s1 = [kb.sb(st, "e_s1%d" % i, [128, 512], F32) for i in range(2)]
            wrep = [kb.sb(st, "e_wrep%d" % i, [128, 512], F32) for i in range(2)]
            ph1 = [kb.ps(st, "e_ph1%d" % i, [128, 512]) for i in range(2)]
            ph3 = [kb.ps(st, "e_ph3%d" % i, [128, 512]) for i in range(2)]
            py = [kb.ps(st, "e_py%d" % i, [128, 512]) for i in range(2)]
            pw = kb.ps(st, "e_pw", [128, 512])
            cnt = 0
            for ex in range(NEXP):
                b = ex % 2
                kb.dma("sp", w1f[:], W["ew1"][li, ex].rearrange("(k p) f -> p k f", p=128), w=[w1f])
                kb.dma("pool", w3f[:], W["ew3"][li, ex].rearrange("(k p) f -> p k f", p=128), w=[w3f])
                kb.dma("sp", w2f[:], W["ew2"][li, ex].rearrange("(k p) n -> p k n", p=128), w=[w2f])
                kb.op("pool", lambda e, b=b: e.tensor_copy(out=w1b[b][:], in_=w1f[:]), r=[w1f], w=[w1b[b]])
                kb.op("pool", lambda e, b=b: e.tensor_copy(out=w3b[b][:], in_=w3f[:]), r=[w3f], w=[w3b[b]])
                kb.op("pool", lambda e, b=b: e.tensor_copy(out=w2b[b][:], in_=w2f[:]), r=[w2f], w=[w2b[b]])
                kb.op("pool", lambda e, ex=ex: e.tensor_copy(out=sel[:], in_=ident_f[0:32, ex:ex + 1].to_broadcast([32, 128])),
                      r=[ident_f], w=[sel])
                for gi, (g0, gs) in enumerate(groups_of(HT)):
                    at = actT[gi % 2]
                    wr = wrep[gi % 2]
                    kb.op("pe", lambda e, g0=g0, gs=gs: e.matmul(pw[:, 0:gs], lhsT=sel[:], rhs=RWT[:, g0:g0 + gs],
                                                                start=True, stop=True), r=[sel, RWT], w=[pw])
                    kb.op("act", lambda e, wr=wr, gs=gs: e.copy(out=wr[:, 0:gs], in_=pw[:, 0:gs]), r=[pw], w=[wr])
                    for fc in range(FC):
                        p1, p3 = ph1[cnt % 2], ph3[cnt % 2]
                        s_ = s1[cnt % 2]
                        cnt += 1
                        for kc in range(KC):
                            kb.op("pe", lambda e, p1=p1, kc=kc, fc=fc, b=b, g0=g0, gs=gs: e.matmul(
                                p1[:, 0:gs], lhsT=w1b[b][:, kc, fc * 128:(fc + 1) * 128], rhs=hq[:, kc, g0:g0 + gs],
                                start=(kc == 0), stop=(kc == KC - 1)), r=[w1b[b], hq], w=[p1])
                        for kc in range(KC):
                            kb.op("pe", lambda e, p3=p3, kc=kc, fc=fc, b=b, g0=g0, gs=gs: e.matmul(
                                p3[:, 0:gs], lhsT=w3b[b][:, kc, fc * 128:(fc + 1) * 128], rhs=hq[:, kc, g0:g0 + gs],
                                start=(kc == 0), stop=(kc == KC - 1)), r=[w3b[b], hq], w=[p3])
                        kb.op("act", lambda e, p1=p1, s_=s_, gs=gs: e.activation(out=s_[:, 0:gs], in_=p1[:, 0:gs], func=AF.Silu),
                              r=[p1], w=[s_])
                        kb.op("dve", lambda e, p3=p3, s_=s_, gs=gs: e.tensor_tensor(out=s_[:, 0:gs], in0=p3[:, 0:gs],
                                                                                    in1=s_[:, 0:gs], op=ALU.mult),
                              r=[p3, s_], w=[s_])
                        kb.op("dve", lambda e, s_=s_, at=at, wr=wr, fc=fc, gs=gs: e.tensor_tensor(
                            out=at[:, fc, 0:gs], in0=s_[:, 0:gs], in1=wr[:, 0:gs], op=ALU.mult), r=[s_, wr], w=[at])
                    for dc in range(KC):
                        p = py[dc % 2]
                        for fc in range(FC):
                            kb.op("pe", lambda e, p=p, fc=fc, dc=dc, b=b, at=at, gs=gs: e.matmul(
                                p[:, 0:gs], lhsT=w2b[b][:, fc, dc * 128:(dc + 1) * 128], rhs=at[:, fc, 0:gs],
                                start=(fc == 0), stop=(fc == FC - 1)), r=[w2b[b], at], w=[p])
                        kb.op("dve", lambda e, p=p, dc=dc, g0=g0, gs=gs: e.tensor_tensor(
                            out=yacc[:, dc, g0:g0 + gs], in0=p[:, 0:gs], in1=yacc[:, dc, g0:g0 + gs], op=ALU.add),
                            r=[p, yacc], w=[yacc])
            xr = [kb.sb(st, "e_xr%d" % i, [128, KC, 512], F32) for i in range(1)]
            for gi, (g0, gs) in enumerate(groups_of(HT)):
                x_ = xr[0]
                kb.dma("sp", x_[:, :, 0:gs], XTv[:, :, h0 + g0:h0 + g0 + gs], w=[x_])
                for dc in range(KC):
                    for (s0, s1_, which) in gate_segs(h0 + g0, gs, Lc):
                        kb.op("dve", lambda e, x_=x_, dc=dc, s0=s0, s1_=s1_, which=which, g0=g0: e.scalar_tensor_tensor(
                            out=x_[:, dc, s0:s1_], in0=yacc[:, dc, g0 + s0:g0 + s1_], scalar=modv[:, 40 + dc, which:which + 1],
                            in1=x_[:, dc, s0:s1_], op0=ALU.mult, op1=ALU.add), r=[yacc, x_, modv], w=[x_])
                kb.dma("sp", XTv[:, :, h0 + g0:h0 + g0 + gs], x_[:, :, 0:gs], r=[x_])
            kb.barrier()
            kb.flush()


def phase_final(kb, cfg, C, XT, fin_w, out_d, load_vec_fm):
    T, Lc, Ll = cfg.T, cfg.Lc, cfg.Ll
    ones_f, ident_f = C["ones_f"], C["ident_f"]
    with ExitStack() as st:
        ABf = kb.sb(st, "f_AB", [128, 1, 2, KC, 2], F32)
        ptv = kb.ps(st, "f_ptv", [128, 128])
        kb.op("pool", lambda e: e.memset(ABf[:], 0.0), w=[ABf])
        for c_ in range(2):
            load_vec_fm(st, ptv, ABf, ABf[:, 0, 0, :, c_], fin_w.rearrange("o (k p) -> (o k) p", p=128), KC)
        xg = [kb.sb(st, "f_xg%d" % i, [128, KC, 512], F32) for i in range(2)]
        sq = [kb.sb(st, "f_sq%d" % i, [128, KC, 512], F32) for i in range(2)]
        rs = [kb.sb(st, "f_rs%d" % i, [128, 512], F32) for i in range(2)]
        pss = [kb.ps(st, "f_pss%d" % i, [128, 512]) for i in range(2)]
        hf = [kb.sb(st, "f_hf%d" % i, [128, KC, 512], F32) for i in range(2)]
        ot = [kb.sb(st, "f_ot%d" % i, [128, D], F32) for i in range(2)]
        pt = [kb.ps(st, "f_pt%d" % i, [128, 4, 128]) for i in range(4)]
        n = 0
        for gi, (g0, gs) in enumerate(groups_of(Ll)):
            h = hf[gi % 2]
            emit_norm(kb, None, XT, ABf, 0, h, Lc + g0, gs, 0, [xg[gi % 2]], [sq[gi % 2]], [rs[gi % 2]], [pss[gi % 2]],
                      ones_f, hbase=0)
            for ti in range(gs // 128):
                o = ot[n % 2]
                for hh in range(2):
                    p = pt[(n * 2 + hh) % 4]
                    for j in range(4):
                        kc = hh * 4 + j
                        kb.op("pe", lambda e, p=p, j=j, kc=kc, h=h, ti=ti: e.transpose(
                            p[:, j, :], h[:, kc, ti * 128:(ti + 1) * 128], ident_f[:]), r=[h, ident_f], w=[p])
                    if hh == 0:
                        kb.op("act", lambda e, p=p, o=o: e.copy(out=o[:, 0:512].rearrange("p (j f) -> p j f", j=4), in_=p[:]),
                              r=[p], w=[o])
                    else:
                        kb.op("dve", lambda e, p=p, o=o: e.tensor_copy(out=o[:, 512:1024].rearrange("p (j f) -> p j f", j=4),
                                                                       in_=p[:]), r=[p], w=[o])
                kb.dma("sp", out_d[g0 + ti * 128:g0 + (ti + 1) * 128, :], o[:], r=[o])
                n += 1
        kb.barrier()
        kb.flush()


def kernel(**inputs):
    cfg = Cfg()
    nc, _ = build(cfg)
    B = inputs["x"].shape[0]
    in_maps = [core_inputs(inputs, i % B, cfg) for i in range(8)]
    res = run_bass_kernel_spmd(nc, in_maps, core_ids=list(range(8)))
    out = np.stack([np.asarray(res.results[b]["out"], dtype=np.float32) for b in range(B)], axis=0)
    return out


class View:
    def __init__(self, ap, name, parent):
        self.ap = ap
        self.name = name
        self.parent = parent

    @property
    def lw(self):
        return self.parent.lw

    @lw.setter
    def lw(self, v):
        self.parent.lw = v

    @property
    def rd(self):
        return self.parent.rd

    @rd.setter
    def rd(self, v):
        self.parent.rd = v

    def __getitem__(self, k):
        return self.ap[k]


def mk_tri(kb, t, pattern, cm, cmp):
    kb.op("pool", lambda e: e.memset(t[:], 1.0), w=[t])
    kb.op("pool", lambda e: e.affine_select(out=t[:], in_=t[:], pattern=pattern, compare_op=cmp, fill=0.0, base=0,
                                             channel_multiplier=cm), r=[t], w=[t])


def phase_gdn(kb, cfg, li, W, C, P, PS, OG, BR, load_vec_fm):
    T, Lc = cfg.T, cfg.Lc
    NT = T // 128
    NTc = Lc // 128
    ident_f, ident_b, ones_f = C["ident_f"], C["ident_b"], C["ones_f"]
    SCALE = 128 ** -0.5
    with ExitStack() as st:
        L = [kb.sb(st, "g_L%d" % d, [128, 128], F32) for d in range(2)]
        MS = [kb.sb(st, "g_MS%d" % d, [128, 128], F32) for d in range(2)]
        mk_tri(kb, L[0], [[1, 128]], -1, ALU.is_ge)
        mk_tri(kb, MS[0], [[1, 128]], -1, ALU.is_gt)
        mk_tri(kb, L[1], [[-1, 128]], 1, ALU.is_ge)
        mk_tri(kb, MS[1], [[-1, 128]], 1, ALU.is_gt)
        ptv = kb.ps(st, "g_ptv", [128, 128])
        BDs = []
        for bs in (16, 32, 64):
            nb = 128 // bs
            E = kb.sb(st, "g_E%d" % bs, [nb, 128], F32)
            kb.op("pool", lambda e, E=E: e.memset(E[:], 1.0), w=[E])
            kb.op("pool", lambda e, E=E, bs=bs: e.affine_select(out=E[:], in_=E[:], pattern=[[1, 128]], compare_op=ALU.is_ge,
                                                                fill=0.0, base=0, channel_multiplier=-bs), r=[E], w=[E])
            kb.op("pool", lambda e, E=E, bs=bs: e.affine_select(out=E[:], in_=E[:], pattern=[[-1, 128]], compare_op=ALU.is_ge,
                                                                fill=0.0, base=bs - 1, channel_multiplier=bs), r=[E], w=[E])
            BD = kb.sb(st, "g_BD%d" % bs, [128, 128], F32)
            kb.op("pe", lambda e, E=E: e.matmul(ptv[:], lhsT=E[:], rhs=E[:], start=True, stop=True), r=[E], w=[ptv])
            kb.op("dve", lambda e, BD=BD: e.tensor_copy(out=BD[:], in_=ptv[:]), r=[ptv], w=[BD])
            BDs.append(BD)
        BD16 = BDs[0]
        OFF = [kb.sb(st, "g_OFF%d" % i, [128, 128], F32) for i in range(3)]
        kb.op("dve", lambda e: e.tensor_tensor(out=OFF[0][:], in0=BDs[1][:], in1=BDs[0][:], op=ALU.subtract), r=[BDs[0], BDs[1]], w=[OFF[0]])
        kb.op("dve", lambda e: e.tensor_tensor(out=OFF[1][:], in0=BDs[2][:], in1=BDs[1][:], op=ALU.subtract), r=[BDs[1], BDs[2]], w=[OFF[1]])
        kb.op("dve", lambda e: e.tensor_scalar(out=OFF[2][:], in0=BDs[2][:], scalar1=-1.0, scalar2=1.0, op0=ALU.mult, op1=ALU.add),
              r=[BDs[2]], w=[OFF[2]])
        cw = kb.sb(st, "g_cw", [128, 96], F32)
        load_vec_fm(st, ptv, cw, cw[:], W["gdn_conv_w"][li].rearrange("j (k p) -> (j k) p", p=128), 96)
        pst = kb.sb(st, "g_pst", [128, NT, 32], F32)
        for t0_ in range(0, NT, 8):
            t1_ = min(NT, t0_ + 8)
            kb.dma("sp", pst[:, t0_:t1_, :], PS.rearrange("(t p) n -> p t n", p=128)[:, t0_:t1_, 0:32], w=[pst])
        alog = kb.sb(st, "g_alog", [128, 16], F32)
        dtb = kb.sb(st, "g_dtb", [128, 16], F32)
        kb.dma("sp", alog[:], W["gdn_a_log"][li:li + 1, :].to_broadcast([128, 16]), w=[alog])
        kb.dma("sp", dtb[:], W["gdn_dt_bias"][li:li + 1, :].to_broadcast([128, 16]), w=[dtb])
        names = ["beta", "negb", "g", "gcs", "gtot", "egcs", "kdsc", "etot"]
        tb = {n: kb.sb(st, "g_" + n, [128, NT, 16], F32) for n in names}
        beta, negb, g, gcs, gtot, egcs, kdsc, etot = [tb[n] for n in names]
        kb.op("act", lambda e: e.activation(out=beta[:], in_=pst[:, :, 0:16], func=AF.Sigmoid), r=[pst], w=[beta])
        kb.op("dve", lambda e: e.tensor_scalar(out=negb[:], in0=beta[:], scalar1=-1.0, scalar2=None, op0=ALU.mult),
              r=[beta], w=[negb])
        kb.op("dve", lambda e: e.tensor_tensor(out=g[:], in0=pst[:, :, 16:32],
                                                in1=dtb[:].unsqueeze(1).to_broadcast([128, NT, 16]), op=ALU.add),
              r=[pst, dtb], w=[g])
        kb.op("act", lambda e: e.activation(out=g[:], in_=g[:], func=AF.Exp), r=[g], w=[g])
        kb.op("act", lambda e: e.activation(out=g[:], in_=g[:], func=AF.Ln, bias=1.0), r=[g], w=[g])
        kb.op("act", lambda e: e.activation(out=alog[:], in_=alog[:], func=AF.Exp), r=[alog], w=[alog])
        kb.op("dve", lambda e: e.tensor_scalar(out=alog[:], in0=alog[:], scalar1=-1.0, scalar2=None, op0=ALU.mult),
              r=[alog], w=[alog])
        kb.op("dve", lambda e: e.tensor_tensor(out=g[:], in0=g[:], in1=alog[:].unsqueeze(1).to_broadcast([128, NT, 16]),
                                                op=ALU.mult), r=[g, alog], w=[g])
        pcs = [kb.ps(st, "g_pcs%d" % i, [128, 512]) for i in range(2)]
        TG = 32
        for d in range(2):
            for t0 in range(0, NT, TG):
                t1 = min(NT, t0 + TG)
                n = (t1 - t0) * 8
                kb.op("pe", lambda e, d=d, t0=t0, t1=t1, n=n: e.matmul(
                    pcs[0][:, 0:n].rearrange("p (t h) -> p t h", h=8), lhsT=L[d][:], rhs=g[:, t0:t1, d * 8:(d + 1) * 8],
                    start=True, stop=True), r=[L[d], g], w=[pcs[0]])
                kb.op("dve", lambda e, d=d, t0=t0, t1=t1, n=n: e.tensor_copy(
                    out=gcs[:, t0:t1, d * 8:(d + 1) * 8], in_=pcs[0][:, 0:n].rearrange("p (t h) -> p t h", h=8)),
                    r=[pcs[0]], w=[gcs])
                kb.op("pe", lambda e, d=d, t0=t0, t1=t1, n=n: e.matmul(
                    pcs[1][:, 0:n].rearrange("p (t h) -> p t h", h=8), lhsT=ones_f[:], rhs=g[:, t0:t1, d * 8:(d + 1) * 8],
                    start=True, stop=True), r=[ones_f, g], w=[pcs[1]])
                kb.op("dve", lambda e, d=d, t0=t0, t1=t1, n=n: e.tensor_copy(
                    out=gtot[:, t0:t1, d * 8:(d + 1) * 8], in_=pcs[1][:, 0:n].rearrange("p (t h) -> p t h", h=8)),
                    r=[pcs[1]], w=[gtot])
        kb.op("act", lambda e: e.activation(out=egcs[:], in_=gcs[:], func=AF.Exp), r=[gcs], w=[egcs])
        kb.op("dve", lambda e: e.tensor_tensor(out=kdsc[:], in0=gtot[:], in1=gcs[:], op=ALU.subtract), r=[gtot, gcs], w=[kdsc])
        kb.op("act", lambda e: e.activation(out=kdsc[:], in_=kdsc[:], func=AF.Exp), r=[kdsc], w=[kdsc])
        kb.op("act", lambda e: e.activation(out=etot[:], in_=gtot[:], func=AF.Exp), r=[gtot], w=[etot])
        kb.barrier()
        kb.flush()
        if "stopD1" in cfg.dbg:
            return
        import os as _os
        if _os.environ.get("GPAD"):
            _pad = kb.sb(st, "g_pad", [128, int(_os.environ["GPAD"]) * 1024], mybir.dt.uint8)
        xb = kb.sb(st, "g_xb", [128, T], BF16)
        cacc = kb.sb(st, "g_cacc", [128, T], F32)
        qn = kb.sb(st, "g_qn", [128, T], BF16)
        kn = kb.sb(st, "g_kn", [128, T], BF16)
        vc = kb.sb(st, "g_vc", [128, T], BF16)
        sqt = [kb.sb(st, "g_sq%d" % i, [128, 512], F32) for i in range(2)]
        rst = [kb.sb(st, "g_rs%d" % i, [128, 512], F32) for i in range(2)]
        Sf = [kb.sb(st, "g_Sf%d" % d, [128, 128], F32) for d in range(2)]
        Sb = [kb.sb(st, "g_Sb%d" % d, [128, 128], BF16) for d in range(2)]
        bankF = [kb.ps(st, "g_bf%d" % i, [128, 512]) for i in range(4)]
        bankB = kb.ps(st, "g_bb", [128, 8, 128], BF16)

        def views(u):
            a, b = bankF[(u % 2) * 2], bankF[(u % 2) * 2 + 1]
            v = {}
            v["G"] = View(a[:, 0:128], "vG", a)
            v["KK"] = View(a[:, 128:256], "vKK", a)
            v["X"] = View(a[:, 256:384], "vX", a)
            v["u0"] = View(a[:, 384:512], "vu0", a)
            v["pm"] = View(b[:, 0:256], "vpm", b)
            v["pn"] = View(b[:, 256:384], "vpn", b)
            v["at"] = View(b[:, 384:512], "vat", b)
            o = (u % 2) * 4
            v["tk"] = View(bankB[:, o + 0, :], "vtk", bankB)
            v["tv"] = View(bankB[:, o + 1, :], "vtv", bankB)
            v["tn"] = View(bankB[:, o + 2, :], "vtn", bankB)
            return v
        vsets = [views(0), views(1)]
        seqv = []
        for d in range(2):
            c_ = pcs[d]
            seqv.append({"wS": View(c_[:, 0:128], "vwS", c_), "o": View(c_[:, 128:256], "vo", c_),
                         "sn": View(c_[:, 256:384], "vsn", c_)})

        def tmpset(u):
            t = {}
            for nm in ("gL", "Dm", "Er", "dS", "dI", "NTf", "TTf", "u0b", "osb"):
                t[nm] = kb.sb(st, "g_%s%d" % (nm, u), [128, 128], F32)
            for nm in ("kg", "kd", "vT", "qg", "Pb0", "Pb1", "XTb", "atb", "vn", "Nb", "Na", "Nc", "Nd", "Tn", "Wb"):
                t[nm] = kb.sb(st, "g_%s%d" % (nm, u), [128, 128], BF16)
            t["PT0"] = kb.sb(st, "g_PT0%d" % u, [128, 256], BF16)
            t["PT1"] = kb.sb(st, "g_PT1%d" % u, [128, 256], BF16)
            return t
        tsets = [tmpset(0), tmpset(1), tmpset(2), tmpset(3)]
        order = [list(range(NT)), list(range(NTc - 1, -1, -1)) + list(range(NT - 1, NTc - 1, -1))]
        unit = 0
        for h in range(8):
            for sect, dst in ((0, qn), (1, kn), (2, vc)):
                kb.dma("sp", xb[:], P[P_Q + sect * 1024 + h * 128:P_Q + sect * 1024 + (h + 1) * 128, :], w=[xb])
                col = sect * 8 + h
                conv_fm(kb, "dve", cacc, xb, cw, [cw[:, j * 24 + col:j * 24 + col + 1] for j in range(4)], None,
                        [(0, Lc), (Lc, T)])
                if sect == 2:
                    kb.op("act", lambda e: e.activation(out=vc[:], in_=cacc[:], func=AF.Silu), r=[cacc], w=[vc])
                    continue
                kb.op("act", lambda e: e.activation(out=cacc[:], in_=cacc[:], func=AF.Silu), r=[cacc], w=[cacc])
                for gi, (g0, gs) in enumerate(groups_of(T)):
                    sq_, rs_ = sqt[gi % 2], rst[gi % 2]
                    pn_ = bankF[gi % 4]
                    kb.op("act", lambda e, sq_=sq_, g0=g0, gs=gs: e.activation(out=sq_[:, 0:gs], in_=cacc[:, g0:g0 + gs],
                                                                              func=AF.Square), r=[cacc], w=[sq_])
                    kb.op("pe", lambda e, pn_=pn_, sq_=sq_, gs=gs: e.matmul(pn_[:, 0:gs], lhsT=ones_f[:], rhs=sq_[:, 0:gs],
                                                                          start=True, stop=True), r=[ones_f, sq_], w=[pn_])
                    kb.op("act", lambda e, pn_=pn_, rs_=rs_, gs=gs: e.activation(out=rs_[:, 0:gs], in_=pn_[:, 0:gs],
                                                                               func=AF.Sqrt, bias=EPS), r=[pn_], w=[rs_])
                    kb.op("dve", lambda e, rs_=rs_, gs=gs: e.reciprocal(out=rs_[:, 0:gs], in_=rs_[:, 0:gs]), r=[rs_], w=[rs_])
                    kb.op("dve", lambda e, rs_=rs_, dst=dst, g0=g0, gs=gs, sect=sect: e.scalar_tensor_tensor(
                        out=dst[:, g0:g0 + gs], in0=cacc[:, g0:g0 + gs], scalar=(SCALE if sect == 0 else 1.0),
                        in1=rs_[:, 0:gs], op0=ALU.mult, op1=ALU.mult), r=[cacc, rs_], w=[dst])
            for d in range(2):
                kb.op("pool", lambda e, d=d: e.memset(Sf[d][:], 0.0), w=[Sf[d]])
                kb.op("pool", lambda e, d=d: e.memset(Sb[d][:], 0.0), w=[Sb[d]])
            kb.barrier()
            if "stopD2" in cfg.dbg:
                kb.flush()
                return
            import os as _os
            if _os.environ.get("GCUT") and h == 0:
                print("chunk loop starts at nins", kb.nins)
                kb.limit = kb.nins + int(_os.environ["GCUT"])
            def gdn_unit(d, s, h=h):
                ti = order[d][s]
                c0 = ti * 128
                n = d * 8 + h
                V = vsets[d]
                t = tsets[d * 2 + (s % 2)]
                SV = seqv[d]
                kb.capture()
                kb.op("pe", lambda e, V=V, c0=c0: e.transpose(V["tk"][:], kn[:, c0:c0 + 128], ident_b[:]),
                      r=[kn, ident_b], w=[V["tk"]])
                kb.op("pe", lambda e, V=V, c0=c0: e.transpose(V["tv"][:], vc[:, c0:c0 + 128], ident_b[:]),
                      r=[vc, ident_b], w=[V["tv"]])
                kb.op("dve", lambda e, V=V, t=t, ti=ti, n=n: e.tensor_scalar(
                    out=t["kg"][:], in0=V["tk"][:], scalar1=egcs[:, ti, n:n + 1], scalar2=None, op0=ALU.mult),
                    r=[V["tk"], egcs], w=[t["kg"]])
                kb.op("dve", lambda e, V=V, t=t, ti=ti, n=n: e.tensor_scalar(
                    out=t["kd"][:], in0=V["tk"][:], scalar1=kdsc[:, ti, n:n + 1], scalar2=None, op0=ALU.mult),
                    r=[V["tk"], kdsc], w=[t["kd"]])
                kb.op("dve", lambda e, V=V, t=t: e.tensor_copy(out=t["vT"][:], in_=V["tv"][:]), r=[V["tv"]], w=[t["vT"]])
                kb.op("pool", lambda e, t=t, d=d, ti=ti, n=n: e.tensor_scalar(
                    out=t["gL"][:], in0=L[d][:], scalar1=g[:, ti, n:n + 1], scalar2=None, op0=ALU.mult),
                    r=[L[d], g], w=[t["gL"]])
                kb.op("pe", lambda e, V=V, t=t: e.matmul(V["G"][:], lhsT=ones_f[:], rhs=t["gL"][:], start=True, stop=True),
                      r=[ones_f, t["gL"]], w=[V["G"]])
                kb.op("dve", lambda e, V=V, t=t: e.tensor_copy(out=t["Er"][:], in_=V["G"][:]), r=[V["G"]], w=[t["Er"]])
                kb.op("dve", lambda e, V=V, t=t, ti=ti, n=n: e.tensor_scalar(
                    out=t["Dm"][:], in0=t["Er"][:], scalar1=gcs[:, ti, n:n + 1], scalar2=0.0, op0=ALU.subtract, op1=ALU.min),
                    r=[t["Er"], gcs], w=[t["Dm"]])
                kb.op("act", lambda e, t=t: e.activation(out=t["Dm"][:], in_=t["Dm"][:], func=AF.Exp), r=[t["Dm"]], w=[t["Dm"]])
                kb.op("act", lambda e, V=V, t=t: e.activation(out=t["Er"][:], in_=t["Er"][:], func=AF.Exp), r=[t["Er"]], w=[t["Er"]])
                kb.op("dve", lambda e, t=t, c0=c0: e.tensor_tensor(out=t["qg"][:], in0=qn[:, c0:c0 + 128], in1=t["Er"][:],
                                                                    op=ALU.mult), r=[qn, t["Er"]], w=[t["qg"]])
                kb.op("pool", lambda e, t=t, d=d: e.tensor_tensor(out=t["dS"][:], in0=t["Dm"][:], in1=MS[d][:], op=ALU.mult),
                      r=[t["Dm"], MS[d]], w=[t["dS"]])
                kb.op("pool", lambda e, t=t, d=d: e.tensor_tensor(out=t["dI"][:], in0=t["Dm"][:], in1=L[d][:], op=ALU.mult),
                      r=[t["Dm"], L[d]], w=[t["dI"]])
                kb.op("pe", lambda e, V=V, c0=c0: e.matmul(V["KK"][:], lhsT=kn[:, c0:c0 + 128], rhs=kn[:, c0:c0 + 128],
                                                           start=True, stop=True), r=[kn], w=[V["KK"]])
                kb.op("dve", lambda e, V=V, t=t, ti=ti, n=n: e.scalar_tensor_tensor(
                    out=t["NTf"][:], in0=V["KK"][:], scalar=negb[:, ti, n:n + 1], in1=t["dS"][:], op0=ALU.mult, op1=ALU.mult),
                    r=[V["KK"], negb, t["dS"]], w=[t["NTf"]])
                kb.op("act", lambda e, t=t: e.copy(out=t["atb"][:], in_=t["NTf"][:]), r=[t["NTf"]], w=[t["atb"]])
                kb.op("pe", lambda e, V=V, t=t: e.transpose(V["tn"][:], t["atb"][:], ident_b[:]),
                      r=[t["atb"], ident_b], w=[V["tn"]])
                kb.op("dve", lambda e, V=V, t=t: e.tensor_copy(out=t["Nb"][:], in_=V["tn"][:]), r=[V["tn"]], w=[t["Nb"]])
                for li_, nm in enumerate(("Na", "Nc", "Nd")):
                    kb.op("pool", lambda e, t=t, nm=nm, li_=li_: e.tensor_tensor(out=t[nm][:], in0=t["Nb"][:], in1=OFF[li_][:],
                                                                              op=ALU.mult), r=[t["Nb"], OFF[li_]], w=[t[nm]])
                kb.op("dve", lambda e, t=t: e.tensor_tensor(out=t["NTf"][:], in0=t["NTf"][:], in1=BD16[:], op=ALU.mult),
                      r=[t["NTf"], BD16], w=[t["NTf"]])
                kb.op("act", lambda e, t=t: e.copy(out=t["PT1"][:, 0:128], in_=t["NTf"][:]), r=[t["NTf"]], w=[t["PT1"]])
                kb.op("pool", lambda e, t=t: e.tensor_tensor(out=t["Pb1"][:], in0=t["Nb"][:], in1=BD16[:], op=ALU.mult),
                      r=[t["Nb"], BD16], w=[t["Pb1"]])
                kb.op("dve", lambda e, t=t: e.tensor_tensor(out=t["TTf"][:], in0=t["NTf"][:], in1=ident_f[:], op=ALU.add),
                      r=[t["NTf"], ident_f], w=[t["TTf"]])
                kb.op("pe", lambda e, V=V, t=t: e.matmul(V["pm"][:, 0:128], lhsT=t["Pb1"][:], rhs=t["PT1"][:, 0:128],
                                                         start=True, stop=True), r=[t["Pb1"], t["PT1"]], w=[V["pm"]])
                kb.op("pe", lambda e, V=V, t=t: e.matmul(V["pn"][:], lhsT=t["PT1"][:, 0:128], rhs=t["Pb1"][:],
                                                         start=True, stop=True), r=[t["Pb1"], t["PT1"]], w=[V["pn"]])
                kb.op("dve", lambda e, V=V, t=t: e.tensor_copy(out=t["PT0"][:, 0:128], in_=V["pm"][:, 0:128]), r=[V["pm"]], w=[t["PT0"]])
                kb.op("act", lambda e, t=t: e.copy(out=t["PT0"][:, 128:256], in_=t["TTf"][:]), r=[t["TTf"]], w=[t["PT0"]])
                kb.op("dve", lambda e, V=V, t=t: e.tensor_copy(out=t["Pb0"][:], in_=V["pn"][:]), r=[V["pn"]], w=[t["Pb0"]])
                cur = 0
                LAST = 3
                for k in range(1, LAST + 1):
                    PTc, Pbc = t["PT%d" % cur], t["Pb%d" % cur]
                    PTn, Pbn = t["PT%d" % (1 - cur)], t["Pb%d" % (1 - cur)]
                    if k < LAST:
                        kb.op("pe", lambda e, V=V, PTc=PTc, Pbc=Pbc: e.matmul(V["pm"][:], lhsT=Pbc[:], rhs=PTc[:],
                                                                              start=True, stop=True), r=[PTc, Pbc], w=[V["pm"]])
                        kb.op("pe", lambda e, V=V, PTc=PTc, Pbc=Pbc: e.matmul(V["pn"][:], lhsT=PTc[:, 0:128], rhs=Pbc[:],
                                                                              start=True, stop=True), r=[PTc, Pbc], w=[V["pn"]])
                    else:
                        kb.op("pe", lambda e, V=V, PTc=PTc, Pbc=Pbc: e.matmul(V["pm"][:, 128:256], lhsT=Pbc[:],
                                                                              rhs=PTc[:, 128:256], start=True, stop=True),
                              r=[PTc, Pbc], w=[V["pm"]])
                    kb.op("dve", lambda e, V=V, t=t: e.tensor_tensor(out=t["TTf"][:], in0=V["pm"][:, 128:256], in1=t["TTf"][:],
                                                                    op=ALU.add), r=[V["pm"], t["TTf"]], w=[t["TTf"]])
                    kb.op("act", lambda e, t=t, PTn=PTn: e.copy(out=PTn[:, 128:256], in_=t["TTf"][:]), r=[t["TTf"]], w=[PTn])
                    if k < LAST:
                        kb.op("dve", lambda e, V=V, PTn=PTn: e.tensor_copy(out=PTn[:, 0:128], in_=V["pm"][:, 0:128]), r=[V["pm"]], w=[PTn])
                        kb.op("dve", lambda e, V=V, Pbn=Pbn: e.tensor_copy(out=Pbn[:], in_=V["pn"][:]), r=[V["pn"]], w=[Pbn])
                    cur = 1 - cur
                for nm in ("Na", "Nc", "Nd"):
                    PTc = t["PT%d" % cur]
                    PTn = t["PT%d" % (1 - cur)]
                    kb.op("pe", lambda e, V=V, PTc=PTc: e.transpose(V["tn"][:], PTc[:, 128:256], ident_b[:]),
                          r=[PTc, ident_b], w=[V["tn"]])
                    kb.op("dve", lambda e, V=V, t=t: e.tensor_copy(out=t["Tn"][:], in_=V["tn"][:]), r=[V["tn"]], w=[t["Tn"]])
                    kb.op("pe", lambda e, V=V, t=t, nm=nm, PTc=PTc: e.matmul(V["pn"][:], lhsT=t[nm][:], rhs=PTc[:, 128:256],
                                                                             start=True, stop=True), r=[t[nm], PTc], w=[V["pn"]])
                    kb.op("dve", lambda e, V=V, t=t: e.tensor_copy(out=t["Wb"][:], in_=V["pn"][:]), r=[V["pn"]], w=[t["Wb"]])
                    kb.op("pe", lambda e, V=V, t=t: e.matmul(V["pm"][:, 128:256], lhsT=t["Tn"][:], rhs=t["Wb"][:],
                                                             start=True, stop=True), r=[t["Tn"], t["Wb"]], w=[V["pm"]])
                    kb.op("dve", lambda e, V=V, t=t: e.tensor_tensor(out=t["TTf"][:], in0=V["pm"][:, 128:256], in1=t["TTf"][:],
                                                                    op=ALU.add), r=[V["pm"], t["TTf"]], w=[t["TTf"]])
                    kb.op("act", lambda e, t=t, PTn=PTn: e.copy(out=PTn[:, 128:256], in_=t["TTf"][:]), r=[t["TTf"]], w=[PTn])
                    cur = 1 - cur
                TT = t["PT%d" % cur]
                kb.op("pe", lambda e, V=V, t=t, TT=TT: e.matmul(V["X"][:], lhsT=t["kg"][:], rhs=TT[:, 128:256],
                                                                start=True, stop=True), r=[t["kg"], TT], w=[V["X"]])
                kb.op("dve", lambda e, V=V, t=t: e.tensor_copy(out=t["XTb"][:], in_=V["X"][:]), r=[V["X"]], w=[t["XTb"]])
                kb.op("pe", lambda e, V=V, t=t, TT=TT: e.matmul(V["u0"][:], lhsT=TT[:, 128:256], rhs=t["vT"][:],
                                                                start=True, stop=True), r=[t["vT"], TT], w=[V["u0"]])
                kb.op("dve", lambda e, V=V, t=t, ti=ti, n=n: e.tensor_scalar(
                    out=t["u0b"][:], in0=V["u0"][:], scalar1=beta[:, ti, n:n + 1], scalar2=None, op0=ALU.mult),
                    r=[V["u0"], beta], w=[t["u0b"]])
                kb.op("pe", lambda e, V=V, c0=c0: e.matmul(V["at"][:], lhsT=kn[:, c0:c0 + 128], rhs=qn[:, c0:c0 + 128],
                                                           start=True, stop=True), r=[kn, qn], w=[V["at"]])
                kb.op("dve", lambda e, V=V, t=t: e.tensor_tensor(out=t["atb"][:], in0=V["at"][:], in1=t["dI"][:], op=ALU.mult),
                      r=[V["at"], t["dI"]], w=[t["atb"]])
                par = kb.end_capture()
                kb.capture()
                kb.op("pe", lambda e, SV=SV, t=t, d=d: e.matmul(SV["wS"][:], lhsT=t["XTb"][:], rhs=Sb[d][:], start=True, stop=True),
                      r=[t["XTb"], Sb[d]], w=[SV["wS"]])
                kb.op("dve", lambda e, SV=SV, t=t, ti=ti, n=n: e.scalar_tensor_tensor(
                    out=t["vn"][:], in0=SV["wS"][:], scalar=negb[:, ti, n:n + 1], in1=t["u0b"][:], op0=ALU.mult, op1=ALU.add),
                    r=[SV["wS"], negb, t["u0b"]], w=[t["vn"]])
                kb.op("pe", lambda e, SV=SV, t=t, d=d: e.matmul(SV["o"][:], lhsT=t["qg"][:], rhs=Sb[d][:], start=True, stop=False),
                      r=[t["qg"], Sb[d]], w=[SV["o"]])
                kb.op("pe", lambda e, SV=SV, t=t: e.matmul(SV["o"][:], lhsT=t["atb"][:], rhs=t["vn"][:], start=False, stop=True),
                      r=[t["atb"], t["vn"]], w=[SV["o"]])
                kb.op("dve", lambda e, SV=SV, t=t: e.tensor_copy(out=t["osb"][:], in_=SV["o"][:]), r=[SV["o"]], w=[t["osb"]])
                kb.dma("sp", OG[d, c0:c0 + 128, h * 128:(h + 1) * 128], t["osb"][:], r=[t["osb"]])
                kb.op("pe", lambda e, SV=SV, t=t: e.matmul(SV["sn"][:], lhsT=t["kd"][:], rhs=t["vn"][:], start=True, stop=True),
                      r=[t["kd"], t["vn"]], w=[SV["sn"]])
                kb.op("dve", lambda e, SV=SV, d=d, ti=ti, n=n: e.scalar_tensor_tensor(
                    out=Sf[d][:], in0=Sf[d][:], scalar=etot[:, ti, n:n + 1], in1=SV["sn"][:], op0=ALU.mult, op1=ALU.add),
                    r=[Sf[d], etot, SV["sn"]], w=[Sf[d]])
                kb.op("act", lambda e, d=d: e.copy(out=Sb[d][:], in_=Sf[d][:]), r=[Sf[d]], w=[Sb[d]])
                sq_ = kb.end_capture()
                return par, sq_
            nxt = [gdn_unit(0, 0), gdn_unit(1, 0)]
            kb.emit_rr([nxt[0][0], nxt[1][0]])
            for s in range(NT):
                cur_ = nxt
                lists = [cur_[0][1], cur_[1][1]]
                if s + 1 < NT:
                    nxt = [gdn_unit(0, s + 1), gdn_unit(1, s + 1)]
                    lists += [nxt[0][0], nxt[1][0]]
                kb.emit_rr(lists)
            kb.barrier()
            if "stopD3" in cfg.dbg or "stopD4" in cfg.dbg:
                kb.flush()
                return
        kb.barrier()
        kb.flush()
    with ExitStack() as st:
        ptv = kb.ps(st, "n_ptv", [128, 128])
        nw = kb.sb(st, "n_nw", [128, 1], F32)
        load_vec_fm(st, ptv, nw, nw[:], W["gdn_norm_w"][li:li + 1, :], 1)
        o0 = [kb.sb(st, "n_o0%d" % i, [128, 1024], F32) for i in range(2)]
        o1 = [kb.sb(st, "n_o1%d" % i, [128, 1024], F32) for i in range(2)]
        sq = kb.sb(st, "n_sq", [128, 1024], F32)
        ss = kb.sb(st, "n_ss", [128, 8], F32)
        yb = [kb.sb(st, "n_yb%d" % i, [128, 8, 128], BF16) for i in range(2)]
        zt = [kb.sb(st, "n_zt%d" % i, [128, 8, 128], BF16) for i in range(2)]
        sz = kb.sb(st, "n_sz", [128, 8, 128], F32)
        ob = [kb.sb(st, "n_ob%d" % i, [128, 8, 128], BF16) for i in range(2)]
        ptr = [kb.ps(st, "n_ptr%d" % i, [128, 8, 128], BF16) for i in range(2)]
        for ti in range(NT):
            a_, b_, y_, z_, o_, p_ = o0[ti % 2], o1[ti % 2], yb[ti % 2], zt[ti % 2], ob[ti % 2], ptr[ti % 2]
            c0 = ti * 128
            kb.dma("sp", a_[:], OG[0, c0:c0 + 128, :], w=[a_])
            kb.dma("pool", b_[:], OG[1, c0:c0 + 128, :], w=[b_])
            kb.dma("sp", z_[:], P[P_GZ:P_GZ + 1024, :].rearrange("(h p) t -> p h t", p=128)[:, :, c0:c0 + 128], w=[z_])
            kb.op("dve", lambda e, a_=a_, b_=b_: e.tensor_tensor(out=a_[:], in0=a_[:], in1=b_[:], op=ALU.add), r=[a_, b_], w=[a_])
            kb.op("act", lambda e, a_=a_: e.activation(out=sq[:], in_=a_[:], func=AF.Square), r=[a_], w=[sq])
            kb.op("dve", lambda e: e.tensor_reduce(out=ss[:], in_=sq[:].rearrange("p (h v) -> p h v", h=8), axis=AX.X, op=ALU.add),
                  r=[sq], w=[ss])
            kb.op("act", lambda e: e.activation(out=ss[:], in_=ss[:], func=AF.Sqrt, scale=1.0 / 128, bias=EPS), r=[ss], w=[ss])
            kb.op("dve", lambda e: e.reciprocal(out=ss[:], in_=ss[:]), r=[ss], w=[ss])
            kb.op("dve", lambda e, a_=a_, y_=y_: e.tensor_tensor(
                out=y_[:], in0=a_[:].rearrange("p (h v) -> p h v", h=8), in1=ss[:].unsqueeze(2).to_broadcast([128, 8, 128]),
                op=ALU.mult), r=[a_, ss], w=[y_])
            for hh in range(8):
                kb.op("pe", lambda e, p_=p_, y_=y_, hh=hh: e.transpose(p_[:, hh, :], y_[:, hh, :], ident_b[:]),
                      r=[y_, ident_b], w=[p_])
            kb.op("act", lambda e, z_=z_: e.activation(out=sz[:], in_=z_[:], func=AF.Silu), r=[z_], w=[sz])
            kb.op("dve", lambda e, p_=p_, o_=o_: e.scalar_tensor_tensor(out=o_[:], in0=p_[:], scalar=nw[:, 0:1], in1=sz[:],
                                                                       op0=ALU.mult, op1=ALU.mult), r=[p_, nw, sz], w=[o_])
            kb.dma("sp", BR[1].rearrange("(h p) t -> p h t", p=128)[:, :, c0:c0 + 128], o_[:], r=[o_])
        kb.barrier()
        kb.flush()


def phase_ssd(kb, cfg, li, W, C, P, PS, XC, YS, BR, load_vec_fm):
    T, Lc, rows = cfg.T, cfg.Lc, cfg.rows
    NT = T // 128
    NTc = Lc // 128
    cpc = 128 // rows
    ident_f, ident_b, ones_f = C["ident_f"], C["ident_b"], C["ones_f"]
    with ExitStack() as st:
        ptv = kb.ps(st, "s_ptv", [128, 128])
        cw = kb.sb(st, "s_cw", [128, 48], F32)
        cb = kb.sb(st, "s_cb", [128, 12], F32)
        load_vec_fm(st, ptv, cw, cw[:], W["ssd_conv_w"][li].rearrange("j (k p) -> (j k) p", p=128), 48)
        load_vec_fm(st, ptv, cb, cb[:], W["ssd_conv_b"][li].rearrange("(k p) -> k p", p=128), 12)
        raw = [kb.sb(st, "s_raw%d" % i, [128, T], BF16) for i in range(2)]
        xp = [kb.sb(st, "s_xp%d" % i, [128, T], BF16) for i in range(2)]
        acc = kb.sb(st, "s_acc", [128, T], F32)
        xo = [kb.sb(st, "s_xo%d" % i, [128, T], BF16) for i in range(2)]
        for ct in range(12):
            r_, p_, o_ = raw[ct % 2], xp[ct % 2], xo[ct % 2]
            kb.dma("sp", r_[:], P[P_XS + ct * 128:P_XS + (ct + 1) * 128, :], w=[r_])
            kb.op("act", lambda e, r_=r_, p_=p_: e.copy(out=p_[:, 0:Lc], in_=r_[:, 0:Lc]), r=[r_], w=[p_])
            kb.op("pool", lambda e, r_=r_, p_=p_: e.tensor_copy(
                out=p_[:, Lc:T].rearrange("p (w r) -> p w r", r=rows),
                in_=r_[:, Lc:T].rearrange("p (r w) -> p w r", w=64)), r=[r_], w=[p_])
            conv_fm(kb, "dve", acc, p_, cw, [cw[:, j * 12 + ct:j * 12 + ct + 1] for j in range(4)], cb[:, ct:ct + 1],
                    [(0, Lc), (Lc, T)], bias_buf=cb)
            kb.op("act", lambda e, o_=o_: e.activation(out=o_[:], in_=acc[:], func=AF.Silu), r=[acc], w=[o_])
            kb.dma("sp", XC[ct * 128:(ct + 1) * 128, :], o_[:], r=[o_])
        kb.barrier()
        kb.flush()
    with ExitStack() as st:
        L = [kb.sb(st, "s_L%d" % d, [128, 128], F32) for d in range(2)]
        mk_tri(kb, L[0], [[1, 128]], -1, ALU.is_ge)
        mk_tri(kb, L[1], [[-1, 128]], 1, ALU.is_ge)
        alog = kb.sb(st, "s_alog", [128, 32], F32)
        dtb = kb.sb(st, "s_dtb", [128, 32], F32)
        dsk2 = kb.sb(st, "s_dsk2", [128, 32], F32)
        dsk = kb.sb(st, "s_dsk", [128, 16], F32)
        kb.dma("sp", alog[:], W["ssd_a_log"][li:li + 1, :].to_broadcast([128, 32]), w=[alog])
        kb.dma("sp", dtb[:], W["ssd_dt_bias"][li:li + 1, :].to_broadcast([128, 32]), w=[dtb])
        kb.dma("sp", dsk2[:], W["ssd_d"][li:li + 1, :].to_broadcast([128, 32]), w=[dsk2])
        kb.op("act", lambda e: e.activation(out=alog[:], in_=alog[:], func=AF.Exp), r=[alog], w=[alog])
        kb.op("dve", lambda e: e.tensor_scalar(out=alog[:], in0=alog[:], scalar1=-1.0, scalar2=None, op0=ALU.mult),
              r=[alog], w=[alog])
        kb.op("dve", lambda e: e.tensor_tensor(out=dsk[:], in0=dsk2[:, 0:16], in1=dsk2[:, 16:32], op=ALU.add), r=[dsk2], w=[dsk])
        STf = [kb.sb(st, "s_STf%d" % d, [128, 2, 512], F32) for d in range(2)]
        STb = [kb.sb(st, "s_STb%d" % d, [128, 2, 512], BF16) for d in range(2)]
        for d in range(2):
            kb.op("pool", lambda e, d=d: e.memset(STf[d][:], 0.0), w=[STf[d]])
            kb.op("pool", lambda e, d=d: e.memset(STb[d][:], 0.0), w=[STb[d]])
        pxT = kb.ps(st, "s_pxT", [128, 8, 128], BF16)
        pbT = kb.ps(st, "s_pbT", [128, 2, 128], BF16)
        psm = kb.ps(st, "s_psm", [128, 512])
        pA = kb.ps(st, "s_pA", [128, 4, 128])
        pyI = kb.ps(st, "s_pyI", [128, 512])
        pyS = kb.ps(st, "s_pyS", [128, 512])
        psn = kb.ps(st, "s_psn", [128, 512])

        def tset(u):
            t = {}
            t["X"] = kb.sb(st, "s_X%d" % u, [128, 12, 128], BF16)
            t["dtr"] = kb.sb(st, "s_dtr%d" % u, [128, 32], F32)
            t["dt"] = kb.sb(st, "s_dt%d" % u, [128, 32], F32)
            t["a"] = kb.sb(st, "s_a%d" % u, [128, 16], F32)
            t["acs"] = kb.sb(st, "s_acs%d" % u, [128, 16], F32)
            t["eacs"] = kb.sb(st, "s_eacs%d" % u, [128, 16], F32)
            t["dsc"] = kb.sb(st, "s_dsc%d" % u, [128, 16], F32)
            t["etot"] = kb.sb(st, "s_etot%d" % u, [128, 16], F32)
            t["xT"] = kb.sb(st, "s_xT%d" % u, [128, 16, 64], BF16)
            t["xdt"] = kb.sb(st, "s_xdt%d" % u, [128, 16, 64], BF16)
            t["xdec"] = kb.sb(st, "s_xdec%d" % u, [128, 16, 64], BF16)
            t["bT"] = kb.sb(st, "s_bT%d" % u, [128, 2, 128], BF16)
            t["cbL"] = kb.sb(st, "s_cbL%d" % u, [128, 2, 128], F32)
            t["rhs2"] = kb.sb(st, "s_rhs2%d" % u, [128, 16, 128], F32)
            t["Dm"] = kb.sb(st, "s_Dm%d" % u, [128, 4, 128], F32)
            t["MT"] = kb.sb(st, "s_MT%d" % u, [128, 16, 128], BF16)
            t["ys"] = kb.sb(st, "s_ys%d" % u, [128, 16, 64], F32)
            t["y"] = kb.sb(st, "s_y%d" % u, [128, 16, 64], F32)
            return t
        tsets = [tset(0), tset(1)]
        order = [list(range(NT)), list(range(NTc - 1, -1, -1)) + list(range(NT - 1, NTc - 1, -1))]
        PSlat = PS[Lc:T, :].rearrange("(r w) n -> w r n", w=64)
        def ssd_unit(d, s):
            ti = order[d][s]
            c0 = ti * 128
            t = tsets[d]
            kb.capture()
            X = t["X"]
            kb.dma("sp", X[:], XC.rearrange("(k p) t -> p k t", p=128)[:, :, c0:c0 + 128], w=[X])
            if ti < NTc:
                kb.dma("pool", t["dtr"][:], PS[c0:c0 + 128, 32:64], w=[t["dtr"]])
            else:
                w0 = (ti - NTc) * cpc
                for wi in range(cpc):
                    kb.dma("pool", t["dtr"][wi * rows:(wi + 1) * rows, :], PSlat[w0 + wi, :, 32:64], w=[t["dtr"]])
            kb.op("dve", lambda e, t=t: e.tensor_tensor(out=t["dt"][:], in0=t["dtr"][:], in1=dtb[:], op=ALU.add),
                  r=[t["dtr"], dtb], w=[t["dt"]])
            kb.op("act", lambda e, t=t: e.activation(out=t["dt"][:], in_=t["dt"][:], func=AF.Exp), r=[t["dt"]], w=[t["dt"]])
            kb.op("act", lambda e, t=t: e.activation(out=t["dt"][:], in_=t["dt"][:], func=AF.Ln, bias=1.0), r=[t["dt"]], w=[t["dt"]])
            dts = lambda t=t, d=d: t["dt"][:, d * 16:(d + 1) * 16]
            kb.op("dve", lambda e, t=t, d=d: e.tensor_tensor(out=t["a"][:], in0=t["dt"][:, d * 16:(d + 1) * 16],
                                                              in1=alog[:, d * 16:(d + 1) * 16], op=ALU.mult),
                  r=[t["dt"], alog], w=[t["a"]])
            kb.op("pe", lambda e, t=t, d=d: e.matmul(psm[:, 0:16], lhsT=L[d][:], rhs=t["a"][:], start=True, stop=True),
                  r=[L[d], t["a"]], w=[psm])
            kb.op("pe", lambda e, t=t: e.matmul(psm[:, 16:32], lhsT=ones_f[:], rhs=t["a"][:], start=True, stop=True),
                  r=[ones_f, t["a"]], w=[psm])
            kb.op("dve", lambda e, t=t: e.tensor_copy(out=t["acs"][:], in_=psm[:, 0:16]), r=[psm], w=[t["acs"]])
            kb.op("act", lambda e, t=t: e.activation(out=t["eacs"][:], in_=t["acs"][:], func=AF.Exp), r=[t["acs"]], w=[t["eacs"]])
            kb.op("dve", lambda e, t=t: e.tensor_copy(out=t["etot"][:], in_=psm[:, 16:32]), r=[psm], w=[t["etot"]])
            kb.op("act", lambda e, t=t: e.activation(out=t["etot"][:], in_=t["etot"][:], func=AF.Exp), r=[t["etot"]], w=[t["etot"]])
            kb.op("dve", lambda e, t=t: e.tensor_tensor(out=t["dsc"][:], in0=psm[:, 16:32], in1=t["acs"][:], op=ALU.subtract),
                  r=[psm, t["acs"]], w=[t["dsc"]])
            kb.op("act", lambda e, t=t: e.activation(out=t["dsc"][:], in_=t["dsc"][:], func=AF.Exp), r=[t["dsc"]], w=[t["dsc"]])
            for ct in range(8):
                kb.op("pe", lambda e, X=X, ct=ct: e.transpose(pxT[:, ct, :], X[:, ct, :], ident_b[:]), r=[X, ident_b], w=[pxT])
            for g_ in range(2):
                kb.op("pe", lambda e, X=X, g_=g_: e.transpose(pbT[:, g_, :], X[:, 8 + g_, :], ident_b[:]), r=[X, ident_b], w=[pbT])
            kb.op("dve", lambda e, t=t: e.tensor_copy(out=t["xT"][:], in_=pxT[:].rearrange("p c (e q) -> p (c e) q", q=64)),
                  r=[pxT], w=[t["xT"]])
            kb.op("dve", lambda e, t=t: e.tensor_copy(out=t["bT"][:], in_=pbT[:]), r=[pbT], w=[t["bT"]])
            kb.op("dve", lambda e, t=t, d=d: e.tensor_tensor(
                out=t["xdt"][:], in0=t["xT"][:], in1=t["dt"][:, d * 16:(d + 1) * 16].unsqueeze(2).to_broadcast([128, 16, 64]),
                op=ALU.mult), r=[t["xT"], t["dt"]], w=[t["xdt"]])
            kb.op("dve", lambda e, t=t: e.tensor_tensor(
                out=t["xdec"][:], in0=t["xdt"][:], in1=t["dsc"][:].unsqueeze(2).to_broadcast([128, 16, 64]),
                op=ALU.mult), r=[t["xdt"], t["dsc"]], w=[t["xdec"]])
            for g_ in range(2):
                kb.op("pe", lambda e, X=X, g_=g_: e.matmul(psm[:, 128 + g_ * 128:256 + g_ * 128], lhsT=X[:, 8 + g_, :],
                                                           rhs=X[:, 10 + g_, :], start=True, stop=True), r=[X], w=[psm])
            kb.op("dve", lambda e, t=t, d=d: e.tensor_tensor(
                out=t["cbL"][:], in0=psm[:, 128:384].rearrange("p (g l) -> p g l", g=2),
                in1=L[d][:].unsqueeze(1).to_broadcast([128, 2, 128]), op=ALU.mult), r=[psm, L[d]], w=[t["cbL"]])
            kb.op("pool", lambda e, t=t, d=d: e.tensor_tensor(
                out=t["rhs2"][:], in0=L[d][:].unsqueeze(1).to_broadcast([128, 16, 128]),
                in1=t["a"][:].unsqueeze(2).to_broadcast([128, 16, 128]), op=ALU.mult), r=[L[d], t["a"]], w=[t["rhs2"]])
            for hq in range(4):
                g_ = hq // 2
                kb.op("pe", lambda e, t=t, hq=hq: e.matmul(pA[:], lhsT=ones_f[:], rhs=t["rhs2"][:, hq * 4:(hq + 1) * 4, :],
                                                           start=True, stop=True), r=[ones_f, t["rhs2"]], w=[pA])
                kb.op("dve", lambda e, t=t, hq=hq: e.tensor_tensor(
                    out=t["Dm"][:], in0=pA[:], in1=t["acs"][:, hq * 4:(hq + 1) * 4].unsqueeze(2).to_broadcast([128, 4, 128]),
                    op=ALU.subtract), r=[pA, t["acs"]], w=[t["Dm"]])
                kb.op("dve", lambda e, t=t: e.tensor_scalar(out=t["Dm"][:], in0=t["Dm"][:], scalar1=0.0, scalar2=None,
                                                            op0=ALU.min), r=[t["Dm"]], w=[t["Dm"]])
                kb.op("act", lambda e, t=t: e.activation(out=t["Dm"][:], in_=t["Dm"][:], func=AF.Exp), r=[t["Dm"]], w=[t["Dm"]])
                kb.op("dve", lambda e, t=t, hq=hq, g_=g_: e.tensor_tensor(
                    out=t["MT"][:, hq * 4:(hq + 1) * 4, :], in0=t["Dm"][:],
                    in1=t["cbL"][:, g_, :].unsqueeze(1).to_broadcast([128, 4, 128]), op=ALU.mult),
                    r=[t["Dm"], t["cbL"]], w=[t["MT"]])
            for g_ in range(2):
                for e_ in range(8):
                    hh = g_ * 8 + e_
                    kb.op("pe", lambda e, t=t, hh=hh, e_=e_: e.matmul(pyI[:, e_ * 64:(e_ + 1) * 64], lhsT=t["MT"][:, hh, :],
                                                                      rhs=t["xdt"][:, hh, :], start=True, stop=True),
                          r=[t["MT"], t["xdt"]], w=[pyI])
                kb.op("pe", lambda e, X=X, g_=g_, d=d: e.matmul(pyS[:], lhsT=X[:, 10 + g_, :], rhs=STb[d][:, g_, :],
                                                                start=True, stop=True), r=[X, STb[d]], w=[pyS])
                kb.op("dve", lambda e, t=t, g_=g_: e.tensor_tensor(
                    out=t["ys"][:, g_ * 8:(g_ + 1) * 8, :], in0=pyS[:].rearrange("p (e q) -> p e q", q=64),
                    in1=t["eacs"][:, g_ * 8:(g_ + 1) * 8].unsqueeze(2).to_broadcast([128, 8, 64]), op=ALU.mult),
                    r=[pyS, t["eacs"]], w=[t["ys"]])
                kb.op("dve", lambda e, t=t, g_=g_: e.tensor_tensor(
                    out=t["y"][:, g_ * 8:(g_ + 1) * 8, :], in0=pyI[:].rearrange("p (e q) -> p e q", q=64),
                    in1=t["ys"][:, g_ * 8:(g_ + 1) * 8, :], op=ALU.add), r=[pyI, t["ys"]], w=[t["y"]])
                kb.op("pe", lambda e, t=t, g_=g_: e.matmul(
                    psn[:], lhsT=t["bT"][:, g_, :], rhs=t["xdec"][:, g_ * 8:(g_ + 1) * 8, :].rearrange("p e q -> p (e q)"),
                    start=True, stop=True), r=[t["bT"], t["xdec"]], w=[psn])
                kb.op("dve", lambda e, t=t, g_=g_, d=d: e.tensor_tensor(
                    out=STf[d][:, g_, :].rearrange("p (e q) -> p e q", q=64),
                    in0=STf[d][:, g_, :].rearrange("p (e q) -> p e q", q=64),
                    in1=t["etot"][:, g_ * 8:(g_ + 1) * 8].unsqueeze(2).to_broadcast([128, 8, 64]), op=ALU.mult),
                    r=[STf[d], t["etot"]], w=[STf[d]])
                kb.op("dve", lambda e, g_=g_, d=d: e.tensor_tensor(out=STf[d][:, g_, :], in0=STf[d][:, g_, :], in1=psn[:],
                                                                    op=ALU.add), r=[STf[d], psn], w=[STf[d]])
                kb.op("act", lambda e, g_=g_, d=d: e.copy(out=STb[d][:, g_, :], in_=STf[d][:, g_, :]), r=[STf[d]], w=[STb[d]])
            if d == 0:
                kb.op("dve", lambda e, t=t: e.tensor_tensor(
                    out=t["ys"][:], in0=t["xT"][:], in1=dsk[:].unsqueeze(2).to_broadcast([128, 16, 64]), op=ALU.mult),
                    r=[t["xT"], dsk], w=[t["ys"]])
                kb.op("dve", lambda e, t=t: e.tensor_tensor(out=t["y"][:], in0=t["y"][:], in1=t["ys"][:], op=ALU.add),
                      r=[t["y"], t["ys"]], w=[t["y"]])
            kb.dma("sp", YS[d, c0:c0 + 128, :], t["y"][:].rearrange("p e q -> p (e q)"), r=[t["y"]])
            return kb.end_capture()
        for s in range(NT):
            kb.emit_rr([ssd_unit(0, s)])
            kb.emit_rr([ssd_unit(1, s)])
        kb.barrier()
        kb.flush()
    with ExitStack() as st:
        ptv = kb.ps(st, "z_ptv", [128, 128])
        nw = kb.sb(st, "z_nw", [128, 8], F32)
        load_vec_fm(st, ptv, nw, nw[:], W["ssd_norm_w"][li].rearrange("(k p) -> k p", p=128), 8)
        y0 = [kb.sb(st, "z_y0%d" % i, [128, 1024], F32) for i in range(2)]
        y1 = [kb.sb(st, "z_y1%d" % i, [128, 1024], F32) for i in range(2)]
        zt = [kb.sb(st, "z_zt%d" % i, [128, 8, 128], BF16) for i in range(2)]
        sz = kb.sb(st, "z_sz", [128, 8, 128], F32)
        yz = kb.sb(st, "z_yz", [128, 8, 128], F32)
        sq = kb.sb(st, "z_sq", [128, 8, 128], F32)
        rs = kb.sb(st, "z_rs", [128, 2, 128], F32)
        ob = [kb.sb(st, "z_ob%d" % i, [128, 8, 128], BF16) for i in range(2)]
        pt = [kb.ps(st, "z_pt%d" % i, [128, 4, 128]) for i in range(4)]
        pss = kb.ps(st, "z_pss", [128, 2, 128])
        YSlat = [YS[d, Lc:T, :].rearrange("(w r) n -> r w n", r=rows) for d in range(2)]
        for ti in range(NT):
            a_, b_, z_, o_ = y0[ti % 2], y1[ti % 2], zt[ti % 2], ob[ti % 2]
            c0 = ti * 128
            if ti < NTc:
                kb.dma("sp", a_[:], YS[0, c0:c0 + 128, :], w=[a_])
                kb.dma("pool", b_[:], YS[1, c0:c0 + 128, :], w=[b_])
            else:
                r0 = (ti - NTc) * 2
                for k in range(2):
                    kb.dma("sp", a_[k * 64:(k + 1) * 64, :], YSlat[0][r0 + k], w=[a_])
                    kb.dma("pool", b_[k * 64:(k + 1) * 64, :], YSlat[1][r0 + k], w=[b_])
            kb.dma("sp", z_[:], P[P_SZ:P_SZ + 1024, :].rearrange("(k p) t -> p k t", p=128)[:, :, c0:c0 + 128], w=[z_])
            kb.op("dve", lambda e, a_=a_, b_=b_: e.tensor_tensor(out=a_[:], in0=a_[:], in1=b_[:], op=ALU.add), r=[a_, b_], w=[a_])
            kb.op("act", lambda e, z_=z_: e.activation(out=sz[:], in_=z_[:], func=AF.Silu), r=[z_], w=[sz])
            for hh in range(2):
                p = pt[(ti * 2 + hh) % 4]
                for j in range(4):
                    kc = hh * 4 + j
                    kb.op("pe", lambda e, p=p, j=j, kc=kc, a_=a_: e.transpose(p[:, j, :], a_[:, kc * 128:(kc + 1) * 128], ident_f[:]),
                          r=[a_, ident_f], w=[p])
                kb.op("dve", lambda e, p=p, hh=hh: e.tensor_tensor(out=yz[:, hh * 4:(hh + 1) * 4, :], in0=p[:],
                                                                    in1=sz[:, hh * 4:(hh + 1) * 4, :], op=ALU.mult),
                      r=[p, sz], w=[yz])
            kb.op("act", lambda e: e.activation(out=sq[:], in_=yz[:], func=AF.Square), r=[yz], w=[sq])
            for g_ in range(2):
                for j in range(4):
                    kb.op("pe", lambda e, g_=g_, j=j: e.matmul(pss[:, g_, :], lhsT=ones_f[:], rhs=sq[:, g_ * 4 + j, :],
                                                               start=(j == 0), stop=(j == 3)), r=[ones_f, sq], w=[pss])
            kb.op("act", lambda e: e.activation(out=rs[:], in_=pss[:], func=AF.Sqrt, scale=1.0 / 512, bias=EPS), r=[pss], w=[rs])
            kb.op("dve", lambda e: e.reciprocal(out=rs[:], in_=rs[:]), r=[rs], w=[rs])
            for ct in range(8):
                kb.op("dve", lambda e, ct=ct, o_=o_: e.scalar_tensor_tensor(
                    out=o_[:, ct, :], in0=yz[:, ct, :], scalar=nw[:, ct:ct + 1], in1=rs[:, ct // 4, :], op0=ALU.mult, op1=ALU.mult),
                    r=[yz, nw, rs], w=[o_])
            kb.dma("sp", BR[2].rearrange("(k p) t -> p k t", p=128)[:, :, c0:c0 + 128], o_[:], r=[o_])
        kb.barrier()
        kb.flush()
```

```python
import numpy as np
from contextlib import ExitStack
import concourse.bass as bass
import concourse.mybir as mybir
from concourse.bass_utils import run_bass_kernel_spmd

F32 = mybir.dt.float32
BF16 = mybir.dt.bfloat16
I32 = mybir.dt.int32
ALU = mybir.AluOpType
AF = mybir.ActivationFunctionType
AX = mybir.AxisListType

D = 1024
KC = 8
NEXP = 32
NGRP = 4
EPG = 8
EPS = 1e-6
IN_W = 11840
SAME_ENG_SYNC = True


class Buf:
    def __init__(self, t, name):
        self.t = t
        self.name = name
        self.lw = None
        self.rd = {}

    def __getitem__(self, k):
        return self.t[k]


class CutHere(Exception):
    pass


class KB:
    ENG = ["pe", "act", "dve", "pool", "sp"]

    def __init__(self, nc, es):
        self.nc = nc
        self.es = es
        self.stream = {e: [] for e in self.ENG}
        self.semh = {}
        self.cnt = {e: 0 for e in self.ENG}
        self.waited = {e: {} for e in self.ENG}
        for e in self.ENG:
            self.semh["c_" + e] = es.enter_context(nc.semaphore("c_" + e))
        self.slots = {}
        self.slot_i = {}
        for q, n in (("sp", 8), ("act", 4), ("pool", 6)):
            self.slots[q] = []
            for i in range(n):
                key = "d_%s%d" % (q, i)
                self.semh[key] = es.enter_context(nc.semaphore(key))
                self.slots[q].append([key, 0])
            self.slot_i[q] = 0
        self.nins = 0

    def sb(self, st, name, shape, dt):
        self.uid = getattr(self, "uid", 0) + 1
        name = "%s_%d" % (name, self.uid)
        return Buf(st.enter_context(self.nc.sbuf_tensor(name, list(shape), dt)), name)

    def ps(self, st, name, shape, dt=F32):
        self.uid = getattr(self, "uid", 0) + 1
        name = "%s_%d" % (name, self.uid)
        return Buf(st.enter_context(self.nc.psum_tensor(name, list(shape), dt)), name)

    def _deps(self, r, w):
        deps = {}

        def add(s, v):
            if deps.get(s, 0) < v:
                deps[s] = v
        for b in r:
            if b.lw is not None:
                add(*b.lw)
        for b in w:
            if b.lw is not None:
                add(*b.lw)
            for s, v in b.rd.items():
                add(s, v)
        return deps

    def _filter(self, eng, deps):
        out = []
        wd = self.waited[eng]
        for s, v in deps.items():
            if s == "c_" + eng and (eng == "pe" or not SAME_ENG_SYNC):
                continue
            if wd.get(s, 0) >= v:
                continue
            wd[s] = v
            out.append((s, v))
        return out

    def capture(self):
        self._cap = []

    def end_capture(self):
        c = self._cap
        self._cap = None
        return c

    def emit_rr(self, lists):
        its = [iter(l) for l in lists if l]
        while its:
            for it in list(its):
                try:
                    item = next(it)
                except StopIteration:
                    its.remove(it)
                    continue
                if item[0] == "op":
                    self.op(*item[1:])
                else:
                    self.dma(item[1], item[2], item[3], r=item[4], w=item[5], **item[6])

    def op(self, eng, fn, r=(), w=()):
        if getattr(self, "_cap", None) is not None:
            self._cap.append(("op", eng, fn, r, w))
            return
        waits = self._filter(eng, self._deps(r, w))
        self.cnt[eng] += 1
        seq = self.cnt[eng]
        key = "c_" + eng
        self.stream[eng].append((waits, fn, key, 1))
        for b in r:
            b.rd[key] = seq
        for b in w:
            b.lw = (key, seq)
            b.rd = {}
        self.nins += 1

    def dma(self, q, out, in_, r=(), w=(), **kw):
        if getattr(self, "_cap", None) is not None:
            self._cap.append(("dma", q, out, in_, r, w, kw))
            return
        sl = self.slots[q][self.slot_i[q]]
        self.slot_i[q] = (self.slot_i[q] + 1) % len(self.slots[q])
        deps = self._deps(r, w)
        if sl[1] > 0 and deps.get(sl[0], 0) < 16 * sl[1]:
            deps[sl[0]] = 16 * sl[1]
        waits = self._filter(q, deps)
        sl[1] += 1
        val = 16 * sl[1]
        self.stream[q].append((waits, lambda e: e.dma_start(out=out, in_=in_, **kw), sl[0], 16))
        for b in r:
            b.rd[sl[0]] = val
        for b in w:
            b.lw = (sl[0], val)
            b.rd = {}
        self.nins += 1

    def barrier(self):
        allv = {}
        for e in self.ENG:
            if self.cnt[e] > 0:
                allv["c_" + e] = self.cnt[e]
        for q in self.slots:
            for key, uses in self.slots[q]:
                if uses > 0:
                    allv[key] = 16 * uses
        for e in self.ENG:
            d = {s: v for s, v in allv.items() if s != "c_" + e}
            waits = self._filter(e, d)
            if waits:
                self.stream[e].append((waits, None, None, 0))

    def flush(self):
        nc = self.nc
        semh = self.semh
        with nc.Block() as block:
            for eng, deco in (("pe", block.tensor), ("act", block.scalar), ("dve", block.vector),
                              ("pool", block.gpsimd), ("sp", block.sync)):
                items = self.stream[eng]
                self.stream[eng] = []
                if not items:
                    continue

                def body(e, items=items):
                    for waits, fn, key, inc in items:
                        for s, v in waits:
                            e.wait_ge(semh[s], v)
                        if fn is not None:
                            fn(e).then_inc(semh[key], inc)
                deco(body)


def groups_of(n, g=512):
    out = []
    s = 0
    while s < n:
        out.append((s, min(g, n - s)))
        s += g
    return out


SEGS = [(0, 48, 0), (6176, 20, 48 * 128), (8768, 24, 68 * 128)]
P_LX, P_LY, P_Q, P_K, P_V, P_GZ = 0, 1024, 2048, 3072, 4096, 5120
P_SZ, P_XS, P_BM, P_CM, P_GATE = 6144, 7168, 8192, 8448, 8704
P_ROWS = 92 * 128


class Cfg:
    def __init__(self, Lc=256, Ll=8192, depth=4, ff=512, dbg=()):
        self.Lc, self.Ll, self.depth, self.ff = Lc, Ll, depth, ff
        self.T = Lc + Ll
        self.rows = Ll // 64
        self.dbg = tuple(dbg)


def build(cfg):
    nc = bass.Bass("TRN2", target_bir_lowering=False)
    T, Lc, Ll, DEPTH, FF = cfg.T, cfg.Lc, cfg.Ll, cfg.depth, cfg.ff
    NT = T // 128
    dt_in = {}

    def din(name, shape):
        dt_in[name] = nc.dram_tensor(name, list(shape), F32, kind="ExternalInput").ap()
        return dt_in[name]

    x_in = din("x", [Ll, D])
    c_in = din("c", [1, D])
    ctx_in = din("ctx", [Lc, D])
    cctx_in = din("c_ctx", [1, D])
    w_mod = din("w_mod", [DEPTH, D, 6 * D])
    b_mod = din("b_mod", [DEPTH, 6 * D])
    norm1_w = din("norm1_w", [DEPTH, D])
    norm2_w = din("norm2_w", [DEPTH, D])
    w_in = din("w_in", [DEPTH, D, IN_W])
    lru_conv_w = din("lru_conv_w", [DEPTH, 4, 1024])
    lru_conv_b = din("lru_conv_b", [DEPTH, 1024])
    lru_wa = din("lru_wa", [DEPTH, 2, 8, 128, 128])
    lru_ba = din("lru_ba", [DEPTH, 2, 1024])
    lru_wi = din("lru_wi", [DEPTH, 2, 8, 128, 128])
    lru_bi = din("lru_bi", [DEPTH, 2, 1024])
    lru_lambda = din("lru_lambda", [DEPTH, 2, 1024])
    gdn_conv_w = din("gdn_conv_w", [DEPTH, 4, 3072])
    gdn_a_log = din("gdn_a_log", [DEPTH, 16])
    gdn_dt_bias = din("gdn_dt_bias", [DEPTH, 16])
    gdn_norm_w = din("gdn_norm_w", [DEPTH, 128])
    ssd_conv_w = din("ssd_conv_w", [DEPTH, 4, 1536])
    ssd_conv_b = din("ssd_conv_b", [DEPTH, 1536])
    ssd_a_log = din("ssd_a_log", [DEPTH, 32])
    ssd_dt_bias = din("ssd_dt_bias", [DEPTH, 32])
    ssd_d = din("ssd_d", [DEPTH, 32])
    ssd_norm_w = din("ssd_norm_w", [DEPTH, 1024])
    w_branch = din("w_branch", [DEPTH, 3, 1024, D])
    w_out = din("w_out", [DEPTH, D, D])
    rg_w = din("router_group_w", [DEPTH, D, NGRP])
    rg_b = din("router_group_b", [DEPTH, NGRP])
    re_w = din("router_expert_w", [DEPTH, D, NEXP])
    re_b = din("router_expert_b", [DEPTH, NEXP])
    ew1 = din("expert_w1", [DEPTH, NEXP, D, FF])
    ew3 = din("expert_w3", [DEPTH, NEXP, D, FF])
    ew2 = din("expert_w2", [DEPTH, NEXP, FF, D])
    fin_w = din("final_norm_w", [1, D])

    out_d = nc.dram_tensor("out", [Ll, D], F32, kind="ExternalOutput").ap()
    dbg_out = {}

    def dbg_tensor(name, shape, dt=F32):
        dbg_out[name] = nc.dram_tensor("dbg_" + name, list(shape), dt, kind="ExternalOutput").ap()
        return dbg_out[name]

    def scratch(name, shape, dt):
        if name in cfg.dbg:
            return dbg_tensor(name, shape, dt)
        return nc.dram_tensor("s_" + name, list(shape), dt, kind="Internal").ap()

    XT = scratch("XT", [D, T], F32)
    P = scratch("P", [P_ROWS, T], BF16)
    PS = scratch("PS", [T, 64], F32)
    BR = scratch("BR", [3, 1024, T], BF16)
    OG = scratch("OG", [2, T, 1024], F32)
    YS = scratch("YS", [2, T, 1024], F32)
    XC = scratch("XC", [1536, T], BF16)
    RW = scratch("RW", [T, NEXP], F32)

    with ExitStack() as es:
        kb = KB(nc, es)
        ident_f = kb.sb(es, "ident_f", [128, 128], F32)
        ident_b = kb.sb(es, "ident_b", [128, 128], BF16)
        ones_f = kb.sb(es, "ones_f", [128, 128], F32)
        ones_b = kb.sb(es, "ones_b", [128, 128], BF16)
        act_lc = kb.sb(es, "act_lc", [128, KC, 2], F32)
        modv = kb.sb(es, "modv", [128, 48, 2], F32)
        AB = kb.sb(es, "AB", [128, 2, 2, KC, 2], F32)

        def mk_ident(t, dt):
            kb.op("pool", lambda e: e.memset(t[:], 0.0), w=[t])
            kb.op("pool", lambda e: e.affine_select(out=t[:], in_=t[:], pattern=[[-1, 128]],
                                                     compare_op=ALU.not_equal, fill=1.0, base=0,
                                                     channel_multiplier=1), r=[t], w=[t])
        mk_ident(ident_f, F32)
        mk_ident(ident_b, BF16)
        kb.op("pool", lambda e: e.memset(ones_f[:], 1.0), w=[ones_f])
        kb.op("pool", lambda e: e.memset(ones_b[:], 1.0), w=[ones_b])

        def load_vec_fm(st, pst, dst, dst_ap, src2d, n):
            tmp = kb.sb(st, "lv_tmp%d" % kb.nins, [n, 128], F32)
            kb.dma("sp", tmp[:], src2d, w=[tmp])
            kb.op("pe", lambda e: e.transpose(pst[:, 0:n], tmp[:], ident_f[0:n, 0:n]), r=[tmp, ident_f], w=[pst])
            kb.op("dve", lambda e: e.tensor_copy(out=dst_ap, in_=pst[:, 0:n]), r=[pst], w=[dst])

        with ExitStack() as st:
            xin = [kb.sb(st, "xin%d" % i, [128, D], F32) for i in range(2)]
            xo = [kb.sb(st, "xo%d" % i, [128, KC, 128], F32) for i in range(2)]
            pt = [kb.ps(st, "pt%d" % i, [128, 4, 128]) for i in range(4)]
            cv = kb.sb(st, "cv", [128, KC, 2], F32)
            sg = kb.sb(st, "sg", [128, KC, 2], F32)
            ptv = kb.ps(st, "ptv", [128, 128])
            load_vec_fm(st, ptv, cv, cv[:, :, 0], c_in.rearrange("o (k p) -> (o k) p", p=128), KC)
            load_vec_fm(st, ptv, cv, cv[:, :, 1], cctx_in.rearrange("o (k p) -> (o k) p", p=128), KC)
            kb.op("act", lambda e: e.activation(out=sg[:], in_=cv[:], func=AF.Sigmoid), r=[cv], w=[sg])
            kb.op("dve", lambda e: e.tensor_tensor(out=act_lc[:], in0=cv[:], in1=sg[:], op=ALU.mult),
                  r=[cv, sg], w=[act_lc])
            for ti in range(NT):
                src = ctx_in[ti * 128:(ti + 1) * 128, :] if ti < Lc // 128 else \
                    x_in[ti * 128 - Lc:(ti + 1) * 128 - Lc, :]
                xi = xin[ti % 2]
                xoo = xo[ti % 2]
                kb.dma("sp", xi[:], src, w=[xi])
                for hh in range(2):
                    p = pt[(ti * 2 + hh) % 4]
                    for j in range(4):
                        kc = hh * 4 + j
                        kb.op("pe", lambda e, p=p, j=j, kc=kc, xi=xi: e.transpose(
                            p[:, j, :], xi[:, kc * 128:(kc + 1) * 128], ident_f[:]), r=[xi, ident_f], w=[p])
                    eng = "act" if hh == 0 else "dve"
                    if eng == "act":
                        kb.op("act", lambda e, p=p, xoo=xoo, hh=hh: e.copy(out=xoo[:, hh * 4:(hh + 1) * 4, :], in_=p[:]),
                              r=[p], w=[xoo])
                    else:
                        kb.op("dve", lambda e, p=p, xoo=xoo, hh=hh: e.tensor_copy(out=xoo[:, hh * 4:(hh + 1) * 4, :], in_=p[:]),
                              r=[p], w=[xoo])
                kb.dma("sp", XT.rearrange("(k p) t -> p k t", p=128)[:, :, ti * 128:(ti + 1) * 128], xoo[:], r=[xoo])
            kb.barrier()
            kb.flush()

        for li in range(DEPTH):
            last = li == DEPTH - 1
            if "stop0" in cfg.dbg:
                break
            with ExitStack() as st:
                wm = [kb.sb(st, "wm%d" % i, [128, KC, 512], F32) for i in range(2)]
                pm = kb.ps(st, "pm", [128, 48, 2])
                ptm = kb.ps(st, "ptm", [128, 128])
                bm = kb.sb(st, "bm", [128, 48], F32)
                nw = kb.sb(st, "nw", [128, 2, KC], F32)
                load_vec_fm(st, ptm, bm, bm[:], b_mod[li].rearrange("(k p) -> k p", p=128), 48)
                load_vec_fm(st, ptm, nw, nw[:, 0, :], norm1_w[li].rearrange("(k p) -> k p", p=128), KC)
                load_vec_fm(st, ptm, nw, nw[:, 1, :], norm2_w[li].rearrange("(k p) -> k p", p=128), KC)
                for cb in range(12):
                    w = wm[cb % 2]
                    kb.dma("sp" if cb % 2 == 0 else "pool", w[:],
                           w_mod[li].rearrange("(k p) n -> p k n", p=128)[:, :, cb * 512:(cb + 1) * 512], w=[w])
                    for j in range(4):
                        cc = cb * 4 + j
                        for kc in range(KC):
                            kb.op("pe", lambda e, w=w, j=j, kc=kc, cc=cc: e.matmul(
                                pm[:, cc, :], lhsT=w[:, kc, j * 128:(j + 1) * 128], rhs=act_lc[:, kc, :],
                                start=(kc == 0), stop=(kc == KC - 1)), r=[w, act_lc], w=[pm])
                kb.op("dve", lambda e: e.tensor_tensor(out=modv[:], in0=pm[:],
                                                        in1=bm[:].unsqueeze(2).to_broadcast([128, 48, 2]), op=ALU.add),
                      r=[pm, bm], w=[modv])
                for ni, (sh, sc) in enumerate(((0, 1), (3, 4))):
                    kb.op("dve", lambda e, ni=ni, sc=sc: e.scalar_tensor_tensor(
                        out=AB[:, ni, 0, :, :], in0=modv[:, sc * 8:(sc + 1) * 8, :], scalar=1.0,
                        in1=nw[:, ni, :].unsqueeze(2).to_broadcast([128, KC, 2]), op0=ALU.add, op1=ALU.mult),
                        r=[modv, nw], w=[AB])
                    kb.op("dve", lambda e, ni=ni, sh=sh: e.tensor_copy(
                        out=AB[:, ni, 1, :, :], in_=modv[:, sh * 8:(sh + 1) * 8, :]), r=[modv], w=[AB])
                kb.barrier()
                kb.flush()

            if "stopM" in cfg.dbg:
                break
            halves = [(0, T)] if T <= 4224 else [(0, T // 2 // 128 * 128), (T // 2 // 128 * 128, T)]
            for (h0, h1) in halves:
                HT = h1 - h0
                with ExitStack() as st:
                    hT = kb.sb(st, "hT", [128, KC, HT], BF16)
                    xg = [kb.sb(st, "xg%d" % i, [128, KC, 512], F32) for i in range(2)]
                    sq = [kb.sb(st, "sq%d" % i, [128, KC, 512], F32) for i in range(2)]
                    rs = [kb.sb(st, "rs%d" % i, [128, 512], F32) for i in range(2)]
                    pss = [kb.ps(st, "pss%d" % i, [128, 512]) for i in range(2)]
                    emit_norm(kb, nc, XT, AB, 0, hT, h0, HT, Lc, xg, sq, rs, pss, ones_f)
                    if "stopA1" in cfg.dbg:
                        kb.barrier()
                        kb.flush()
                        break
                    wsf = kb.sb(st, "wsf", [128, KC, 64], F32)
                    wsb = kb.sb(st, "wsb", [128, KC, 64], BF16)
                    wv = w_in[li].rearrange("(k p) n -> p k n", p=128)
                    kb.dma("sp", wsf[:, :, 0:32], wv[:, :, 6144:6176], w=[wsf])
                    kb.dma("sp", wsf[:, :, 32:64], wv[:, :, 8736:8768], w=[wsf])
                    kb.op("pool", lambda e: e.tensor_copy(out=wsb[:], in_=wsf[:]), r=[wsf], w=[wsb])
                    pp = [kb.ps(st, "pp%d" % i, [128, 512]) for i in range(4)]
                    sst = [kb.sb(st, "sst%d" % i, [128, 64], F32) for i in range(2)]
                    for ti in range(HT // 128):
                        p = pp[ti % 4]
                        so = sst[ti % 2]
                        for kc in range(KC):
                            kb.op("pe", lambda e, p=p, kc=kc, ti=ti: e.matmul(
                                p[:, 0:64], lhsT=hT[:, kc, ti * 128:(ti + 1) * 128], rhs=wsb[:, kc, :],
                                start=(kc == 0), stop=(kc == KC - 1)), r=[hT, wsb], w=[p])
                        kb.op("act", lambda e, p=p, so=so: e.copy(out=so[:], in_=p[:, 0:64]), r=[p], w=[so])
                        kb.dma("sp", PS[h0 + ti * 128:h0 + (ti + 1) * 128, :], so[:], r=[so])
                    if "stopA2" in cfg.dbg:
                        kb.barrier()
                        kb.flush()
                        break
                    wf = [kb.sb(st, "wf%d" % i, [128, KC, 512], F32) for i in range(2)]
                    wb = [kb.sb(st, "wb%d" % i, [128, KC, 512], BF16) for i in range(2)]
                    ost = [kb.sb(st, "ost%d" % i, [128, 512], BF16) for i in range(4)]
                    blocks = []
                    for (c0, nch, r0) in SEGS:
                        for b in range(nch // 4):
                            blocks.append((c0 + b * 512, r0 + b * 512))
                    cnt = 0
                    for bi, (c0, r0) in enumerate(blocks):
                        f = wf[bi % 2]
                        wbb = wb[bi % 2]
                        kb.dma("sp" if (bi % 2 == 0 or "spOnly" in cfg.dbg) else "pool", f[:], wv[:, :, c0:c0 + 512], w=[f])
                        kb.op("pool", lambda e, f=f, wbb=wbb: e.tensor_copy(out=wbb[:, 0:4, :], in_=f[:, 0:4, :]),
                              r=[f], w=[wbb])
                        kb.op("pool", lambda e, f=f, wbb=wbb: e.tensor_copy(out=wbb[:, 4:8, :], in_=f[:, 4:8, :]),
                              r=[f], w=[wbb])
                        for (g0, gs) in groups_of(HT):
                            for j in range(4):
                                p = pp[cnt % 4]
                                o = ost[cnt % 4]
                                for kc in range(KC):
                                    kb.op("pe", lambda e, p=p, kc=kc, j=j, wbb=wbb, g0=g0, gs=gs: e.matmul(
                                        p[:, 0:gs], lhsT=wbb[:, kc, j * 128:(j + 1) * 128], rhs=hT[:, kc, g0:g0 + gs],
                                        start=(kc == 0), stop=(kc == KC - 1)), r=[wbb, hT], w=[p])
                                if cnt % 2 == 0:
                                    kb.op("act", lambda e, p=p, o=o, gs=gs: e.copy(out=o[:, 0:gs], in_=p[:, 0:gs]),
                                          r=[p], w=[o])
                                else:
                                    kb.op("dve", lambda e, p=p, o=o, gs=gs: e.tensor_copy(out=o[:, 0:gs], in_=p[:, 0:gs]),
                                          r=[p], w=[o])
                                if "noPout" not in cfg.dbg:
                                    kb.dma("sp", P[r0 + j * 128:r0 + (j + 1) * 128, h0 + g0:h0 + g0 + gs], o[:, 0:gs], r=[o])
                                cnt += 1
                    kb.barrier()
                    kb.flush()
            if "stopA" in cfg.dbg:
                break
            W = dict(lru_conv_w=lru_conv_w, lru_conv_b=lru_conv_b, lru_wa=lru_wa, lru_ba=lru_ba, lru_wi=lru_wi,
                     lru_bi=lru_bi, lru_lambda=lru_lambda, w_branch=w_branch, w_out=w_out, rg_w=rg_w, rg_b=rg_b,
                     re_w=re_w, re_b=re_b, ew1=ew1, ew3=ew3, ew2=ew2)
            C = dict(ident_f=ident_f, ident_b=ident_b, ones_f=ones_f, ones_b=ones_b, modv=modv, AB=AB)
            phase_lru(kb, cfg, li, W, C, P, BR, load_vec_fm)
            if "noGDN" not in cfg.dbg:
                phase_gdn(kb, cfg, li, dict(W, gdn_conv_w=gdn_conv_w, gdn_a_log=gdn_a_log, gdn_dt_bias=gdn_dt_bias,
                                            gdn_norm_w=gdn_norm_w), C, P, PS, OG, BR, load_vec_fm)
            if "stopD" in cfg.dbg:
                break
            if "noSSD" not in cfg.dbg:
                phase_ssd(kb, cfg, li, dict(ssd_conv_w=ssd_conv_w, ssd_conv_b=ssd_conv_b, ssd_a_log=ssd_a_log,
                                            ssd_dt_bias=ssd_dt_bias, ssd_d=ssd_d, ssd_norm_w=ssd_norm_w),
                          C, P, PS, XC, YS, BR, load_vec_fm)
            if "stopS" in cfg.dbg:
                break
            zl = [k for k, f in ((1, "noGDN"), (2, "noSSD")) if f in cfg.dbg]
            if zl:
                with ExitStack() as st:
                    z = kb.sb(st, "zbr", [128, T], BF16)
                    kb.op("pool", lambda e: e.memset(z[:], 0.0), w=[z])
                    for k in zl:
                        for ct in range(8):
                            kb.dma("sp", BR[k, ct * 128:(ct + 1) * 128, :], z[:], r=[z])
                    kb.barrier()
                    kb.flush()
            if "stopL" in cfg.dbg:
                break
            phase_merge(kb, cfg, li, W, C, P, BR, XT)
            if "stopG" in cfg.dbg:
                break
            phase_moe(kb, cfg, li, W, C, XT, load_vec_fm)

        if not any(k.startswith("stop") for k in cfg.dbg):
            phase_final(kb, cfg, C, XT, fin_w, out_d, load_vec_fm)
        kb.barrier()
        kb.flush()
    return nc, dbg_out


def emit_norm(kb, nc, XT, AB, ni, hT, h0, HT, Lc, xg, sq, rs, pss, ones_f, hbase=None):
    XTv = XT.rearrange("(k p) t -> p k t", p=128)
    for gi, (g0, gs) in enumerate(groups_of(HT)):
        x = xg[gi % len(xg)]
        s = sq[gi % len(sq)]
        r = rs[gi % len(rs)]
        p = pss[gi % len(pss)]
        kb.dma("sp" if gi % 2 == 0 else "pool", x[:, :, 0:gs], XTv[:, :, h0 + g0:h0 + g0 + gs], w=[x])
        kb.op("act", lambda e, x=x, s=s, gs=gs: e.activation(out=s[:, :, 0:gs], in_=x[:, :, 0:gs], func=AF.Square),
              r=[x], w=[s])
        for kc in range(KC):
            kb.op("pe", lambda e, p=p, s=s, kc=kc, gs=gs: e.matmul(p[:, 0:gs], lhsT=ones_f[:], rhs=s[:, kc, 0:gs],
                                                                  start=(kc == 0), stop=(kc == KC - 1)),
                  r=[s, ones_f], w=[p])
        kb.op("act", lambda e, p=p, r=r, gs=gs: e.activation(out=r[:, 0:gs], in_=p[:, 0:gs], func=AF.Sqrt,
                                                            scale=1.0 / D, bias=EPS), r=[p], w=[r])
        kb.op("dve", lambda e, r=r, gs=gs: e.reciprocal(out=r[:, 0:gs], in_=r[:, 0:gs]), r=[r], w=[r])
        kb.op("dve", lambda e, x=x, r=r, gs=gs: e.tensor_tensor(
            out=x[:, :, 0:gs], in0=x[:, :, 0:gs], in1=r[:, 0:gs].unsqueeze(1).to_broadcast([128, KC, gs]),
            op=ALU.mult), r=[x, r], w=[x])
        a0 = h0 + g0
        segs = []
        if a0 < Lc:
            segs.append((0, min(gs, Lc - a0), 1))
        if a0 + gs > Lc:
            segs.append((max(0, Lc - a0), gs, 0))
        for kc in range(KC):
            for (s0, s1, which) in segs:
                eng = "pool" if kc % 2 == 0 else "dve"
                kb.op(eng, lambda e, x=x, kc=kc, s0=s0, s1=s1, which=which, g0=g0: e.tensor_scalar(
                    out=hT[:, kc, g0 + s0:g0 + s1], in0=x[:, kc, s0:s1],
                    scalar1=AB[:, ni, 0, kc, which:which + 1], scalar2=AB[:, ni, 1, kc, which:which + 1],
                    op0=ALU.mult, op1=ALU.add), r=[x, AB], w=[hT])


def core_inputs(inp, b, cfg):
    m = {}
    for k, v in inp.items():
        v = np.asarray(v)
        if k == "x":
            m[k] = np.ascontiguousarray(v[b], dtype=np.float32)
        elif k == "c":
            m[k] = np.ascontiguousarray(v[b:b + 1], dtype=np.float32)
        elif k == "ctx":
            m[k] = np.ascontiguousarray(v[b], dtype=np.float32)
        elif k in ("c_ctx", "final_norm_w"):
            m[k] = np.ascontiguousarray(v.reshape(1, -1), dtype=np.float32)
        elif k in ("gdn_a_log", "gdn_dt_bias", "ssd_a_log", "ssd_dt_bias", "ssd_d"):
            m[k] = np.ascontiguousarray(v.reshape(v.shape[0], -1), dtype=np.float32)
        else:
            m[k] = np.ascontiguousarray(v, dtype=np.float32)
    return m


def conv_fm(kb, eng, acc, x, cw, taps, bias_ap, segs, bias_buf=None):
    for (s0, s1) in segs:
        if bias_ap is not None:
            kb.op(eng, lambda e, s0=s0, s1=s1: e.tensor_scalar(out=acc[:, s0:s1], in0=x[:, s0:s1], scalar1=taps[2],
                                                              scalar2=bias_ap, op0=ALU.mult, op1=ALU.add),
                  r=[x, cw, bias_buf], w=[acc])
        else:
            kb.op(eng, lambda e, s0=s0, s1=s1: e.tensor_scalar(out=acc[:, s0:s1], in0=x[:, s0:s1], scalar1=taps[2],
                                                              scalar2=None, op0=ALU.mult), r=[x, cw], w=[acc])
        for j, off in ((0, -2), (1, -1), (3, 1)):
            if off < 0:
                o0, o1, i0, i1 = s0 - off, s1, s0, s1 + off
            else:
                o0, o1, i0, i1 = s0, s1 - off, s0 + off, s1
            kb.op("dve", lambda e, o0=o0, o1=o1, i0=i0, i1=i1, j=j: e.scalar_tensor_tensor(
                out=acc[:, o0:o1], in0=x[:, i0:i1], scalar=taps[j], in1=acc[:, o0:o1], op0=ALU.mult, op1=ALU.add),
                r=[x, cw, acc], w=[acc])


def phase_lru(kb, cfg, li, W, C, P, BR, load_vec_fm):
    T, Lc = cfg.T, cfg.Lc
    with ExitStack() as st:
        ptv = kb.ps(st, "l_ptv", [128, 128])
        cw = kb.sb(st, "l_cw", [128, 32], F32)
        cbias = kb.sb(st, "l_cb", [128, 8], F32)
        bab = kb.sb(st, "l_ba", [128, 2, 16], F32)
        lam = kb.sb(st, "l_lam", [128, 16], F32)
        cneg = kb.sb(st, "l_cneg", [128, 2, 16], F32)
        load_vec_fm(st, ptv, cw, cw[:], W["lru_conv_w"][li].rearrange("j (k p) -> (j k) p", p=128), 32)
        load_vec_fm(st, ptv, cbias, cbias[:], W["lru_conv_b"][li].rearrange("(k p) -> k p", p=128), 8)
        load_vec_fm(st, ptv, bab, bab[:, 0, :], W["lru_ba"][li].rearrange("d (k p) -> (d k) p", p=128), 16)
        load_vec_fm(st, ptv, bab, bab[:, 1, :], W["lru_bi"][li].rearrange("d (k p) -> (d k) p", p=128), 16)
        load_vec_fm(st, ptv, lam, lam[:], W["lru_lambda"][li].rearrange("d (k p) -> (d k) p", p=128), 16)
        kb.op("act", lambda e: e.activation(out=lam[:], in_=lam[:], func=AF.Exp, scale=-1.0), r=[lam], w=[lam])
        kb.op("act", lambda e: e.activation(out=lam[:], in_=lam[:], func=AF.Ln, bias=1.0), r=[lam], w=[lam])
        kb.op("dve", lambda e: e.tensor_scalar(out=cneg[:, 0, :], in0=lam[:], scalar1=-8.0, scalar2=None, op0=ALU.mult),
              r=[lam], w=[cneg])
        kb.op("dve", lambda e: e.tensor_scalar(out=cneg[:, 1, :], in0=lam[:], scalar1=-16.0, scalar2=None, op0=ALU.mult),
              r=[lam], w=[cneg])
        xb = kb.sb(st, "l_xb", [128, T], BF16)
        ub = kb.sb(st, "l_ub", [128, T], BF16)
        a = kb.sb(st, "l_a", [128, T], F32)
        v = kb.sb(st, "l_v", [128, T], F32)
        hs = kb.sb(st, "l_hs", [128, T], F32)
        hb = kb.sb(st, "l_hb", [128, T], F32)
        wgf = kb.sb(st, "l_wgf", [128, 4, 128], F32)
        wgb = kb.sb(st, "l_wgb", [128, 4, 128], BF16)
        tm = [kb.sb(st, "l_t%d" % i, [128, 512], F32) for i in range(4)]
        ob = [kb.sb(st, "l_ob%d" % i, [128, 512], BF16) for i in range(2)]
        pg = [kb.ps(st, "l_pg%d" % i, [128, 512]) for i in range(4)]
        segs = [(0, Lc), (Lc, T)]
        for ct in range(8):
            kb.dma("sp", xb[:], P[P_LX + ct * 128:P_LX + (ct + 1) * 128, :], w=[xb])
            conv_fm(kb, "dve", v, xb, cw, [cw[:, j * 8 + ct:j * 8 + ct + 1] for j in range(4)], cbias[:, ct:ct + 1], segs, bias_buf=cbias)
            kb.op("act", lambda e: e.copy(out=ub[:], in_=v[:]), r=[v], w=[ub])
            kb.dma("sp", xb[:], P[P_LY + ct * 128:P_LY + (ct + 1) * 128, :], w=[xb])
            for d in range(2):
                kb.dma("sp", wgf[:, d * 2 + 0, :], W["lru_wa"][li, d, ct], w=[wgf])
                kb.dma("sp", wgf[:, d * 2 + 1, :], W["lru_wi"][li, d, ct], w=[wgf])
            kb.op("pool", lambda e: e.tensor_copy(out=wgb[:], in_=wgf[:]), r=[wgf], w=[wgb])
            for d in range(2):
                col = d * 8 + ct
                for gi, (g0, gs) in enumerate(groups_of(T)):
                    pa, pi = pg[(gi % 2) * 2], pg[(gi % 2) * 2 + 1]
                    rt, it, t2 = tm[0], tm[1], tm[2]
                    kb.op("pe", lambda e, pa=pa, d=d, g0=g0, gs=gs: e.matmul(pa[:, 0:gs], lhsT=wgb[:, d * 2, :],
                                                                            rhs=ub[:, g0:g0 + gs], start=True, stop=True),
                          r=[wgb, ub], w=[pa])
                    kb.op("pe", lambda e, pi=pi, d=d, g0=g0, gs=gs: e.matmul(pi[:, 0:gs], lhsT=wgb[:, d * 2 + 1, :],
                                                                            rhs=ub[:, g0:g0 + gs], start=True, stop=True),
                          r=[wgb, ub], w=[pi])
                    kb.op("act", lambda e, pa=pa, gs=gs, col=col: e.activation(
                        out=rt[:, 0:gs], in_=pa[:, 0:gs], func=AF.Sigmoid, bias=bab[:, 0, col:col + 1]),
                        r=[pa, bab], w=[rt])
                    kb.op("act", lambda e, pi=pi, gs=gs, col=col: e.activation(
                        out=it[:, 0:gs], in_=pi[:, 0:gs], func=AF.Sigmoid, bias=bab[:, 1, col:col + 1]),
                        r=[pi, bab], w=[it])
                    kb.op("act", lambda e, g0=g0, gs=gs, col=col: e.activation(
                        out=a[:, g0:g0 + gs], in_=rt[:, 0:gs], func=AF.Exp, scale=cneg[:, 0, col:col + 1]),
                        r=[rt, cneg], w=[a])
                    kb.op("act", lambda e, gs=gs, col=col: e.activation(
                        out=t2[:, 0:gs], in_=rt[:, 0:gs], func=AF.Exp, scale=cneg[:, 1, col:col + 1]),
                        r=[rt, cneg], w=[t2])
                    kb.op("dve", lambda e, gs=gs: e.tensor_scalar(out=t2[:, 0:gs], in0=t2[:, 0:gs], scalar1=-1.0,
                                                                 scalar2=1.0, op0=ALU.mult, op1=ALU.add),
                          r=[t2], w=[t2])
                    kb.op("dve", lambda e, gs=gs: e.tensor_scalar(out=t2[:, 0:gs], in0=t2[:, 0:gs], scalar1=0.0, scalar2=None,
                                                                 op0=ALU.max), r=[t2], w=[t2])
                    kb.op("act", lambda e, gs=gs: e.activation(out=t2[:, 0:gs], in_=t2[:, 0:gs], func=AF.Sqrt),
                          r=[t2], w=[t2])
                    kb.op("dve", lambda e, g0=g0, gs=gs: e.tensor_tensor(out=it[:, 0:gs], in0=it[:, 0:gs],
                                                                        in1=ub[:, g0:g0 + gs], op=ALU.mult),
                          r=[it, ub], w=[it])
                    kb.op("dve", lambda e, g0=g0, gs=gs: e.tensor_tensor(out=v[:, g0:g0 + gs], in0=it[:, 0:gs],
                                                                        in1=t2[:, 0:gs], op=ALU.mult),
                          r=[it, t2], w=[v])
                if d == 0:
                    kb.op("dve", lambda e: e.tensor_tensor_scan(out=hs[:], data0=a[:], data1=v[:], initial=0.0,
                                                                op0=ALU.mult, op1=ALU.add), r=[a, v], w=[hs])
                else:
                    kb.op("dve", lambda e: e.tensor_tensor_scan(out=hb[:, 0:Lc][:, ::-1], data0=a[:, 0:Lc][:, ::-1],
                                                                data1=v[:, 0:Lc][:, ::-1], initial=0.0,
                                                                op0=ALU.mult, op1=ALU.add), r=[a, v], w=[hb])
                    kb.op("dve", lambda e: e.tensor_tensor_scan(out=hb[:, Lc:T][:, ::-1], data0=a[:, Lc:T][:, ::-1],
                                                                data1=v[:, Lc:T][:, ::-1], initial=hb[:, 0:1],
                                                                op0=ALU.mult, op1=ALU.add), r=[a, v, hb], w=[hb])
            for gi, (g0, gs) in enumerate(groups_of(T)):
                t0, t1 = tm[0], tm[1]
                o = ob[gi % 2]
                kb.op("dve", lambda e, g0=g0, gs=gs: e.tensor_tensor(out=t0[:, 0:gs], in0=xb[:, g0:g0 + gs],
                                                                    in1=xb[:, g0:g0 + gs], op=ALU.mult), r=[xb], w=[t0])
                kb.op("dve", lambda e, gs=gs: e.tensor_scalar(out=t0[:, 0:gs], in0=t0[:, 0:gs], scalar1=0.044715,
                                                             scalar2=1.0, op0=ALU.mult, op1=ALU.add), r=[t0], w=[t0])
                kb.op("dve", lambda e, g0=g0, gs=gs: e.tensor_tensor(out=t0[:, 0:gs], in0=t0[:, 0:gs],
                                                                    in1=xb[:, g0:g0 + gs], op=ALU.mult), r=[t0, xb], w=[t0])
                kb.op("act", lambda e, gs=gs: e.activation(out=t0[:, 0:gs], in_=t0[:, 0:gs], func=AF.Sigmoid,
                                                          scale=1.5957691216057308), r=[t0], w=[t0])
                kb.op("dve", lambda e, g0=g0, gs=gs: e.tensor_tensor(out=t0[:, 0:gs], in0=t0[:, 0:gs],
                                                                    in1=xb[:, g0:g0 + gs], op=ALU.mult), r=[t0, xb], w=[t0])
                kb.op("dve", lambda e, g0=g0, gs=gs: e.tensor_tensor(out=t1[:, 0:gs], in0=hs[:, g0:g0 + gs],
                                                                    in1=hb[:, g0:g0 + gs], op=ALU.add), r=[hs, hb], w=[t1])
                kb.op("dve", lambda e, gs=gs, o=o: e.tensor_tensor(out=o[:, 0:gs], in0=t0[:, 0:gs], in1=t1[:, 0:gs],
                                                                  op=ALU.mult), r=[t0, t1], w=[o])
                kb.dma("sp", BR[0, ct * 128:(ct + 1) * 128, g0:g0 + gs], o[:, 0:gs], r=[o])
        kb.barrier()
        kb.flush()


def gate_segs(a0, gs, Lc):
    segs = []
    if a0 < Lc:
        segs.append((0, min(gs, Lc - a0), 1))
    if a0 + gs > Lc:
        segs.append((max(0, Lc - a0), gs, 0))
    return segs


def phase_merge(kb, cfg, li, W, C, P, BR, XT):
    T, Lc = cfg.T, cfg.Lc
    modv = C["modv"]
    with ExitStack() as st:
        wbr = kb.sb(st, "m_wbr", [128, 3, KC, D], BF16)
        wo = kb.sb(st, "m_wo", [128, KC, D], BF16)
        stg = [kb.sb(st, "m_stg%d" % i, [128, KC, 512], F32) for i in range(1)]
        n = 0
        for k in range(4):
            src = (W["w_branch"][li, k] if k < 3 else W["w_out"][li]).rearrange("(k p) n -> p k n", p=128)
            for hh in range(2):
                s = stg[0]
                kb.dma("sp" if n % 2 == 0 else "pool", s[:], src[:, :, hh * 512:(hh + 1) * 512], w=[s])
                dst = wbr[:, k, :, hh * 512:(hh + 1) * 512] if k < 3 else wo[:, :, hh * 512:(hh + 1) * 512]
                kb.op("pool" if n % 2 == 0 else "act",
                      (lambda e, dst=dst, s=s: e.tensor_copy(out=dst, in_=s[:])) if n % 2 == 0 else
                      (lambda e, dst=dst, s=s: e.copy(out=dst, in_=s[:])), r=[s], w=[wbr if k < 3 else wo])
                n += 1
        brt = [kb.sb(st, "m_br%d" % i, [128, 3, KC, 512], BF16) for i in range(1)]
        gt = [kb.sb(st, "m_gt%d" % i, [128, 3, KC, 512], BF16) for i in range(1)]
        sg = [kb.sb(st, "m_sg%d" % i, [128, 512], F32) for i in range(2)]
        acc = kb.sb(st, "m_acc", [128, KC, 512], F32)
        accb = kb.sb(st, "m_accb", [128, KC, 512], BF16)
        xg = [kb.sb(st, "m_xg%d" % i, [128, KC, 512], F32) for i in range(1)]
        pp = [kb.ps(st, "m_pp%d" % i, [128, 512]) for i in range(4)]
        XTv = XT.rearrange("(k p) t -> p k t", p=128)
        cnt = 0
        for gi, (g0, gs) in enumerate(groups_of(T)):
            b_, g_, x_ = brt[0], gt[0], xg[0]
            for k in range(3):
                kb.dma("sp", b_[:, k, :, 0:gs], BR[k].rearrange("(k p) t -> p k t", p=128)[:, :, g0:g0 + gs], w=[b_])
                kb.dma("pool", g_[:, k, :, 0:gs],
                       P[P_GATE + k * 1024:P_GATE + (k + 1) * 1024, :].rearrange("(k p) t -> p k t", p=128)[:, :, g0:g0 + gs],
                       w=[g_])
            kb.dma("sp", x_[:, :, 0:gs], XTv[:, :, g0:g0 + gs], w=[x_])
            for dc in range(KC):
                for k in range(3):
                    p = pp[cnt % 4]
                    s_ = sg[cnt % 2]
                    cnt += 1
                    for kc in range(KC):
                        kb.op("pe", lambda e, p=p, k=k, kc=kc, dc=dc, b_=b_, gs=gs: e.matmul(
                            p[:, 0:gs], lhsT=wbr[:, k, kc, dc * 128:(dc + 1) * 128], rhs=b_[:, k, kc, 0:gs],
                            start=(kc == 0), stop=(kc == KC - 1)), r=[wbr, b_], w=[p])
                    kb.op("act", lambda e, s_=s_, g_=g_, k=k, dc=dc, gs=gs: e.activation(
                        out=s_[:, 0:gs], in_=g_[:, k, dc, 0:gs], func=AF.Sigmoid), r=[g_], w=[s_])
                    if k == 0:
                        kb.op("dve", lambda e, p=p, s_=s_, dc=dc, gs=gs: e.tensor_tensor(
                            out=acc[:, dc, 0:gs], in0=p[:, 0:gs], in1=s_[:, 0:gs], op=ALU.mult), r=[p, s_], w=[acc])
                    else:
                        kb.op("dve", lambda e, p=p, s_=s_, gs=gs: e.tensor_tensor(
                            out=s_[:, 0:gs], in0=p[:, 0:gs], in1=s_[:, 0:gs], op=ALU.mult), r=[p, s_], w=[s_])
                        kb.op("dve", lambda e, s_=s_, dc=dc, gs=gs: e.tensor_tensor(
                            out=acc[:, dc, 0:gs], in0=acc[:, dc, 0:gs], in1=s_[:, 0:gs], op=ALU.add), r=[acc, s_], w=[acc])
            kb.op("act", lambda e, gs=gs: e.copy(out=accb[:, :, 0:gs], in_=acc[:, :, 0:gs]), r=[acc], w=[accb])
            for dc in range(KC):
                p = pp[cnt % 4]
                cnt += 1
                for kc in range(KC):
                    kb.op("pe", lambda e, p=p, kc=kc, dc=dc, gs=gs: e.matmul(
                        p[:, 0:gs], lhsT=wo[:, kc, dc * 128:(dc + 1) * 128], rhs=accb[:, kc, 0:gs],
                        start=(kc == 0), stop=(kc == KC - 1)), r=[wo, accb], w=[p])
                for (s0, s1, which) in gate_segs(g0, gs, Lc):
                    kb.op("dve", lambda e, p=p, x_=x_, dc=dc, s0=s0, s1=s1, which=which: e.scalar_tensor_tensor(
                        out=x_[:, dc, s0:s1], in0=p[:, s0:s1], scalar=modv[:, 16 + dc, which:which + 1],
                        in1=x_[:, dc, s0:s1], op0=ALU.mult, op1=ALU.add), r=[p, x_, modv], w=[x_])
            kb.dma("sp", XTv[:, :, g0:g0 + gs], x_[:, :, 0:gs], r=[x_])
        kb.barrier()
        kb.flush()


def phase_moe(kb, cfg, li, W, C, XT, load_vec_fm):
    T, Lc, FF = cfg.T, cfg.Lc, cfg.ff
    FC = FF // 128
    modv, AB, ones_f, ident_f = C["modv"], C["AB"], C["ones_f"], C["ident_f"]
    NT_ = T // 128
    nq = (NT_ + 13) // 14
    bounds = [(NT_ * i // nq) * 128 for i in range(nq + 1)]
    quarters = [(bounds[i], bounds[i + 1]) for i in range(nq)]
    XTv = XT.rearrange("(k p) t -> p k t", p=128)
    for (h0, h1) in quarters:
        HT = h1 - h0
        NTq = HT // 128
        with ExitStack() as st:
            hq = kb.sb(st, "e_hq", [128, KC, HT], BF16)
            yacc = kb.sb(st, "e_yacc", [128, KC, HT], BF16)
            with ExitStack() as st2:
                xg = [kb.sb(st2, "e_xg%d" % i, [128, KC, 512], F32) for i in range(2)]
                sq = [kb.sb(st2, "e_sq%d" % i, [128, KC, 512], F32) for i in range(2)]
                rs = [kb.sb(st2, "e_rs%d" % i, [128, 512], F32) for i in range(2)]
                pss = [kb.ps(st2, "e_pss%d" % i, [128, 512]) for i in range(2)]
                emit_norm(kb, None, XT, AB, 1, hq, h0, HT, Lc, xg, sq, rs, pss, ones_f)
                kb.barrier()
                kb.flush()
            wrf = kb.sb(st, "e_wrf", [128, KC, 36], F32)
            wrb = kb.sb(st, "e_wrb", [128, KC, 36], BF16)
            kb.dma("sp", wrf[:, :, 0:4], W["rg_w"][li].rearrange("(k p) n -> p k n", p=128), w=[wrf])
            kb.dma("sp", wrf[:, :, 4:36], W["re_w"][li].rearrange("(k p) n -> p k n", p=128), w=[wrf])
            kb.op("pool", lambda e: e.tensor_copy(out=wrb[:], in_=wrf[:]), r=[wrf], w=[wrb])
            rb = kb.sb(st, "e_rb", [128, 36], F32)
            kb.dma("sp", rb[:, 0:4], W["rg_b"][li:li + 1, :].to_broadcast([128, 4]), w=[rb])
            kb.dma("sp", rb[:, 4:36], W["re_b"][li:li + 1, :].to_broadcast([128, 32]), w=[rb])
            L = kb.sb(st, "e_L", [128, NTq, 36], F32)
            st3 = ExitStack()
            pl = [kb.ps(st3, "e_pl%d" % i, [128, 64]) for i in range(2)]
            for ti in range(NTq):
                p = pl[ti % 2]
                for kc in range(KC):
                    kb.op("pe", lambda e, p=p, kc=kc, ti=ti: e.matmul(
                        p[:, 0:36], lhsT=hq[:, kc, ti * 128:(ti + 1) * 128], rhs=wrb[:, kc, :],
                        start=(kc == 0), stop=(kc == KC - 1)), r=[hq, wrb], w=[p])
                kb.op("dve", lambda e, p=p, ti=ti: e.tensor_tensor(out=L[:, ti, :], in0=p[:, 0:36], in1=rb[:], op=ALU.add),
                      r=[p, rb], w=[L])
            gm = kb.sb(st, "e_gm", [128, NTq], F32)
            og = kb.sb(st, "e_og", [128, NTq, 4], F32)
            eg = kb.sb(st, "e_eg", [128, NTq, 4], F32)
            pgp = kb.sb(st, "e_pgp", [128, NTq], F32)
            t48 = kb.sb(st, "e_t48", [128, NTq, 4, 8], F32)
            es8 = kb.sb(st, "e_es8", [128, NTq, 8], F32)
            eq1 = kb.sb(st, "e_eq1", [128, NTq, 8], F32)
            eq2 = kb.sb(st, "e_eq2", [128, NTq, 8], F32)
            v1 = kb.sb(st, "e_v1", [128, NTq], F32)
            v2 = kb.sb(st, "e_v2", [128, NTq], F32)
            wa_ = kb.sb(st, "e_wa", [128, NTq], F32)
            wb_ = kb.sb(st, "e_wb", [128, NTq], F32)
            RW = kb.sb(st, "e_RW", [128, NTq, 4, 8], F32)

            def bc(ap, shape):
                return ap.to_broadcast(shape)
            dv = lambda fn, r, w: kb.op("dve", fn, r=r, w=w)
            dv(lambda e: e.tensor_reduce(out=gm[:], in_=L[:, :, 0:4], axis=AX.X, op=ALU.max), [L], [gm])
            dv(lambda e: e.tensor_tensor(out=og[:], in0=L[:, :, 0:4], in1=bc(gm[:].unsqueeze(2), [128, NTq, 4]),
                                         op=ALU.is_equal), [L, gm], [og])
            dv(lambda e: e.tensor_tensor(out=eg[:], in0=L[:, :, 0:4], in1=bc(gm[:].unsqueeze(2), [128, NTq, 4]),
                                         op=ALU.subtract), [L, gm], [eg])
            kb.op("act", lambda e: e.activation(out=eg[:], in_=eg[:], func=AF.Exp), r=[eg], w=[eg])
            dv(lambda e: e.tensor_reduce(out=pgp[:], in_=eg[:], axis=AX.X, op=ALU.add), [eg], [pgp])
            dv(lambda e: e.reciprocal(out=pgp[:], in_=pgp[:]), [pgp], [pgp])
            dv(lambda e: e.tensor_tensor(out=t48[:], in0=L[:, :, 4:36].rearrange("p t (g i) -> p t g i", g=4),
                                         in1=bc(og[:].unsqueeze(3), [128, NTq, 4, 8]), op=ALU.mult), [L, og], [t48])
            dv(lambda e: e.tensor_reduce(out=es8[:], in_=t48[:].rearrange("p t g i -> p t i g"), axis=AX.X, op=ALU.add),
               [t48], [es8])
            dv(lambda e: e.tensor_reduce(out=v1[:], in_=es8[:], axis=AX.X, op=ALU.max), [es8], [v1])
            dv(lambda e: e.tensor_tensor(out=eq1[:], in0=es8[:], in1=bc(v1[:].unsqueeze(2), [128, NTq, 8]),
                                         op=ALU.is_equal), [es8, v1], [eq1])
            dv(lambda e: e.scalar_tensor_tensor(out=es8[:], in0=eq1[:], scalar=-1e30, in1=es8[:], op0=ALU.mult,
                                                op1=ALU.add), [eq1, es8], [es8])
            dv(lambda e: e.tensor_reduce(out=v2[:], in_=es8[:], axis=AX.X, op=ALU.max), [es8], [v2])
            dv(lambda e: e.tensor_tensor(out=eq2[:], in0=es8[:], in1=bc(v2[:].unsqueeze(2), [128, NTq, 8]),
                                         op=ALU.is_equal), [es8, v2], [eq2])
            dv(lambda e: e.tensor_tensor(out=wa_[:], in0=v2[:], in1=v1[:], op=ALU.subtract), [v1, v2], [wa_])
            kb.op("act", lambda e: e.activation(out=wa_[:], in_=wa_[:], func=AF.Exp), r=[wa_], w=[wa_])
            dv(lambda e: e.tensor_scalar(out=wa_[:], in0=wa_[:], scalar1=1.0, scalar2=None, op0=ALU.add), [wa_], [wa_])
            dv(lambda e: e.reciprocal(out=wa_[:], in_=wa_[:]), [wa_], [wa_])
            dv(lambda e: e.tensor_tensor(out=wa_[:], in0=wa_[:], in1=pgp[:], op=ALU.mult), [wa_, pgp], [wa_])
            dv(lambda e: e.tensor_tensor(out=wb_[:], in0=pgp[:], in1=wa_[:], op=ALU.subtract), [wa_, pgp], [wb_])
            dv(lambda e: e.tensor_tensor(out=eq1[:], in0=eq1[:], in1=bc(wa_[:].unsqueeze(2), [128, NTq, 8]), op=ALU.mult),
               [eq1, wa_], [eq1])
            dv(lambda e: e.tensor_tensor(out=eq2[:], in0=eq2[:], in1=bc(wb_[:].unsqueeze(2), [128, NTq, 8]), op=ALU.mult),
               [eq2, wb_], [eq2])
            dv(lambda e: e.tensor_tensor(out=eq1[:], in0=eq1[:], in1=eq2[:], op=ALU.add), [eq1, eq2], [eq1])
            dv(lambda e: e.tensor_tensor(out=RW[:], in0=bc(eq1[:].unsqueeze(2), [128, NTq, 4, 8]),
                                         in1=bc(og[:].unsqueeze(3), [128, NTq, 4, 8]), op=ALU.mult), [eq1, og], [RW])
            RWT = kb.sb(st, "e_RWT", [32, HT], F32)
            ptr = [kb.ps(st3, "e_ptr%d" % i, [32, 128]) for i in range(2)]
            for ti in range(NTq):
                p = ptr[ti % 2]
                kb.op("pe", lambda e, p=p, ti=ti: e.transpose(p[:], RW[:, ti, :, :].rearrange("p g i -> p (g i)"), ident_f[:]),
                      r=[RW, ident_f], w=[p])
                kb.op("act", lambda e, p=p, ti=ti: e.copy(out=RWT[:, ti * 128:(ti + 1) * 128], in_=p[:]), r=[p], w=[RWT])
            kb.op("pool", lambda e: e.memset(yacc[:], 0.0), w=[yacc])
            kb.barrier()
            kb.flush()
            st3.close()
            w1f = kb.sb(st, "e_w1f", [128, KC, FF], F32)
            w3f = kb.sb(st, "e_w3f", [128, KC, FF], F32)
            w2f = kb.sb(st, "e_w2f", [128, FC, D], F32)
            w1b = [kb.sb(st, "e_w1b%d" % i, [128, KC, FF], BF16) for i in range(2)]
            w3b = [kb.sb(st, "e_w3b%d" % i, [128, KC, FF], BF16) for i in range(2)]
            w2b = [kb.sb(st, "e_w2b%d" % i, [128, FC, D], BF16) for i in range(2)]
            sel = kb.sb(st, "e_sel", [32, 128], F32)
            actT = [kb.sb(st, "e_act%d" % i, [128, FC, 512], BF16) for i in range(2)]
            s1 = [kb.sb(st, "e_s1%d" % i, [128, 512], F32) for i in range(2)]
            wrep = [kb.sb(st, "e_wrep%d" % i, [128, 512], F32) for i in range(2)]
            ph1 = [kb.ps(st, "e_ph1%d" % i, [128, 512]) for i in range(2)]
            ph3 = [kb.ps(st, "e_ph3%d" % i, [128, 512]) for i in range(2)]
            py = [kb.ps(st, "e_py%d" % i, [128, 512]) for i in range(2)]
            pw = kb.ps(st, "e_pw", [128, 512])
            cnt = 0
            for ex in range(NEXP):
                b = ex % 2
                kb.dma("sp", w1f[:], W["ew1"][li, ex].rearrange("(k p) f -> p k f", p=128), w=[w1f])
                kb.dma("pool", w3f[:], W["ew3"][li, ex].rearrange("(k p) f -> p k f", p=128), w=[w3f])
                kb.dma("sp", w2f[:], W["ew2"][li, ex].rearrange("(k p) n -> p k n", p=128), w=[w2f])
                kb.op("pool", lambda e, b=b: e.tensor_copy(out=w1b[b][:], in_=w1f[:]), r=[w1f], w=[w1b[b]])
                kb.op("pool", lambda e, b=b: e.tensor_copy(out=w3b[b][:], in_=w3f[:]), r=[w3f], w=[w3b[b]])
                kb.op("pool", lambda e, b=b: e.tensor_copy(out=w2b[b][:], in_=w2f[:]), r=[w2f], w=[w2b[b]])
                kb.op("pool", lambda e, ex=ex: e.tensor_copy(out=sel[:], in_=ident_f[0:32, ex:ex + 1].to_broadcast([32, 128])),
                      r=[ident_f], w=[sel])
                for gi, (g0, gs) in enumerate(groups_of(HT)):
                    at = actT[gi % 2]
                    wr = wrep[gi % 2]
                    kb.op("pe", lambda e, g0=g0, gs=gs: e.matmul(pw[:, 0:gs], lhsT=sel[:], rhs=RWT[:, g0:g0 + gs],
                                                                start=True, stop=True), r=[sel, RWT], w=[pw])
                    kb.op("act", lambda e, wr=wr, gs=gs: e.copy(out=wr[:, 0:gs], in_=pw[:, 0:gs]), r=[pw], w=[wr])
                    for fc in range(FC):
                        p1, p3 = ph1[cnt % 2], ph3[cnt % 2]
                        s_ = s1[cnt % 2]
                        cnt += 1
                        for kc in range(KC):
                            kb.op("pe", lambda e, p1=p1, kc=kc, fc=fc, b=b, g0=g0, gs=gs: e.matmul(
                                p1[:, 0:gs], lhsT=w1b[b][:, kc, fc * 128:(fc + 1) * 128], rhs=hq[:, kc, g0:g0 + gs],
                                start=(kc == 0), stop=(kc == KC - 1)), r=[w1b[b], hq], w=[p1])
                        for kc in range(KC):
                            kb.op("pe", lambda e, p3=p3, kc=kc, fc=fc, b=b, g0=g0, gs=gs: e.matmul(
                                p3[:, 0:gs], lhsT=w3b[b][:, kc, fc * 128:(fc + 1) * 128], rhs=hq[:, kc, g0:g0 + gs],
                                start=(kc == 0), stop=(kc == KC - 1)), r=[w3b[b], hq], w=[p3])
                        kb.op("act", lambda e, p1=p1, s_=s_, gs=gs: e.activation(out=s_[:, 0:gs], in_=p1[:, 0:gs], func=AF.Silu),
                              r=[p1], w=[s_])
                        kb.op("dve", lambda e, p3=p3, s_=s_, gs=gs: e.tensor_tensor(out=s_[:, 0:gs], in0=p3[:, 0:gs],
                                                                                    in1=s_[:, 0:gs], op=ALU.mult),
                              r=[p3, s_], w=[s_])
                        kb.op("dve", lambda e, s_=s_, at=at, wr=wr, fc=fc, gs=gs: e.tensor_tensor(
                            out=at[:, fc, 0:gs], in0=s_[:, 0:gs], in1=wr[:, 0:gs], op=ALU.mult), r=[s_, wr], w=[at])
                    for dc in range(KC):
                        p = py[dc % 2]
                        for fc in range(FC):
                            kb.op("pe", lambda e, p=p, fc=fc, dc=dc, b=b, at=at, gs=gs: e.matmul(
                                p[:, 0:gs], lhsT=w2b[b][:, fc, dc * 128:(dc + 1) * 128], rhs=at[:, fc, 0:gs],
                                start=(fc == 0), stop=(fc == FC - 1)), r=[w2b[b], at], w=[p])
                        kb.op("dve", lambda e, p=p, dc=dc, g0=g0, gs=gs: e.tensor_tensor(
                            out=yacc[:, dc, g0:g0 + gs], in0=p[:, 0:gs], in1=yacc[:, dc, g0:g0 + gs], op=ALU.add),
                            r=[p, yacc], w=[yacc])
            xr = [kb.sb(st, "e_xr%d" % i, [128, KC, 512], F32) for i in range(1)]
            for gi, (g0, gs) in enumerate(groups_of(HT)):
                x_ = xr[0]
                kb.dma("sp", x_[:, :, 0:gs], XTv[:, :, h0 + g0:h0 + g0 + gs], w=[x_])
                for dc in range(KC):
                    for (s0, s1_, which) in gate_segs(h0 + g0, gs, Lc):
                        kb.op("dve", lambda e, x_=x_, dc=dc, s0=s0, s1_=s1_, which=which, g0=g0: e.scalar_tensor_tensor(
                            out=x_[:, dc, s0:s1_], in0=yacc[:, dc, g0 + s0:g0 + s1_], scalar=modv[:, 40 + dc, which:which + 1],
                            in1=x_[:, dc, s0:s1_], op0=ALU.mult, op1=ALU.add), r=[yacc, x_, modv], w=[x_])
                kb.dma("sp", XTv[:, :, h0 + g0:h0 + g0 + gs], x_[:, :, 0:gs], r=[x_])
            kb.barrier()
            kb.flush()


def phase_final(kb, cfg, C, XT, fin_w, out_d, load_vec_fm):
    T, Lc, Ll = cfg.T, cfg.Lc, cfg.Ll
    ones_f, ident_f = C["ones_f"], C["ident_f"]
    with ExitStack() as st:
        ABf = kb.sb(st, "f_AB", [128, 1, 2, KC, 2], F32)
        ptv = kb.ps(st, "f_ptv", [128, 128])
        kb.op("pool", lambda e: e.memset(ABf[:], 0.0), w=[ABf])
        for c_ in range(2):
            load_vec_fm(st, ptv, ABf, ABf[:, 0, 0, :, c_], fin_w.rearrange("o (k p) -> (o k) p", p=128), KC)
        xg = [kb.sb(st, "f_xg%d" % i, [128, KC, 512], F32) for i in range(2)]
        sq = [kb.sb(st, "f_sq%d" % i, [128, KC, 512], F32) for i in range(2)]
        rs = [kb.sb(st, "f_rs%d" % i, [128, 512], F32) for i in range(2)]
        pss = [kb.ps(st, "f_pss%d" % i, [128, 512]) for i in range(2)]
        hf = [kb.sb(st, "f_hf%d" % i, [128, KC, 512], F32) for i in range(2)]
        ot = [kb.sb(st, "f_ot%d" % i, [128, D], F32) for i in range(2)]
        pt = [kb.ps(st, "f_pt%d" % i, [128, 4, 128]) for i in range(4)]
        n = 0
        for gi, (g0, gs) in enumerate(groups_of(Ll)):
            h = hf[gi % 2]
            emit_norm(kb, None, XT, ABf, 0, h, Lc + g0, gs, 0, [xg[gi % 2]], [sq[gi % 2]], [rs[gi % 2]], [pss[gi % 2]],
                      ones_f, hbase=0)
            for ti in range(gs // 128):
                o = ot[n % 2]
                for hh in range(2):
                    p = pt[(n * 2 + hh) % 4]
                    for j in range(4):
                        kc = hh * 4 + j
                        kb.op("pe", lambda e, p=p, j=j, kc=kc, h=h, ti=ti: e.transpose(
                            p[:, j, :], h[:, kc, ti * 128:(ti + 1) * 128], ident_f[:]), r=[h, ident_f], w=[p])
                    if hh == 0:
                        kb.op("act", lambda e, p=p, o=o: e.copy(out=o[:, 0:512].rearrange("p (j f) -> p j f", j=4), in_=p[:]),
                              r=[p], w=[o])
                    else:
                        kb.op("dve", lambda e, p=p, o=o: e.tensor_copy(out=o[:, 512:1024].rearrange("p (j f) -> p j f", j=4),
                                                                       in_=p[:]), r=[p], w=[o])
                kb.dma("sp", out_d[g0 + ti * 128:g0 + (ti + 1) * 128, :], o[:], r=[o])
                n += 1
        kb.barrier()
        kb.flush()


def kernel(**inputs):
    cfg = Cfg()
    nc, _ = build(cfg)
    B = inputs["x"].shape[0]
    in_maps = [core_inputs(inputs, i % B, cfg) for i in range(8)]
    res = run_bass_kernel_spmd(nc, in_maps, core_ids=list(range(8)))
    out = np.stack([np.asarray(res.results[b]["out"], dtype=np.float32) for b in range(B)], axis=0)
    return out


class View:
    def __init__(self, ap, name, parent):
        self.ap = ap
        self.name = name
        self.parent = parent

    @property
    def lw(self):
        return self.parent.lw

    @lw.setter
    def lw(self, v):
        self.parent.lw = v

    @property
    def rd(self):
        return self.parent.rd

    @rd.setter
    def rd(self, v):
        self.parent.rd = v

    def __getitem__(self, k):
        return self.ap[k]


def mk_tri(kb, t, pattern, cm, cmp):
    kb.op("pool", lambda e: e.memset(t[:], 1.0), w=[t])
    kb.op("pool", lambda e: e.affine_select(out=t[:], in_=t[:], pattern=pattern, compare_op=cmp, fill=0.0, base=0,
                                             channel_multiplier=cm), r=[t], w=[t])


def phase_gdn(kb, cfg, li, W, C, P, PS, OG, BR, load_vec_fm):
    T, Lc = cfg.T, cfg.Lc
    NT = T // 128
    NTc = Lc // 128
    ident_f, ident_b, ones_f = C["ident_f"], C["ident_b"], C["ones_f"]
    SCALE = 128 ** -0.5
    with ExitStack() as st:
        L = [kb.sb(st, "g_L%d" % d, [128, 128], F32) for d in range(2)]
        MS = [kb.sb(st, "g_MS%d" % d, [128, 128], F32) for d in range(2)]
        mk_tri(kb, L[0], [[1, 128]], -1, ALU.is_ge)
        mk_tri(kb, MS[0], [[1, 128]], -1, ALU.is_gt)
        mk_tri(kb, L[1], [[-1, 128]], 1, ALU.is_ge)
        mk_tri(kb, MS[1], [[-1, 128]], 1, ALU.is_gt)
        ptv = kb.ps(st, "g_ptv", [128, 128])
        BDs = []
        for bs in (16, 32, 64):
            nb = 128 // bs
            E = kb.sb(st, "g_E%d" % bs, [nb, 128], F32)
            kb.op("pool", lambda e, E=E: e.memset(E[:], 1.0), w=[E])
            kb.op("pool", lambda e, E=E, bs=bs: e.affine_select(out=E[:], in_=E[:], pattern=[[1, 128]], compare_op=ALU.is_ge,
                                                                fill=0.0, base=0, channel_multiplier=-bs), r=[E], w=[E])
            kb.op("pool", lambda e, E=E, bs=bs: e.affine_select(out=E[:], in_=E[:], pattern=[[-1, 128]], compare_op=ALU.is_ge,
                                                                fill=0.0, base=bs - 1, channel_multiplier=bs), r=[E], w=[E])
            BD = kb.sb(st, "g_BD%d" % bs, [128, 128], F32)
            kb.op("pe", lambda e, E=E: e.matmul(ptv[:], lhsT=E[:], rhs=E[:], start=True, stop=True), r=[E], w=[ptv])
            kb.op("dve", lambda e, BD=BD: e.tensor_copy(out=BD[:], in_=ptv[:]), r=[ptv], w=[BD])
            BDs.append(BD)
        BD16 = BDs[0]
        OFF = [kb.sb(st, "g_OFF%d" % i, [128, 128], F32) for i in range(3)]
        kb.op("dve", lambda e: e.tensor_tensor(out=OFF[0][:], in0=BDs[1][:], in1=BDs[0][:], op=ALU.subtract), r=[BDs[0], BDs[1]], w=[OFF[0]])
        kb.op("dve", lambda e: e.tensor_tensor(out=OFF[1][:], in0=BDs[2][:], in1=BDs[1][:], op=ALU.subtract), r=[BDs[1], BDs[2]], w=[OFF[1]])
        kb.op("dve", lambda e: e.tensor_scalar(out=OFF[2][:], in0=BDs[2][:], scalar1=-1.0, scalar2=1.0, op0=ALU.mult, op1=ALU.add),
              r=[BDs[2]], w=[OFF[2]])
        cw = kb.sb(st, "g_cw", [128, 96], F32)
        load_vec_fm(st, ptv, cw, cw[:], W["gdn_conv_w"][li].rearrange("j (k p) -> (j k) p", p=128), 96)
        pst = kb.sb(st, "g_pst", [128, NT, 32], F32)
        for t0_ in range(0, NT, 8):
            t1_ = min(NT, t0_ + 8)
            kb.dma("sp", pst[:, t0_:t1_, :], PS.rearrange("(t p) n -> p t n", p=128)[:, t0_:t1_, 0:32], w=[pst])
        alog = kb.sb(st, "g_alog", [128, 16], F32)
        dtb = kb.sb(st, "g_dtb", [128, 16], F32)
        kb.dma("sp", alog[:], W["gdn_a_log"][li:li + 1, :].to_broadcast([128, 16]), w=[alog])
        kb.dma("sp", dtb[:], W["gdn_dt_bias"][li:li + 1, :].to_broadcast([128, 16]), w=[dtb])
        names = ["beta", "negb", "g", "gcs", "gtot", "egcs", "kdsc", "etot"]
        tb = {n: kb.sb(st, "g_" + n, [128, NT, 16], F32) for n in names}
        beta, negb, g, gcs, gtot, egcs, kdsc, etot = [tb[n] for n in names]
        kb.op("act", lambda e: e.activation(out=beta[:], in_=pst[:, :, 0:16], func=AF.Sigmoid), r=[pst], w=[beta])
        kb.op("dve", lambda e: e.tensor_scalar(out=negb[:], in0=beta[:], scalar1=-1.0, scalar2=None, op0=ALU.mult),
              r=[beta], w=[negb])
        kb.op("dve", lambda e: e.tensor_tensor(out=g[:], in0=pst[:, :, 16:32],
                                                in1=dtb[:].unsqueeze(1).to_broadcast([128, NT, 16]), op=ALU.add),
              r=[pst, dtb], w=[g])
        kb.op("act", lambda e: e.activation(out=g[:], in_=g[:], func=AF.Exp), r=[g], w=[g])
        kb.op("act", lambda e: e.activation(out=g[:], in_=g[:], func=AF.Ln, bias=1.0), r=[g], w=[g])
        kb.op("act", lambda e: e.activation(out=alog[:], in_=alog[:], func=AF.Exp), r=[alog], w=[alog])
        kb.op("dve", lambda e: e.tensor_scalar(out=alog[:], in0=alog[:], scalar1=-1.0, scalar2=None, op0=ALU.mult),
              r=[alog], w=[alog])
        kb.op("dve", lambda e: e.tensor_tensor(out=g[:], in0=g[:], in1=alog[:].unsqueeze(1).to_broadcast([128, NT, 16]),
                                                op=ALU.mult), r=[g, alog], w=[g])
        pcs = [kb.ps(st, "g_pcs%d" % i, [128, 512]) for i in range(2)]
        TG = 32
        for d in range(2):
            for t0 in range(0, NT, TG):
                t1 = min(NT, t0 + TG)
                n = (t1 - t0) * 8
                kb.op("pe", lambda e, d=d, t0=t0, t1=t1, n=n: e.matmul(
                    pcs[0][:, 0:n].rearrange("p (t h) -> p t h", h=8), lhsT=L[d][:], rhs=g[:, t0:t1, d * 8:(d + 1) * 8],
                    start=True, stop=True), r=[L[d], g], w=[pcs[0]])
                kb.op("dve", lambda e, d=d, t0=t0, t1=t1, n=n: e.tensor_copy(
                    out=gcs[:, t0:t1, d * 8:(d + 1) * 8], in_=pcs[0][:, 0:n].rearrange("p (t h) -> p t h", h=8)),
                    r=[pcs[0]], w=[gcs])
                kb.op("pe", lambda e, d=d, t0=t0, t1=t1, n=n: e.matmul(
                    pcs[1][:, 0:n].rearrange("p (t h) -> p t h", h=8), lhsT=ones_f[:], rhs=g[:, t0:t1, d * 8:(d + 1) * 8],
                    start=True, stop=True), r=[ones_f, g], w=[pcs[1]])
                kb.op("dve", lambda e, d=d, t0=t0, t1=t1, n=n: e.tensor_copy(
                    out=gtot[:, t0:t1, d * 8:(d + 1) * 8], in_=pcs[1][:, 0:n].rearrange("p (t h) -> p t h", h=8)),
                    r=[pcs[1]], w=[gtot])
        kb.op("act", lambda e: e.activation(out=egcs[:], in_=gcs[:], func=AF.Exp), r=[gcs], w=[egcs])
        kb.op("dve", lambda e: e.tensor_tensor(out=kdsc[:], in0=gtot[:], in1=gcs[:], op=ALU.subtract), r=[gtot, gcs], w=[kdsc])
        kb.op("act", lambda e: e.activation(out=kdsc[:], in_=kdsc[:], func=AF.Exp), r=[kdsc], w=[kdsc])
        kb.op("act", lambda e: e.activation(out=etot[:], in_=gtot[:], func=AF.Exp), r=[gtot], w=[etot])
        kb.barrier()
        kb.flush()
        if "stopD1" in cfg.dbg:
            return
        import os as _os
        if _os.environ.get("GPAD"):
            _pad = kb.sb(st, "g_pad", [128, int(_os.environ["GPAD"]) * 1024], mybir.dt.uint8)
        xb = kb.sb(st, "g_xb", [128, T], BF16)
        cacc = kb.sb(st, "g_cacc", [128, T], F32)
        qn = kb.sb(st, "g_qn", [128, T], BF16)
        kn = kb.sb(st, "g_kn", [128, T], BF16)
        vc = kb.sb(st, "g_vc", [128, T], BF16)
        sqt = [kb.sb(st, "g_sq%d" % i, [128, 512], F32) for i in range(2)]
        rst = [kb.sb(st, "g_rs%d" % i, [128, 512], F32) for i in range(2)]
        Sf = [kb.sb(st, "g_Sf%d" % d, [128, 128], F32) for d in range(2)]
        Sb = [kb.sb(st, "g_Sb%d" % d, [128, 128], BF16) for d in range(2)]
        bankF = [kb.ps(st, "g_bf%d" % i, [128, 512]) for i in range(4)]
        bankB = kb.ps(st, "g_bb", [128, 8, 128], BF16)

        def views(u):
            a, b = bankF[(u % 2) * 2], bankF[(u % 2) * 2 + 1]
            v = {}
            v["G"] = View(a[:, 0:128], "vG", a)
            v["KK"] = View(a[:, 128:256], "vKK", a)
            v["X"] = View(a[:, 256:384], "vX", a)
            v["u0"] = View(a[:, 384:512], "vu0", a)
            v["pm"] = View(b[:, 0:256], "vpm", b)
            v["pn"] = View(b[:, 256:384], "vpn", b)
            v["at"] = View(b[:, 384:512], "vat", b)
            o = (u % 2) * 4
            v["tk"] = View(bankB[:, o + 0, :], "vtk", bankB)
            v["tv"] = View(bankB[:, o + 1, :], "vtv", bankB)
            v["tn"] = View(bankB[:, o + 2, :], "vtn", bankB)
            return v
        vsets = [views(0), views(1)]
        seqv = []
        for d in range(2):
            c_ = pcs[d]
            seqv.append({"wS": View(c_[:, 0:128], "vwS", c_), "o": View(c_[:, 128:256], "vo", c_),
                         "sn": View(c_[:, 256:384], "vsn", c_)})

        def tmpset(u):
            t = {}
            for nm in ("gL", "Dm", "Er", "dS", "dI", "NTf", "TTf", "u0b", "osb"):
                t[nm] = kb.sb(st, "g_%s%d" % (nm, u), [128, 128], F32)
            for nm in ("kg", "kd", "vT", "qg", "Pb0", "Pb1", "XTb", "atb", "vn", "Nb", "Na", "Nc", "Nd", "Tn", "Wb"):
                t[nm] = kb.sb(st, "g_%s%d" % (nm, u), [128, 128], BF16)
            t["PT0"] = kb.sb(st, "g_PT0%d" % u, [128, 256], BF16)
            t["PT1"] = kb.sb(st, "g_PT1%d" % u, [128, 256], BF16)
            return t
        tsets = [tmpset(0), tmpset(1), tmpset(2), tmpset(3)]
        order = [list(range(NT)), list(range(NTc - 1, -1, -1)) + list(range(NT - 1, NTc - 1, -1))]
        unit = 0
        for h in range(8):
            for sect, dst in ((0, qn), (1, kn), (2, vc)):
                kb.dma("sp", xb[:], P[P_Q + sect * 1024 + h * 128:P_Q + sect * 1024 + (h + 1) * 128, :], w=[xb])
                col = sect * 8 + h
                conv_fm(kb, "dve", cacc, xb, cw, [cw[:, j * 24 + col:j * 24 + col + 1] for j in range(4)], None,
                        [(0, Lc), (Lc, T)])
                if sect == 2:
                    kb.op("act", lambda e: e.activation(out=vc[:], in_=cacc[:], func=AF.Silu), r=[cacc], w=[vc])
                    continue
                kb.op("act", lambda e: e.activation(out=cacc[:], in_=cacc[:], func=AF.Silu), r=[cacc], w=[cacc])
                for gi, (g0, gs) in enumerate(groups_of(T)):
                    sq_, rs_ = sqt[gi % 2], rst[gi % 2]
                    pn_ = bankF[gi % 4]
                    kb.op("act", lambda e, sq_=sq_, g0=g0, gs=gs: e.activation(out=sq_[:, 0:gs], in_=cacc[:, g0:g0 + gs],
                                                                              func=AF.Square), r=[cacc], w=[sq_])
                    kb.op("pe", lambda e, pn_=pn_, sq_=sq_, gs=gs: e.matmul(pn_[:, 0:gs], lhsT=ones_f[:], rhs=sq_[:, 0:gs],
                                                                          start=True, stop=True), r=[ones_f, sq_], w=[pn_])
                    kb.op("act", lambda e, pn_=pn_, rs_=rs_, gs=gs: e.activation(out=rs_[:, 0:gs], in_=pn_[:, 0:gs],
                                                                               func=AF.Sqrt, bias=EPS), r=[pn_], w=[rs_])
                    kb.op("dve", lambda e, rs_=rs_, gs=gs: e.reciprocal(out=rs_[:, 0:gs], in_=rs_[:, 0:gs]), r=[rs_], w=[rs_])
                    kb.op("dve", lambda e, rs_=rs_, dst=dst, g0=g0, gs=gs, sect=sect: e.scalar_tensor_tensor(
                        out=dst[:, g0:g0 + gs], in0=cacc[:, g0:g0 + gs], scalar=(SCALE if sect == 0 else 1.0),
                        in1=rs_[:, 0:gs], op0=ALU.mult, op1=ALU.mult), r=[cacc, rs_], w=[dst])
            for d in range(2):
                kb.op("pool", lambda e, d=d: e.memset(Sf[d][:], 0.0), w=[Sf[d]])
                kb.op("pool", lambda e, d=d: e.memset(Sb[d][:], 0.0), w=[Sb[d]])
            kb.barrier()
            if "stopD2" in cfg.dbg:
                kb.flush()
                return
            import os as _os
            if _os.environ.get("GCUT") and h == 0:
                print("chunk loop starts at nins", kb.nins)
                kb.limit = kb.nins + int(_os.environ["GCUT"])
            def gdn_unit(d, s, h=h):
                ti = order[d][s]
                c0 = ti * 128
                n = d * 8 + h
                V = vsets[d]
                t = tsets[d * 2 + (s % 2)]
                SV = seqv[d]
                kb.capture()
                kb.op("pe", lambda e, V=V, c0=c0: e.transpose(V["tk"][:], kn[:, c0:c0 + 128], ident_b[:]),
                      r=[kn, ident_b], w=[V["tk"]])
                kb.op("pe", lambda e, V=V, c0=c0: e.transpose(V["tv"][:], vc[:, c0:c0 + 128], ident_b[:]),
                      r=[vc, ident_b], w=[V["tv"]])
                kb.op("dve", lambda e, V=V, t=t, ti=ti, n=n: e.tensor_scalar(
                    out=t["kg"][:], in0=V["tk"][:], scalar1=egcs[:, ti, n:n + 1], scalar2=None, op0=ALU.mult),
                    r=[V["tk"], egcs], w=[t["kg"]])
                kb.op("dve", lambda e, V=V, t=t, ti=ti, n=n: e.tensor_scalar(
                    out=t["kd"][:], in0=V["tk"][:], scalar1=kdsc[:, ti, n:n + 1], scalar2=None, op0=ALU.mult),
                    r=[V["tk"], kdsc], w=[t["kd"]])
                kb.op("dve", lambda e, V=V, t=t: e.tensor_copy(out=t["vT"][:], in_=V["tv"][:]), r=[V["tv"]], w=[t["vT"]])
                kb.op("pool", lambda e, t=t, d=d, ti=ti, n=n: e.tensor_scalar(
                    out=t["gL"][:], in0=L[d][:], scalar1=g[:, ti, n:n + 1], scalar2=None, op0=ALU.mult),
                    r=[L[d], g], w=[t["gL"]])
                kb.op("pe", lambda e, V=V, t=t: e.matmul(V["G"][:], lhsT=ones_f[:], rhs=t["gL"][:], start=True, stop=True),
                      r=[ones_f, t["gL"]], w=[V["G"]])
                kb.op("dve", lambda e, V=V, t=t: e.tensor_copy(out=t["Er"][:], in_=V["G"][:]), r=[V["G"]], w=[t["Er"]])
                kb.op("dve", lambda e, V=V, t=t, ti=ti, n=n: e.tensor_scalar(
                    out=t["Dm"][:], in0=t["Er"][:], scalar1=gcs[:, ti, n:n + 1], scalar2=0.0, op0=ALU.subtract, op1=ALU.min),
                    r=[t["Er"], gcs], w=[t["Dm"]])
                kb.op("act", lambda e, t=t: e.activation(out=t["Dm"][:], in_=t["Dm"][:], func=AF.Exp), r=[t["Dm"]], w=[t["Dm"]])
                kb.op("act", lambda e, V=V, t=t: e.activation(out=t["Er"][:], in_=t["Er"][:], func=AF.Exp), r=[t["Er"]], w=[t["Er"]])
                kb.op("dve", lambda e, t=t, c0=c0: e.tensor_tensor(out=t["qg"][:], in0=qn[:, c0:c0 + 128], in1=t["Er"][:],
                                                                    op=ALU.mult), r=[qn, t["Er"]], w=[t["qg"]])
                kb.op("pool", lambda e, t=t, d=d: e.tensor_tensor(out=t["dS"][:], in0=t["Dm"][:], in1=MS[d][:], op=ALU.mult),
                      r=[t["Dm"], MS[d]], w=[t["dS"]])
                kb.op("pool", lambda e, t=t, d=d: e.tensor_tensor(out=t["dI"][:], in0=t["Dm"][:], in1=L[d][:], op=ALU.mult),
                      r=[t["Dm"], L[d]], w=[t["dI"]])
                kb.op("pe", lambda e, V=V, c0=c0: e.matmul(V["KK"][:], lhsT=kn[:, c0:c0 + 128], rhs=kn[:, c0:c0 + 128],
                                                           start=True, stop=True), r=[kn], w=[V["KK"]])
                kb.op("dve", lambda e, V=V, t=t, ti=ti, n=n: e.scalar_tensor_tensor(
                    out=t["NTf"][:], in0=V["KK"][:], scalar=negb[:, ti, n:n + 1], in1=t["dS"][:], op0=ALU.mult, op1=ALU.mult),
                    r=[V["KK"], negb, t["dS"]], w=[t["NTf"]])
                kb.op("act", lambda e, t=t: e.copy(out=t["atb"][:], in_=t["NTf"][:]), r=[t["NTf"]], w=[t["atb"]])
                kb.op("pe", lambda e, V=V, t=t: e.transpose(V["tn"][:], t["atb"][:], ident_b[:]),
                      r=[t["atb"], ident_b], w=[V["tn"]])
                kb.op("dve", lambda e, V=V, t=t: e.tensor_copy(out=t["Nb"][:], in_=V["tn"][:]), r=[V["tn"]], w=[t["Nb"]])
                for li_, nm in enumerate(("Na", "Nc", "Nd")):
                    kb.op("pool", lambda e, t=t, nm=nm, li_=li_: e.tensor_tensor(out=t[nm][:], in0=t["Nb"][:], in1=OFF[li_][:],
                                                                              op=ALU.mult), r=[t["Nb"], OFF[li_]], w=[t[nm]])
                kb.op("dve", lambda e, t=t: e.tensor_tensor(out=t["NTf"][:], in0=t["NTf"][:], in1=BD16[:], op=ALU.mult),
                      r=[t["NTf"], BD16], w=[t["NTf"]])
                kb.op("act", lambda e, t=t: e.copy(out=t["PT1"][:, 0:128], in_=t["NTf"][:]), r=[t["NTf"]], w=[t["PT1"]])
                kb.op("pool", lambda e, t=t: e.tensor_tensor(out=t["Pb1"][:], in0=t["Nb"][:], in1=BD16[:], op=ALU.mult),
                      r=[t["Nb"], BD16], w=[t["Pb1"]])
                kb.op("dve", lambda e, t=t: e.tensor_tensor(out=t["TTf"][:], in0=t["NTf"][:], in1=ident_f[:], op=ALU.add),
                      r=[t["NTf"], ident_f], w=[t["TTf"]])
                kb.op("pe", lambda e, V=V, t=t: e.matmul(V["pm"][:, 0:128], lhsT=t["Pb1"][:], rhs=t["PT1"][:, 0:128],
                                                         start=True, stop=True), r=[t["Pb1"], t["PT1"]], w=[V["pm"]])
                kb.op("pe", lambda e, V=V, t=t: e.matmul(V["pn"][:], lhsT=t["PT1"][:, 0:128], rhs=t["Pb1"][:],
                                                         start=True, stop=True), r=[t["Pb1"], t["PT1"]], w=[V["pn"]])
                kb.op("dve", lambda e, V=V, t=t: e.tensor_copy(out=t["PT0"][:, 0:128], in_=V["pm"][:, 0:128]), r=[V["pm"]], w=[t["PT0"]])
                kb.op("act", lambda e, t=t: e.copy(out=t["PT0"][:, 128:256], in_=t["TTf"][:]), r=[t["TTf"]], w=[t["PT0"]])
                kb.op("dve", lambda e, V=V, t=t: e.tensor_copy(out=t["Pb0"][:], in_=V["pn"][:]), r=[V["pn"]], w=[t["Pb0"]])
                cur = 0
                LAST = 3
                for k in range(1, LAST + 1):
                    PTc, Pbc = t["PT%d" % cur], t["Pb%d" % cur]
                    PTn, Pbn = t["PT%d" % (1 - cur)], t["Pb%d" % (1 - cur)]
                    if k < LAST:
                        kb.op("pe", lambda e, V=V, PTc=PTc, Pbc=Pbc: e.matmul(V["pm"][:], lhsT=Pbc[:], rhs=PTc[:],
                                                                              start=True, stop=True), r=[PTc, Pbc], w=[V["pm"]])
                        kb.op("pe", lambda e, V=V, PTc=PTc, Pbc=Pbc: e.matmul(V["pn"][:], lhsT=PTc[:, 0:128], rhs=Pbc[:],
                                                                              start=True, stop=True), r=[PTc, Pbc], w=[V["pn"]])
                    else:
                        kb.op("pe", lambda e, V=V, PTc=PTc, Pbc=Pbc: e.matmul(V["pm"][:, 128:256], lhsT=Pbc[:],
                                                                              rhs=PTc[:, 128:256], start=True, stop=True),
                              r=[PTc, Pbc], w=[V["pm"]])
                    kb.op("dve", lambda e, V=V, t=t: e.tensor_tensor(out=t["TTf"][:], in0=V["pm"][:, 128:256], in1=t["TTf"][:],
                                                                    op=ALU.add), r=[V["pm"], t["TTf"]], w=[t["TTf"]])
                    kb.op("act", lambda e, t=t, PTn=PTn: e.copy(out=PTn[:, 128:256], in_=t["TTf"][:]), r=[t["TTf"]], w=[PTn])
                    if k < LAST:
                        kb.op("dve", lambda e, V=V, PTn=PTn: e.tensor_copy(out=PTn[:, 0:128], in_=V["pm"][:, 0:128]), r=[V["pm"]], w=[PTn])
                        kb.op("dve", lambda e, V=V, Pbn=Pbn: e.tensor_copy(out=Pbn[:], in_=V["pn"][:]), r=[V["pn"]], w=[Pbn])
                    cur = 1 - cur
                for nm in ("Na", "Nc", "Nd"):
                    PTc = t["PT%d" % cur]
                    PTn = t["PT%d" % (1 - cur)]
                    kb.op("pe", lambda e, V=V, PTc=PTc: e.transpose(V["tn"][:], PTc[:, 128:256], ident_b[:]),
                          r=[PTc, ident_b], w=[V["tn"]])
                    kb.op("dve", lambda e, V=V, t=t: e.tensor_copy(out=t["Tn"][:], in_=V["tn"][:]), r=[V["tn"]], w=[t["Tn"]])
                    kb.op("pe", lambda e, V=V, t=t, nm=nm, PTc=PTc: e.matmul(V["pn"][:], lhsT=t[nm][:], rhs=PTc[:, 128:256],
                                                                             start=True, stop=True), r=[t[nm], PTc], w=[V["pn"]])
                    kb.op("dve", lambda e, V=V, t=t: e.tensor_copy(out=t["Wb"][:], in_=V["pn"][:]), r=[V["pn"]], w=[t["Wb"]])
                    kb.op("pe", lambda e, V=V, t=t: e.matmul(V["pm"][:, 128:256], lhsT=t["Tn"][:], rhs=t["Wb"][:],
                                                             start=True, stop=True), r=[t["Tn"], t["Wb"]], w=[V["pm"]])
                    kb.op("dve", lambda e, V=V, t=t: e.tensor_tensor(out=t["TTf"][:], in0=V["pm"][:, 128:256], in1=t["TTf"][:],
                                                                    op=ALU.add), r=[V["pm"], t["TTf"]], w=[t["TTf"]])
                    kb.op("act", lambda e, t=t, PTn=PTn: e.copy(out=PTn[:, 128:256], in_=t["TTf"][:]), r=[t["TTf"]], w=[PTn])
                    cur = 1 - cur
                TT = t["PT%d" % cur]
                kb.op("pe", lambda e, V=V, t=t, TT=TT: e.matmul(V["X"][:], lhsT=t["kg"][:], rhs=TT[:, 128:256],
                                                                start=True, stop=True), r=[t["kg"], TT], w=[V["X"]])
                kb.op("dve", lambda e, V=V, t=t: e.tensor_copy(out=t["XTb"][:], in_=V["X"][:]), r=[V["X"]], w=[t["XTb"]])
                kb.op("pe", lambda e, V=V, t=t, TT=TT: e.matmul(V["u0"][:], lhsT=TT[:, 128:256], rhs=t["vT"][:],
                                                                start=True, stop=True), r=[t["vT"], TT], w=[V["u0"]])
                kb.op("dve", lambda e, V=V, t=t, ti=ti, n=n: e.tensor_scalar(
                    out=t["u0b"][:], in0=V["u0"][:], scalar1=beta[:, ti, n:n + 1], scalar2=None, op0=ALU.mult),
                    r=[V["u0"], beta], w=[t["u0b"]])
                kb.op("pe", lambda e, V=V, c0=c0: e.matmul(V["at"][:], lhsT=kn[:, c0:c0 + 128], rhs=qn[:, c0:c0 + 128],
                                                           start=True, stop=True), r=[kn, qn], w=[V["at"]])
                kb.op("dve", lambda e, V=V, t=t: e.tensor_tensor(out=t["atb"][:], in0=V["at"][:], in1=t["dI"][:], op=ALU.mult),
                      r=[V["at"], t["dI"]], w=[t["atb"]])
                par = kb.end_capture()
                kb.capture()
                kb.op("pe", lambda e, SV=SV, t=t, d=d: e.matmul(SV["wS"][:], lhsT=t["XTb"][:], rhs=Sb[d][:], start=True, stop=True),
                      r=[t["XTb"], Sb[d]], w=[SV["wS"]])
                kb.op("dve", lambda e, SV=SV, t=t, ti=ti, n=n: e.scalar_tensor_tensor(
                    out=t["vn"][:], in0=SV["wS"][:], scalar=negb[:, ti, n:n + 1], in1=t["u0b"][:], op0=ALU.mult, op1=ALU.add),
                    r=[SV["wS"], negb, t["u0b"]], w=[t["vn"]])
                kb.op("pe", lambda e, SV=SV, t=t: e.matmul(SV["sn"][:], lhsT=t["kd"][:], rhs=t["vn"][:], start=True, stop=True),
                      r=[t["kd"], t["vn"]], w=[SV["sn"]])
                kb.op("pe", lambda e, SV=SV, t=t, d=d: e.matmul(SV["o"][:], lhsT=t["qg"][:], rhs=Sb[d][:], start=True, stop=False),
                      r=[t["qg"], Sb[d]], w=[SV["o"]])
                kb.op("pe", lambda e, SV=SV, t=t: e.matmul(SV["o"][:], lhsT=t["atb"][:], rhs=t["vn"][:], start=False, stop=True),
                      r=[t["atb"], t["vn"]], w=[SV["o"]])
                kb.op("dve", lambda e, SV=SV, d=d, ti=ti, n=n: e.scalar_tensor_tensor(
                    out=Sf[d][:], in0=Sf[d][:], scalar=etot[:, ti, n:n + 1], in1=SV["sn"][:], op0=ALU.mult, op1=ALU.add),
                    r=[Sf[d], etot, SV["sn"]], w=[Sf[d]])
                kb.op("act", lambda e, d=d: e.copy(out=Sb[d][:], in_=Sf[d][:]), r=[Sf[d]], w=[Sb[d]])
                kb.op("dve", lambda e, SV=SV, t=t: e.tensor_copy(out=t["osb"][:], in_=SV["o"][:]), r=[SV["o"]], w=[t["osb"]])
                kb.dma("sp", OG[d, c0:c0 + 128, h * 128:(h + 1) * 128], t["osb"][:], r=[t["osb"]])
                sq_ = kb.end_capture()
                return par, sq_
            nxt = [gdn_unit(0, 0), gdn_unit(1, 0)]
            kb.emit_rr([nxt[0][0], nxt[1][0]])
            for s in range(NT):
                cur_ = nxt
                lists = [cur_[0][1], cur_[1][1]]
                if s + 1 < NT:
                    nxt = [gdn_unit(0, s + 1), gdn_unit(1, s + 1)]
                    lists += [nxt[0][0], nxt[1][0]]
                kb.emit_rr(lists)
            kb.barrier()
            if "stopD3" in cfg.dbg or "stopD4" in cfg.dbg:
                kb.flush()
                return
        kb.barrier()
        kb.flush()
    with ExitStack() as st:
        ptv = kb.ps(st, "n_ptv", [128, 128])
        nw = kb.sb(st, "n_nw", [128, 1], F32)
        load_vec_fm(st, ptv, nw, nw[:], W["gdn_norm_w"][li:li + 1, :], 1)
        o0 = [kb.sb(st, "n_o0%d" % i, [128, 1024], F32) for i in range(2)]
        o1 = [kb.sb(st, "n_o1%d" % i, [128, 1024], F32) for i in range(2)]
        sq = kb.sb(st, "n_sq", [128, 1024], F32)
        ss = kb.sb(st, "n_ss", [128, 8], F32)
        yb = [kb.sb(st, "n_yb%d" % i, [128, 8, 128], BF16) for i in range(2)]
        zt = [kb.sb(st, "n_zt%d" % i, [128, 8, 128], BF16) for i in range(2)]
        sz = kb.sb(st, "n_sz", [128, 8, 128], F32)
        ob = [kb.sb(st, "n_ob%d" % i, [128, 8, 128], BF16) for i in range(2)]
        ptr = [kb.ps(st, "n_ptr%d" % i, [128, 8, 128], BF16) for i in range(2)]
        for ti in range(NT):
            a_, b_, y_, z_, o_, p_ = o0[ti % 2], o1[ti % 2], yb[ti % 2], zt[ti % 2], ob[ti % 2], ptr[ti % 2]
            c0 = ti * 128
            kb.dma("sp", a_[:], OG[0, c0:c0 + 128, :], w=[a_])
            kb.dma("pool", b_[:], OG[1, c0:c0 + 128, :], w=[b_])
            kb.dma("sp", z_[:], P[P_GZ:P_GZ + 1024, :].rearrange("(h p) t -> p h t", p=128)[:, :, c0:c0 + 128], w=[z_])
            kb.op("dve", lambda e, a_=a_, b_=b_: e.tensor_tensor(out=a_[:], in0=a_[:], in1=b_[:], op=ALU.add), r=[a_, b_], w=[a_])
            kb.op("act", lambda e, a_=a_: e.activation(out=sq[:], in_=a_[:], func=AF.Square), r=[a_], w=[sq])
            kb.op("dve", lambda e: e.tensor_reduce(out=ss[:], in_=sq[:].rearrange("p (h v) -> p h v", h=8), axis=AX.X, op=ALU.add),
                  r=[sq], w=[ss])
            kb.op("act", lambda e: e.activation(out=ss[:], in_=ss[:], func=AF.Sqrt, scale=1.0 / 128, bias=EPS), r=[ss], w=[ss])
            kb.op("dve", lambda e: e.reciprocal(out=ss[:], in_=ss[:]), r=[ss], w=[ss])
            kb.op("dve", lambda e, a_=a_, y_=y_: e.tensor_tensor(
                out=y_[:], in0=a_[:].rearrange("p (h v) -> p h v", h=8), in1=ss[:].unsqueeze(2).to_broadcast([128, 8, 128]),
                op=ALU.mult), r=[a_, ss], w=[y_])
            for hh in range(8):
                kb.op("pe", lambda e, p_=p_, y_=y_, hh=hh: e.transpose(p_[:, hh, :], y_[:, hh, :], ident_b[:]),
                      r=[y_, ident_b], w=[p_])
            kb.op("act", lambda e, z_=z_: e.activation(out=sz[:], in_=z_[:], func=AF.Silu), r=[z_], w=[sz])
            kb.op("dve", lambda e, p_=p_, o_=o_: e.scalar_tensor_tensor(out=o_[:], in0=p_[:], scalar=nw[:, 0:1], in1=sz[:],
                                                                       op0=ALU.mult, op1=ALU.mult), r=[p_, nw, sz], w=[o_])
            kb.dma("sp", BR[1].rearrange("(h p) t -> p h t", p=128)[:, :, c0:c0 + 128], o_[:], r=[o_])
        kb.barrier()
        kb.flush()


def phase_ssd(kb, cfg, li, W, C, P, PS, XC, YS, BR, load_vec_fm):
    T, Lc, rows = cfg.T, cfg.Lc, cfg.rows
    NT = T // 128
    NTc = Lc // 128
    cpc = 128 // rows
    ident_f, ident_b, ones_f = C["ident_f"], C["ident_b"], C["ones_f"]
    with ExitStack() as st:
        ptv = kb.ps(st, "s_ptv", [128, 128])
        cw = kb.sb(st, "s_cw", [128, 48], F32)
        cb = kb.sb(st, "s_cb", [128, 12], F32)
        load_vec_fm(st, ptv, cw, cw[:], W["ssd_conv_w"][li].rearrange("j (k p) -> (j k) p", p=128), 48)
        load_vec_fm(st, ptv, cb, cb[:], W["ssd_conv_b"][li].rearrange("(k p) -> k p", p=128), 12)
        raw = [kb.sb(st, "s_raw%d" % i, [128, T], BF16) for i in range(2)]
        xp = [kb.sb(st, "s_xp%d" % i, [128, T], BF16) for i in range(2)]
        acc = kb.sb(st, "s_acc", [128, T], F32)
        xo = [kb.sb(st, "s_xo%d" % i, [128, T], BF16) for i in range(2)]
        for ct in range(12):
            r_, p_, o_ = raw[ct % 2], xp[ct % 2], xo[ct % 2]
            kb.dma("sp", r_[:], P[P_XS + ct * 128:P_XS + (ct + 1) * 128, :], w=[r_])
            kb.op("act", lambda e, r_=r_, p_=p_: e.copy(out=p_[:, 0:Lc], in_=r_[:, 0:Lc]), r=[r_], w=[p_])
            kb.op("pool", lambda e, r_=r_, p_=p_: e.tensor_copy(
                out=p_[:, Lc:T].rearrange("p (w r) -> p w r", r=rows),
                in_=r_[:, Lc:T].rearrange("p (r w) -> p w r", w=64)), r=[r_], w=[p_])
            conv_fm(kb, "dve", acc, p_, cw, [cw[:, j * 12 + ct:j * 12 + ct + 1] for j in range(4)], cb[:, ct:ct + 1],
                    [(0, Lc), (Lc, T)], bias_buf=cb)
            kb.op("act", lambda e, o_=o_: e.activation(out=o_[:], in_=acc[:], func=AF.Silu), r=[acc], w=[o_])
            kb.dma("sp", XC[ct * 128:(ct + 1) * 128, :], o_[:], r=[o_])
        kb.barrier()
        kb.flush()
    with ExitStack() as st:
        L = [kb.sb(st, "s_L%d" % d, [128, 128], F32) for d in range(2)]
        mk_tri(kb, L[0], [[1, 128]], -1, ALU.is_ge)
        mk_tri(kb, L[1], [[-1, 128]], 1, ALU.is_ge)
        alog = kb.sb(st, "s_alog", [128, 32], F32)
        dtb = kb.sb(st, "s_dtb", [128, 32], F32)
        dsk2 = kb.sb(st, "s_dsk2", [128, 32], F32)
        dsk = kb.sb(st, "s_dsk", [128, 16], F32)
        kb.dma("sp", alog[:], W["ssd_a_log"][li:li + 1, :].to_broadcast([128, 32]), w=[alog])
        kb.dma("sp", dtb[:], W["ssd_dt_bias"][li:li + 1, :].to_broadcast([128, 32]), w=[dtb])
        kb.dma("sp", dsk2[:], W["ssd_d"][li:li + 1, :].to_broadcast([128, 32]), w=[dsk2])
        kb.op("act", lambda e: e.activation(out=alog[:], in_=alog[:], func=AF.Exp), r=[alog], w=[alog])
        kb.op("dve", lambda e: e.tensor_scalar(out=alog[:], in0=alog[:], scalar1=-1.0, scalar2=None, op0=ALU.mult),
              r=[alog], w=[alog])
        kb.op("dve", lambda e: e.tensor_tensor(out=dsk[:], in0=dsk2[:, 0:16], in1=dsk2[:, 16:32], op=ALU.add), r=[dsk2], w=[dsk])
        STf = [kb.sb(st, "s_STf%d" % d, [128, 2, 512], F32) for d in range(2)]
        STb = [kb.sb(st, "s_STb%d" % d, [128, 2, 512], BF16) for d in range(2)]
        for d in range(2):
            kb.op("pool", lambda e, d=d: e.memset(STf[d][:], 0.0), w=[STf[d]])
            kb.op("pool", lambda e, d=d: e.memset(STb[d][:], 0.0), w=[STb[d]])
        pxT = kb.ps(st, "s_pxT", [128, 8, 128], BF16)
        pbT = kb.ps(st, "s_pbT", [128, 2, 128], BF16)
        psm = kb.ps(st, "s_psm", [128, 512])
        pA = kb.ps(st, "s_pA", [128, 4, 128])
        pyI = kb.ps(st, "s_pyI", [128, 512])
        pyS = kb.ps(st, "s_pyS", [128, 512])
        psn = kb.ps(st, "s_psn", [128, 512])

        def tset(u):
            t = {}
            t["X"] = kb.sb(st, "s_X%d" % u, [128, 12, 128], BF16)
            t["dtr"] = kb.sb(st, "s_dtr%d" % u, [128, 32], F32)
            t["dt"] = kb.sb(st, "s_dt%d" % u, [128, 32], F32)
            t["a"] = kb.sb(st, "s_a%d" % u, [128, 16], F32)
            t["acs"] = kb.sb(st, "s_acs%d" % u, [128, 16], F32)
            t["eacs"] = kb.sb(st, "s_eacs%d" % u, [128, 16], F32)
            t["dsc"] = kb.sb(st, "s_dsc%d" % u, [128, 16], F32)
            t["etot"] = kb.sb(st, "s_etot%d" % u, [128, 16], F32)
            t["xT"] = kb.sb(st, "s_xT%d" % u, [128, 16, 64], BF16)
            t["xdt"] = kb.sb(st, "s_xdt%d" % u, [128, 16, 64], BF16)
            t["xdec"] = kb.sb(st, "s_xdec%d" % u, [128, 16, 64], BF16)
            t["bT"] = kb.sb(st, "s_bT%d" % u, [128, 2, 128], BF16)
            t["cbL"] = kb.sb(st, "s_cbL%d" % u, [128, 2, 128], F32)
            t["rhs2"] = kb.sb(st, "s_rhs2%d" % u, [128, 16, 128], F32)
            t["Dm"] = kb.sb(st, "s_Dm%d" % u, [128, 4, 128], F32)
            t["MT"] = kb.sb(st, "s_MT%d" % u, [128, 16, 128], BF16)
            t["ys"] = kb.sb(st, "s_ys%d" % u, [128, 16, 64], F32)
            t["y"] = kb.sb(st, "s_y%d" % u, [128, 16, 64], F32)
            return t
        tsets = [tset(0), tset(1)]
        order = [list(range(NT)), list(range(NTc - 1, -1, -1)) + list(range(NT - 1, NTc - 1, -1))]
        PSlat = PS[Lc:T, :].rearrange("(r w) n -> w r n", w=64)
        def ssd_unit(d, s):
            ti = order[d][s]
            c0 = ti * 128
            t = tsets[d]
            kb.capture()
            X = t["X"]
            kb.dma("sp", X[:], XC.rearrange("(k p) t -> p k t", p=128)[:, :, c0:c0 + 128], w=[X])
            if ti < NTc:
                kb.dma("pool", t["dtr"][:], PS[c0:c0 + 128, 32:64], w=[t["dtr"]])
            else:
                w0 = (ti - NTc) * cpc
                for wi in range(cpc):
                    kb.dma("pool", t["dtr"][wi * rows:(wi + 1) * rows, :], PSlat[w0 + wi, :, 32:64], w=[t["dtr"]])
            kb.op("dve", lambda e, t=t: e.tensor_tensor(out=t["dt"][:], in0=t["dtr"][:], in1=dtb[:], op=ALU.add),
                  r=[t["dtr"], dtb], w=[t["dt"]])
            kb.op("act", lambda e, t=t: e.activation(out=t["dt"][:], in_=t["dt"][:], func=AF.Exp), r=[t["dt"]], w=[t["dt"]])
            kb.op("act", lambda e, t=t: e.activation(out=t["dt"][:], in_=t["dt"][:], func=AF.Ln, bias=1.0), r=[t["dt"]], w=[t["dt"]])
            dts = lambda t=t, d=d: t["dt"][:, d * 16:(d + 1) * 16]
            kb.op("dve", lambda e, t=t, d=d: e.tensor_tensor(out=t["a"][:], in0=t["dt"][:, d * 16:(d + 1) * 16],
                                                              in1=alog[:, d * 16:(d + 1) * 16], op=ALU.mult),
                  r=[t["dt"], alog], w=[t["a"]])
            kb.op("pe", lambda e, t=t, d=d: e.matmul(psm[:, 0:16], lhsT=L[d][:], rhs=t["a"][:], start=True, stop=True),
                  r=[L[d], t["a"]], w=[psm])
            kb.op("pe", lambda e, t=t: e.matmul(psm[:, 16:32], lhsT=ones_f[:], rhs=t["a"][:], start=True, stop=True),
                  r=[ones_f, t["a"]], w=[psm])
            kb.op("dve", lambda e, t=t: e.tensor_copy(out=t["acs"][:], in_=psm[:, 0:16]), r=[psm], w=[t["acs"]])
            kb.op("act", lambda e, t=t: e.activation(out=t["eacs"][:], in_=t["acs"][:], func=AF.Exp), r=[t["acs"]], w=[t["eacs"]])
            kb.op("dve", lambda e, t=t: e.tensor_copy(out=t["etot"][:], in_=psm[:, 16:32]), r=[psm], w=[t["etot"]])
            kb.op("act", lambda e, t=t: e.activation(out=t["etot"][:], in_=t["etot"][:], func=AF.Exp), r=[t["etot"]], w=[t["etot"]])
            kb.op("dve", lambda e, t=t: e.tensor_tensor(out=t["dsc"][:], in0=psm[:, 16:32], in1=t["acs"][:], op=ALU.subtract),
                  r=[psm, t["acs"]], w=[t["dsc"]])
            kb.op("act", lambda e, t=t: e.activation(out=t["dsc"][:], in_=t["dsc"][:], func=AF.Exp), r=[t["dsc"]], w=[t["dsc"]])
            for ct in range(8):
                kb.op("pe", lambda e, X=X, ct=ct: e.transpose(pxT[:, ct, :], X[:, ct, :], ident_b[:]), r=[X, ident_b], w=[pxT])
            for g_ in range(2):
                kb.op("pe", lambda e, X=X, g_=g_: e.transpose(pbT[:, g_, :], X[:, 8 + g_, :], ident_b[:]), r=[X, ident_b], w=[pbT])
            kb.op("dve", lambda e, t=t: e.tensor_copy(out=t["xT"][:], in_=pxT[:].rearrange("p c (e q) -> p (c e) q", q=64)),
                  r=[pxT], w=[t["xT"]])
            kb.op("dve", lambda e, t=t: e.tensor_copy(out=t["bT"][:], in_=pbT[:]), r=[pbT], w=[t["bT"]])
            kb.op("dve", lambda e, t=t, d=d: e.tensor_tensor(
                out=t["xdt"][:], in0=t["xT"][:], in1=t["dt"][:, d * 16:(d + 1) * 16].unsqueeze(2).to_broadcast([128, 16, 64]),
                op=ALU.mult), r=[t["xT"], t["dt"]], w=[t["xdt"]])
            kb.op("dve", lambda e, t=t: e.tensor_tensor(
                out=t["xdec"][:], in0=t["xdt"][:], in1=t["dsc"][:].unsqueeze(2).to_broadcast([128, 16, 64]),
                op=ALU.mult), r=[t["xdt"], t["dsc"]], w=[t["xdec"]])
            for g_ in range(2):
                kb.op("pe", lambda e, X=X, g_=g_: e.matmul(psm[:, 128 + g_ * 128:256 + g_ * 128], lhsT=X[:, 8 + g_, :],
                                                           rhs=X[:, 10 + g_, :], start=True, stop=True), r=[X], w=[psm])
            kb.op("dve", lambda e, t=t, d=d: e.tensor_tensor(
                out=t["cbL"][:], in0=psm[:, 128:384].rearrange("p (g l) -> p g l", g=2),
                in1=L[d][:].unsqueeze(1).to_broadcast([128, 2, 128]), op=ALU.mult), r=[psm, L[d]], w=[t["cbL"]])
            kb.op("pool", lambda e, t=t, d=d: e.tensor_tensor(
                out=t["rhs2"][:], in0=L[d][:].unsqueeze(1).to_broadcast([128, 16, 128]),
                in1=t["a"][:].unsqueeze(2).to_broadcast([128, 16, 128]), op=ALU.mult), r=[L[d], t["a"]], w=[t["rhs2"]])
            for hq in range(4):
                g_ = hq // 2
                kb.op("pe", lambda e, t=t, hq=hq: e.matmul(pA[:], lhsT=ones_f[:], rhs=t["rhs2"][:, hq * 4:(hq + 1) * 4, :],
                                                           start=True, stop=True), r=[ones_f, t["rhs2"]], w=[pA])
                kb.op("dve", lambda e, t=t, hq=hq: e.tensor_tensor(
                    out=t["Dm"][:], in0=pA[:], in1=t["acs"][:, hq * 4:(hq + 1) * 4].unsqueeze(2).to_broadcast([128, 4, 128]),
                    op=ALU.subtract), r=[pA, t["acs"]], w=[t["Dm"]])
                kb.op("dve", lambda e, t=t: e.tensor_scalar(out=t["Dm"][:], in0=t["Dm"][:], scalar1=0.0, scalar2=None,
                                                            op0=ALU.min), r=[t["Dm"]], w=[t["Dm"]])
                kb.op("act", lambda e, t=t: e.activation(out=t["Dm"][:], in_=t["Dm"][:], func=AF.Exp), r=[t["Dm"]], w=[t["Dm"]])
                kb.op("dve", lambda e, t=t, hq=hq, g_=g_: e.tensor_tensor(
                    out=t["MT"][:, hq * 4:(hq + 1) * 4, :], in0=t["Dm"][:],
                    in1=t["cbL"][:, g_, :].unsqueeze(1).to_broadcast([128, 4, 128]), op=ALU.mult),
                    r=[t["Dm"], t["cbL"]], w=[t["MT"]])
            for g_ in range(2):
                for e_ in range(8):
                    hh = g_ * 8 + e_
                    kb.op("pe", lambda e, t=t, hh=hh, e_=e_: e.matmul(pyI[:, e_ * 64:(e_ + 1) * 64], lhsT=t["MT"][:, hh, :],
                                                                      rhs=t["xdt"][:, hh, :], start=True, stop=True),
                          r=[t["MT"], t["xdt"]], w=[pyI])
                kb.op("pe", lambda e, X=X, g_=g_, d=d: e.matmul(pyS[:], lhsT=X[:, 10 + g_, :], rhs=STb[d][:, g_, :],
                                                                start=True, stop=True), r=[X, STb[d]], w=[pyS])
                kb.op("dve", lambda e, t=t, g_=g_: e.tensor_tensor(
                    out=t["ys"][:, g_ * 8:(g_ + 1) * 8, :], in0=pyS[:].rearrange("p (e q) -> p e q", q=64),
                    in1=t["eacs"][:, g_ * 8:(g_ + 1) * 8].unsqueeze(2).to_broadcast([128, 8, 64]), op=ALU.mult),
                    r=[pyS, t["eacs"]], w=[t["ys"]])
                kb.op("dve", lambda e, t=t, g_=g_: e.tensor_tensor(
                    out=t["y"][:, g_ * 8:(g_ + 1) * 8, :], in0=pyI[:].rearrange("p (e q) -> p e q", q=64),
                    in1=t["ys"][:, g_ * 8:(g_ + 1) * 8, :], op=ALU.add), r=[pyI, t["ys"]], w=[t["y"]])
                kb.op("pe", lambda e, t=t, g_=g_: e.matmul(
                    psn[:], lhsT=t["bT"][:, g_, :], rhs=t["xdec"][:, g_ * 8:(g_ + 1) * 8, :].rearrange("p e q -> p (e q)"),
                    start=True, stop=True), r=[t["bT"], t["xdec"]], w=[psn])
                kb.op("dve", lambda e, t=t, g_=g_, d=d: e.tensor_tensor(
                    out=STf[d][:, g_, :].rearrange("p (e q) -> p e q", q=64),
                    in0=STf[d][:, g_, :].rearrange("p (e q) -> p e q", q=64),
                    in1=t["etot"][:, g_ * 8:(g_ + 1) * 8].unsqueeze(2).to_broadcast([128, 8, 64]), op=ALU.mult),
                    r=[STf[d], t["etot"]], w=[STf[d]])
                kb.op("dve", lambda e, g_=g_, d=d: e.tensor_tensor(out=STf[d][:, g_, :], in0=STf[d][:, g_, :], in1=psn[:],
                                                                    op=ALU.add), r=[STf[d], psn], w=[STf[d]])
                kb.op("act", lambda e, g_=g_, d=d: e.copy(out=STb[d][:, g_, :], in_=STf[d][:, g_, :]), r=[STf[d]], w=[STb[d]])
            if d == 0:
                kb.op("dve", lambda e, t=t: e.tensor_tensor(
                    out=t["ys"][:], in0=t["xT"][:], in1=dsk[:].unsqueeze(2).to_broadcast([128, 16, 64]), op=ALU.mult),
                    r=[t["xT"], dsk], w=[t["ys"]])
                kb.op("dve", lambda e, t=t: e.tensor_tensor(out=t["y"][:], in0=t["y"][:], in1=t["ys"][:], op=ALU.add),
                      r=[t["y"], t["ys"]], w=[t["y"]])
            kb.dma("sp", YS[d, c0:c0 + 128, :], t["y"][:].rearrange("p e q -> p (e q)"), r=[t["y"]])
            return kb.end_capture()
        for s in range(NT):
            kb.emit_rr([ssd_unit(0, s)])
            kb.emit_rr([ssd_unit(1, s)])
        kb.barrier()
        kb.flush()
    with ExitStack() as st:
        ptv = kb.ps(st, "z_ptv", [128, 128])
        nw = kb.sb(st, "z_nw", [128, 8], F32)
        load_vec_fm(st, ptv, nw, nw[:], W["ssd_norm_w"][li].rearrange("(k p) -> k p", p=128), 8)
        y0 = [kb.sb(st, "z_y0%d" % i, [128, 1024], F32) for i in range(2)]
        y1 = [kb.sb(st, "z_y1%d" % i, [128, 1024], F32) for i in range(2)]
        zt = [kb.sb(st, "z_zt%d" % i, [128, 8, 128], BF16) for i in range(2)]
        sz = kb.sb(st, "z_sz", [128, 8, 128], F32)
        yz = kb.sb(st, "z_yz", [128, 8, 128], F32)
        sq = kb.sb(st, "z_sq", [128, 8, 128], F32)
        rs = kb.sb(st, "z_rs", [128, 2, 128], F32)
        ob = [kb.sb(st, "z_ob%d" % i, [128, 8, 128], BF16) for i in range(2)]
        pt = [kb.ps(st, "z_pt%d" % i, [128, 4, 128]) for i in range(4)]
        pss = kb.ps(st, "z_pss", [128, 2, 128])
        YSlat = [YS[d, Lc:T, :].rearrange("(w r) n -> r w n", r=rows) for d in range(2)]
        for ti in range(NT):
            a_, b_, z_, o_ = y0[ti % 2], y1[ti % 2], zt[ti % 2], ob[ti % 2]
            c0 = ti * 128
            if ti < NTc:
                kb.dma("sp", a_[:], YS[0, c0:c0 + 128, :], w=[a_])
                kb.dma("pool", b_[:], YS[1, c0:c0 + 128, :], w=[b_])
            else:
                r0 = (ti - NTc) * 2
                for k in range(2):
                    kb.dma("sp", a_[k * 64:(k + 1) * 64, :], YSlat[0][r0 + k], w=[a_])
                    kb.dma("pool", b_[k * 64:(k + 1) * 64, :], YSlat[1][r0 + k], w=[b_])
            kb.dma("sp", z_[:], P[P_SZ:P_SZ + 1024, :].rearrange("(k p) t -> p k t", p=128)[:, :, c0:c0 + 128], w=[z_])
            kb.op("dve", lambda e, a_=a_, b_=b_: e.tensor_tensor(out=a_[:], in0=a_[:], in1=b_[:], op=ALU.add), r=[a_, b_], w=[a_])
            kb.op("act", lambda e, z_=z_: e.activation(out=sz[:], in_=z_[:], func=AF.Silu), r=[z_], w=[sz])
            for hh in range(2):
                p = pt[(ti * 2 + hh) % 4]
                for j in range(4):
                    kc = hh * 4 + j
                    kb.op("pe", lambda e, p=p, j=j, kc=kc, a_=a_: e.transpose(p[:, j, :], a_[:, kc * 128:(kc + 1) * 128], ident_f[:]),
                          r=[a_, ident_f], w=[p])
                kb.op("dve", lambda e, p=p, hh=hh: e.tensor_tensor(out=yz[:, hh * 4:(hh + 1) * 4, :], in0=p[:],
                                                                    in1=sz[:, hh * 4:(hh + 1) * 4, :], op=ALU.mult),
                      r=[p, sz], w=[yz])
            kb.op("act", lambda e: e.activation(out=sq[:], in_=yz[:], func=AF.Square), r=[yz], w=[sq])
            for g_ in range(2):
                for j in range(4):
                    kb.op("pe", lambda e, g_=g_, j=j: e.matmul(pss[:, g_, :], lhsT=ones_f[:], rhs=sq[:, g_ * 4 + j, :],
                                                               start=(j == 0), stop=(j == 3)), r=[ones_f, sq], w=[pss])
            kb.op("act", lambda e: e.activation(out=rs[:], in_=pss[:], func=AF.Sqrt, scale=1.0 / 512, bias=EPS), r=[pss], w=[rs])
            kb.op("dve", lambda e: e.reciprocal(out=rs[:], in_=rs[:]), r=[rs], w=[rs])
            for ct in range(8):
                kb.op("dve", lambda e, ct=ct, o_=o_: e.scalar_tensor_tensor(
                    out=o_[:, ct, :], in0=yz[:, ct, :], scalar=nw[:, ct:ct + 1], in1=rs[:, ct // 4, :], op0=ALU.mult, op1=ALU.mult),
                    r=[yz, nw, rs], w=[o_])
            kb.dma("sp", BR[2].rearrange("(k p) t -> p k t", p=128)[:, :, c0:c0 + 128], o_[:], r=[o_])
        kb.barrier()
        kb.flush()
```
